# Optimizing a Trainium2 kernel written in Bass

```python
import math
import jax, jax.numpy as jnp
from jax import lax
import numpy as np

D_MODEL = 1024
BATCH = 4
SEQ = 4096
DEPTH = 2

PLE_DIM = 256
N_EVEN = (DEPTH + 1) // 2
N_ODD = DEPTH // 2
DEEPNORM_ALPHA = (2.0 * DEPTH) ** 0.25
DEEPNORM_BETA = (8.0 * DEPTH) ** -0.25
LN_EPS = 1e-5
A_WIDTH = D_MODEL // 2
A_GROUPS = 8
A_GROUP_DIM = A_WIDTH // A_GROUPS
A_CHUNK = 128
B_HEADS = 4
B_DV = (D_MODEL // 2) // B_HEADS
B_DK = B_DV // 2
B_GATE_RANK = 16
B_TAU = 16.0
B_CHUNK = 64
C_HEADS = 8
C_DH = D_MODEL // (2 * C_HEADS)
C_QBLOCK = 128
FFN_DIM = 2816
N_EXPERTS = 8
TOP_K = 2
EXPERT_DIM = 3584
MOE_BLOCK = 256
E_SPLIT_SIZES = [A_WIDTH, A_WIDTH, B_HEADS * B_DK, B_HEADS * B_DK, B_HEADS * B_DV, B_HEADS * B_DV]
E_IN_COLS = sum(E_SPLIT_SIZES) + B_GATE_RANK
E_MIX_WIDTH = A_WIDTH + B_HEADS * B_DV

kernel_name = "hybrid_gmlp_gla_diffattn_moe_deepnorm"

F32 = jnp.float32


def layer_norm(x, g, b):
    xf = x.astype(F32)
    mu = jnp.mean(xf, axis=-1, keepdims=True)
    var = jnp.mean(jnp.square(xf - mu), axis=-1, keepdims=True)
    return ((xf - mu) * lax.rsqrt(var + LN_EPS) * g + b).astype(x.dtype)


def rms_norm(x, g):
    xf = x.astype(F32)
    return (xf * lax.rsqrt(jnp.mean(jnp.square(xf), axis=-1, keepdims=True) + LN_EPS) * g).astype(x.dtype)


def spatial_gating(u, v, ln_g, ln_b, ws, bs):
    bn, s, _ = v.shape
    nc = s // A_CHUNK
    v = layer_norm(v, ln_g, ln_b).reshape(bn, nc, A_CHUNK, A_GROUPS, A_GROUP_DIM)
    causal = jnp.tril(jnp.ones((A_CHUNK, A_CHUNK), dtype=bool))
    w = jnp.where(causal, ws, 0)
    sg = jnp.einsum('gts,bcsgd->bctgd', w, v) + bs.T[:, :, None]
    return u * sg.reshape(bn, s, A_WIDTH)


def gla_chunked(q, k, v, log_a):
    bn, s, h, dk = q.shape
    dv = v.shape[-1]
    L = B_CHUNK
    nc = s // L
    q, k, v, log_a = [t.reshape(bn, nc, L, h, t.shape[-1]) for t in (q, k, v, log_a)]
    b = jnp.cumsum(log_a, axis=2)
    ref = b[:, :, L // 2 - 1:L // 2]
    b_last = b[:, :, L - 1:L]
    causal = jnp.tril(jnp.ones((L, L), dtype=bool))
    scores = jnp.einsum('bclhk,bcmhk->bchlm', q * jnp.exp(b - ref), k * jnp.exp(ref - b))
    scores = jnp.where(causal, scores, 0.0)
    o_intra = jnp.einsum('bchlm,bcmhv->bclhv', scores, v)
    kv = jnp.einsum('bclhk,bclhv->bchkv', k * jnp.exp(b_last - b), v)
    decay = jnp.exp(b_last[:, :, 0])

    def step(state, inp):
        d, kv_c = inp
        return d[..., None] * state + kv_c, state

    init = jnp.zeros((bn, h, dk, dv), F32)
    _, s_prev = lax.scan(step, init, (jnp.moveaxis(decay, 1, 0), jnp.moveaxis(kv, 1, 0)))
    s_prev = jnp.moveaxis(s_prev, 0, 1)
    o_inter = jnp.einsum('bclhk,bchkv->bclhv', q * jnp.exp(b), s_prev)
    return (o_intra + o_inter).reshape(bn, s, h, dv)


def even_mixer(x, w_in, a_ln_g, a_ln_b, a_ws, a_bs, b_w_gate_up, b_b_gate, b_norm_g, w_out):
    bn, s, _ = x.shape
    z = x @ w_in
    splits = np.cumsum(E_SPLIT_SIZES).tolist()
    u, v, q, k, vb, r, g_low = jnp.split(z, splits, axis=-1)
    ya = spatial_gating(jax.nn.gelu(u), jax.nn.gelu(v), a_ln_g, a_ln_b, a_ws, a_bs)
    q = q.reshape(bn, s, B_HEADS, B_DK).astype(F32) * (B_DK ** -0.5)
    k = k.reshape(bn, s, B_HEADS, B_DK).astype(F32)
    vb = vb.reshape(bn, s, B_HEADS, B_DV).astype(F32)
    log_a = jax.nn.log_sigmoid((g_low @ b_w_gate_up + b_b_gate).astype(F32)) / B_TAU
    log_a = log_a.reshape(bn, s, B_HEADS, B_DK)
    o = rms_norm(gla_chunked(q, k, vb, log_a), b_norm_g)
    yb = o.reshape(bn, s, B_HEADS * B_DV).astype(x.dtype) * jax.nn.silu(r)
    return jnp.concatenate([ya, yb], axis=-1) @ w_out


def diff_attention(x, w_qkv, lam_q1, lam_k1, lam_q2, lam_k2, subln_g, w_out, lambda_init):
    bn, s, _ = x.shape
    q, k, v = jnp.split(x @ w_qkv, 3, axis=-1)
    q = q.reshape(bn, s, C_HEADS, 2, C_DH) * (C_DH ** -0.5)
    k = k.reshape(bn, s, C_HEADS, 2, C_DH)
    v = v.reshape(bn, s, C_HEADS, 2 * C_DH)
    lam = (jnp.exp(jnp.sum(lam_q1 * lam_k1).astype(F32))
           - jnp.exp(jnp.sum(lam_q2 * lam_k2).astype(F32)) + lambda_init)
    nb = s // C_QBLOCK
    qb = jnp.moveaxis(q.reshape(bn, nb, C_QBLOCK, C_HEADS, 2, C_DH), 1, 0)
    kpos = jnp.arange(s)

    def block(args):
        qi, i = args
        sc = jnp.einsum('bqhcd,bkhcd->bhcqk', qi, k).astype(F32)
        qpos = i * C_QBLOCK + jnp.arange(C_QBLOCK)
        sc = jnp.where(kpos[None, :] <= qpos[:, None], sc, -jnp.inf)
        pr = jax.nn.softmax(sc, axis=-1)
        a = pr[:, :, 0] - lam * pr[:, :, 1]
        return jnp.einsum('bhqk,bkhe->bqhe', a.astype(v.dtype), v)

    o = lax.map(block, (qb, jnp.arange(nb)))
    o = jnp.moveaxis(o, 0, 1).reshape(bn, s, C_HEADS, 2 * C_DH)
    o = rms_norm(o, subln_g) * (1.0 - lambda_init)
    return o.reshape(bn, s, C_HEADS * 2 * C_DH) @ w_out


def swiglu(x, wg, wu, wd):
    return (jax.nn.silu(x @ wg) * (x @ wu)) @ wd


def moe_swiglu(x, w_router, wg, wu, wd):
    bn, s, d = x.shape
    n = bn * s
    xf = x.reshape(n, d)
    logits = (xf @ w_router).astype(F32)
    top_val, top_idx = lax.top_k(logits, TOP_K)
    gates = jax.nn.softmax(top_val, axis=-1)
    flat_e = top_idx.reshape(-1)
    flat_tok = jnp.repeat(jnp.arange(n, dtype=jnp.int32), TOP_K)
    flat_g = gates.reshape(-1)
    order = jnp.argsort(flat_e)
    se = flat_e[order]
    counts = jnp.bincount(flat_e, length=N_EXPERTS)
    padded = (counts + MOE_BLOCK - 1) // MOE_BLOCK * MOE_BLOCK
    start = jnp.cumsum(counts) - counts
    pend = jnp.cumsum(padded)
    pstart = pend - padded
    dest = pstart[se] + jnp.arange(n * TOP_K) - start[se]
    n_rows = n * TOP_K + N_EXPERTS * MOE_BLOCK
    n_blocks = n_rows // MOE_BLOCK
    row_tok = jnp.full((n_rows,), n, jnp.int32).at[dest].set(flat_tok[order])
    row_gate = jnp.zeros((n_rows,), F32).at[dest].set(flat_g[order])
    blk_e = jnp.minimum(jnp.searchsorted(pend, jnp.arange(n_blocks) * MOE_BLOCK, side='right'), N_EXPERTS - 1)
    x_pad = jnp.concatenate([xf, jnp.zeros((1, d), xf.dtype)], axis=0)
    xin = x_pad[row_tok].reshape(n_blocks, MOE_BLOCK, d)

    def expert_block(args):
        xb, e = args
        return (jax.nn.silu(xb @ wg[e]) * (xb @ wu[e])) @ wd[e]

    y = lax.map(expert_block, (xin, blk_e)).reshape(n_rows, d)
    out = jnp.zeros((n + 1, d), F32).at[row_tok].add(y.astype(F32) * row_gate[:, None])[:n]
    return out.astype(x.dtype).reshape(bn, s, d)


def per_layer_embed(x, p_i, w_proj, w_gate, b_gate):
    return x + jax.nn.sigmoid(x @ w_gate + b_gate) * (p_i @ w_proj)


def setup_inputs(seed: int = 0) -> dict:
    key = jax.random.key(seed)
    keys = iter(jax.random.split(key, 64))

    def nrm(shape, scale):
        return jax.random.normal(next(keys), shape, F32) * scale

    def gain(shape):
        return 1.0 + nrm(shape, 0.02)

    D = D_MODEL
    beta = DEEPNORM_BETA
    return {
        "x": nrm((BATCH, SEQ, D), 1.0),
        "p": nrm((DEPTH, BATCH, SEQ, PLE_DIM), 1.0),
        "e_w_in": nrm((N_EVEN, D, E_IN_COLS), D ** -0.5),
        "e_a_ln_g": gain((N_EVEN, A_WIDTH)),
        "e_a_ln_b": nrm((N_EVEN, A_WIDTH), 0.02),
        "e_a_ws": nrm((N_EVEN, A_GROUPS, A_CHUNK, A_CHUNK), A_CHUNK ** -0.5),
        "e_a_bs": gain((N_EVEN, A_GROUPS, A_CHUNK)),
        "e_b_w_gate_up": nrm((N_EVEN, B_GATE_RANK, B_HEADS * B_DK), B_GATE_RANK ** -0.5),
        "e_b_b_gate": nrm((N_EVEN, B_HEADS * B_DK), 0.02),
        "e_b_norm_g": gain((N_EVEN, B_DV)),
        "e_w_out": nrm((N_EVEN, E_MIX_WIDTH, D), E_MIX_WIDTH ** -0.5 * beta),
        "e_ln1_g": gain((N_EVEN, D)),
        "e_ln1_b": nrm((N_EVEN, D), 0.02),
        "e_ffn_wg": nrm((N_EVEN, D, FFN_DIM), D ** -0.5),
        "e_ffn_wu": nrm((N_EVEN, D, FFN_DIM), D ** -0.5),
        "e_ffn_wd": nrm((N_EVEN, FFN_DIM, D), FFN_DIM ** -0.5 * beta),
        "e_ln2_g": gain((N_EVEN, D)),
        "e_ln2_b": nrm((N_EVEN, D), 0.02),
        "o_w_qkv": nrm((N_ODD, D, 3 * C_HEADS * 2 * C_DH), D ** -0.5),
        "o_lam_q1": nrm((N_ODD, C_DH), 0.1),
        "o_lam_k1": nrm((N_ODD, C_DH), 0.1),
        "o_lam_q2": nrm((N_ODD, C_DH), 0.1),
        "o_lam_k2": nrm((N_ODD, C_DH), 0.1),
        "o_subln_g": gain((N_ODD, 2 * C_DH)),
        "o_w_out": nrm((N_ODD, C_HEADS * 2 * C_DH, D), (C_HEADS * 2 * C_DH) ** -0.5 * beta),
        "o_ln1_g": gain((N_ODD, D)),
        "o_ln1_b": nrm((N_ODD, D), 0.02),
        "o_router": nrm((N_ODD, D, N_EXPERTS), D ** -0.5),
        "o_exp_wg": nrm((N_ODD, N_EXPERTS, D, EXPERT_DIM), D ** -0.5),
        "o_exp_wu": nrm((N_ODD, N_EXPERTS, D, EXPERT_DIM), D ** -0.5),
        "o_exp_wd": nrm((N_ODD, N_EXPERTS, EXPERT_DIM, D), EXPERT_DIM ** -0.5 * beta),
        "o_ln2_g": gain((N_ODD, D)),
        "o_ln2_b": nrm((N_ODD, D), 0.02),
        "ple_w_proj": nrm((DEPTH, PLE_DIM, D), PLE_DIM ** -0.5),
        "ple_w_gate": nrm((DEPTH, D, D), D ** -0.5),
        "ple_b_gate": nrm((DEPTH, D), 0.02),
    }


def reference(x, p,
              e_w_in, e_a_ln_g, e_a_ln_b, e_a_ws, e_a_bs, e_b_w_gate_up, e_b_b_gate, e_b_norm_g,
              e_w_out, e_ln1_g, e_ln1_b, e_ffn_wg, e_ffn_wu, e_ffn_wd, e_ln2_g, e_ln2_b,
              o_w_qkv, o_lam_q1, o_lam_k1, o_lam_q2, o_lam_k2, o_subln_g, o_w_out, o_ln1_g, o_ln1_b,
              o_router, o_exp_wg, o_exp_wu, o_exp_wd, o_ln2_g, o_ln2_b,
              ple_w_proj, ple_w_gate, ple_b_gate):
    for i in range(DEPTH):
        j = i // 2
        if i % 2 == 0:
            m = even_mixer(x, e_w_in[j], e_a_ln_g[j], e_a_ln_b[j], e_a_ws[j], e_a_bs[j],
                           e_b_w_gate_up[j], e_b_b_gate[j], e_b_norm_g[j], e_w_out[j])
            x = layer_norm(DEEPNORM_ALPHA * x + m, e_ln1_g[j], e_ln1_b[j])
            f = swiglu(x, e_ffn_wg[j], e_ffn_wu[j], e_ffn_wd[j])
            x = layer_norm(DEEPNORM_ALPHA * x + f, e_ln2_g[j], e_ln2_b[j])
        else:
            lambda_init = 0.8 - 0.6 * math.exp(-0.3 * i)
            m = diff_attention(x, o_w_qkv[j], o_lam_q1[j], o_lam_k1[j], o_lam_q2[j], o_lam_k2[j],
                               o_subln_g[j], o_w_out[j], lambda_init)
            x = layer_norm(DEEPNORM_ALPHA * x + m, o_ln1_g[j], o_ln1_b[j])
            f = moe_swiglu(x, o_router[j], o_exp_wg[j], o_exp_wu[j], o_exp_wd[j])
            x = layer_norm(DEEPNORM_ALPHA * x + f, o_ln2_g[j], o_ln2_b[j])
        x = per_layer_embed(x, p[i], ple_w_proj[i], ple_w_gate[i], ple_b_gate[i])
    return x
```

```python
import numpy as np
import ml_dtypes
import concourse.bass as bass
import concourse.mybir as mybir
from concourse.bass_utils import run_bass_kernel_spmd

AF = mybir.ActivationFunctionType
ALU = mybir.AluOpType
AX = mybir.AxisListType
F32 = mybir.dt.float32
BF16 = mybir.dt.bfloat16
I32 = mybir.dt.int32


class Buf:
    __slots__ = ("name", "w", "r", "dsem", "dcount", "genw", "uid")
    _n = [0]

    def __init__(self, name):
        self.name = name
        self.w = None
        self.r = {}
        self.dsem = None
        self.dcount = 0
        self.genw = None
        Buf._n[0] += 1
        self.uid = Buf._n[0]


class Emitter:
    def __init__(self, nc):
        self.nc = nc
        self.eng = {"pe": nc.tensor, "dve": nc.vector, "act": nc.scalar, "pool": nc.gpsimd, "sp": nc.sync}
        self.ep = {e: 0 for e in self.eng}
        self.free_dsems = []
        self.sem = {e: nc.alloc_semaphore("sem_" + e) for e in self.eng}
        self.cnt = {e: 0 for e in self.eng}
        self.seen = {e: {} for e in self.eng}
        self.semobj = {}
        for e, s in self.sem.items():
            self.semobj[("e", e, 0)] = s
        self.nbuf = 0
        self.dbufs = []

    def buf(self, name=None):
        self.nbuf += 1
        return Buf(name or f"b{self.nbuf}")

    def _need(self, e, tok, out):
        if tok is None:
            return
        key, val = tok
        if key[0] == "e" and key[2] < self.ep[key[1]]:
            return
        if key == ("e", e, self.ep[e]):
            if e == "pe":
                return
        if self.seen[e].get(key, 0) >= val:
            return
        if out.get(key, 0) < val:
            out[key] = val

    def _collect(self, e, reads, writes):
        need = {}
        for b in reads:
            self._need(e, b.w, need)
        for b in writes:
            self._need(e, b.w, need)
            for k, v in b.r.items():
                self._need(e, (k, v), need)
        return need

    def _dowaits(self, e, need):
        for key, val in need.items():
            self.eng[e].wait_ge(self.semobj[key], val)
            self.seen[e][key] = val

    def op(self, e, fn, reads=(), writes=()):
        need = self._collect(e, reads, writes)
        self._dowaits(e, need)
        ins = fn(self.eng[e])
        ins.then_inc(self.sem[e], 1)
        self.cnt[e] += 1
        tok = (("e", e, self.ep[e]), self.cnt[e])
        for b in writes:
            b.w = tok
            b.r = {}
            b.genw = None
        for b in reads:
            if b.r.get(tok[0], 0) < tok[1]:
                b.r[tok[0]] = tok[1]
        return tok

    def dma(self, q, out, in_, dst, srcs=(), **kw):
        if dst.dsem is None:
            if self.free_dsems and (q != "pool" or self.nc.free_len() < 2):
                dst.dsem, dst.dcount = self.free_dsems.pop()
            else:
                self.nds = getattr(self, "nds", 0) + 1
                dst.dsem = self.nc.alloc_semaphore(f"dsem_{self.nds}")
            self.semobj[("d", dst.uid)] = dst.dsem
            self.dbufs.append(dst)
        dkey = ("d", dst.uid)
        same_gen = dst.w is not None and dst.w[0] == dkey and not dst.r and dst.genw is not None
        need = {}
        for b in srcs:
            self._need(q, b.w, need)
        if same_gen:
            for key, val in dst.genw.items():
                self._need(q, (key, val), need)
        else:
            gen = {}
            if dst.w is not None:
                gen[dst.w[0]] = dst.w[1]
            for k, v in dst.r.items():
                gen[k] = max(gen.get(k, 0), v)
            for key, val in gen.items():
                self._need(q, (key, val), need)
            dst.genw = gen
        self._dowaits(q, need)
        ins = self.eng[q].dma_start(out=out, in_=in_, **kw)
        dst.dcount += 16
        ins.then_inc(dst.dsem, 16)
        tok = (dkey, dst.dcount)
        dst.w = tok
        dst.r = {}
        for b in srcs:
            if b.r.get(dkey, 0) < dst.dcount:
                b.r[dkey] = dst.dcount
        return tok

    def barrier(self):
        for e in self.eng:
            need = {}
            for f in self.eng:
                if f != e and self.cnt[f] > 0:
                    self._need(e, (("e", f, self.ep[f]), self.cnt[f]), need)
            for b in self.dbufs:
                self._need(e, (("d", b.uid), b.dcount), need)
            self._dowaits(e, need)
        for e in self.eng:
            if self.cnt[e] > 12000:
                self.ep[e] += 1
                self.sem[e] = self.nc.alloc_semaphore(f"sem_{e}_{self.ep[e]}")
                self.semobj[("e", e, self.ep[e])] = self.sem[e]
                self.cnt[e] = 0

    def retire(self, bufs):
        for b in bufs:
            if b.dsem is not None:
                self.free_dsems.append((b.dsem, b.dcount))
                self.dbufs = [x for x in self.dbufs if x is not b]
                b.dsem = None

    def wait_all(self, e, bufs):
        need = {}
        for b in bufs:
            self._need(e, b.w, need)
        self._dowaits(e, need)


class T:
    def __init__(self, h, b, excl=False):
        self.h = h
        self.b = b
        self.excl = excl

    def __getitem__(self, idx):
        return self.h[idx]


def _bufs(xs):
    return [getattr(x, "b", x) for x in xs]


class KB:
    def __init__(self):
        import contextlib
        self.nc = bass.Bass("TRN2", target_bir_lowering=False)
        self.em = Emitter(self.nc)
        self.n = 0
        self.gstack = contextlib.ExitStack()
        self.stack = self.gstack

    def phase(self):
        import contextlib
        self.stack = contextlib.ExitStack()
        self.pbufs = []
        return self.stack

    def end_phase(self):
        self.em.barrier()
        self.em.retire(self.pbufs)
        self.pbufs = []
        self.stack.close()
        self.stack = self.gstack

    def din(self, name, shape, dt=F32):
        return self.nc.dram_tensor(name, list(shape), dt, kind="ExternalInput").ap()

    def dout(self, name, shape, dt=F32):
        return T(self.nc.dram_tensor(name, list(shape), dt, kind="ExternalOutput").ap(), self.em.buf(name))

    def dscr(self, name, shape, dt=F32):
        return T(self.nc.dram_tensor(name, list(shape), dt, kind="Internal").ap(), self.em.buf(name))

    def sb(self, name, shape, dt=F32):
        self.n += 1
        h = self.stack.enter_context(self.nc.sbuf_tensor(f"{name}_{self.n}", list(shape), dt))
        t = T(h, self.em.buf(name))
        if self.stack is not self.gstack:
            self.pbufs.append(t.b)
        return t

    def psum(self, name, shape, dt=F32):
        return self.gstack.enter_context(self.nc.psum_tensor(name, list(shape), dt))

    def view(self, ap, name="v"):
        return T(ap, self.em.buf(name))

    def op(self, e, fn, reads=(), writes=()):
        r, w = [], _bufs(writes)
        for x in reads:
            if getattr(x, "excl", False):
                w.append(x.b)
            else:
                r.append(getattr(x, "b", x))
        return self.em.op(e, fn, r, w)

    def dma(self, q, out, in_, dst, srcs=(), **kw):
        return self.em.dma(q, out, in_, getattr(dst, "b", dst), _bufs(srcs), **kw)

    def mm(self, out, lhsT, rhs, start, stop, reads, writes, skip=False):
        if skip:
            self.op("pe", lambda e: e.matmul(out, lhsT, rhs, start=start, stop=stop, skip_group_check=True), reads, writes)
        else:
            self.op("pe", lambda e: e.matmul(out, lhsT, rhs, start=start, stop=stop), reads, writes)

    def act(self, out, in_, func, reads, writes, bias=None, scale=None, eng="act"):
        kw = {}
        if bias is not None:
            kw["bias"] = bias
        if scale is not None:
            kw["scale"] = scale
        self.op("act", lambda e: e.activation(out=out, in_=in_, func=func, **kw), reads, writes)

    def tt(self, e, out, a, b, op, reads, writes):
        self.op(e, lambda g: g.tensor_tensor(out=out, in0=a, in1=b, op=op), reads, writes)

    def ts(self, e, out, a, s1, s2, op0, op1, reads, writes):
        if s2 is None:
            self.op(e, lambda g: g.tensor_scalar(out=out, in0=a, scalar1=s1, scalar2=None, op0=op0), reads, writes)
        else:
            self.op(e, lambda g: g.tensor_scalar(out=out, in0=a, scalar1=s1, scalar2=s2, op0=op0, op1=op1), reads, writes)

    def stt(self, e, out, a, s, b, op0, op1, reads, writes):
        self.op(e, lambda g: g.scalar_tensor_tensor(out=out, in0=a, scalar=s, in1=b, op0=op0, op1=op1), reads, writes)

    def copy(self, e, out, in_, reads, writes):
        if e == "act":
            self.op(e, lambda g: g.copy(out=out, in_=in_), reads, writes)
        else:
            self.op(e, lambda g: g.tensor_copy(out=out, in_=in_), reads, writes)

    def rsqrt(self, out, in_, scale, eps, reads, writes):
        self.op("act", lambda e: e.activation(out=out, in_=in_, func=AF.Sqrt, bias=eps, scale=scale), reads, writes)
        self.op("dve", lambda e: e.reciprocal(out=out, in_=out), writes, writes)

    def memset(self, e, ap, val, writes):
        self.op(e, lambda g: g.memset(ap, val), (), writes)


ALPHA = (2.0 * 2) ** 0.25
EPS = 1e-5
NT = 2048

COLS = {}
_c = 0
for _nm, _n in [("e_ln1_g", 8), ("e_ln1_b", 8), ("e_ln2_g", 8), ("e_ln2_b", 8), ("o_ln1_g", 8), ("o_ln1_b", 8),
                ("o_ln2_g", 8), ("o_ln2_b", 8), ("ple_b0", 8), ("ple_b1", 8), ("b_norm_g", 1), ("bsT", 8),
                ("flag", 1), ("nbias", 1), ("gsubc", 1)]:
    COLS[_nm] = (_c, _n)
    _c += _n
NCOLS = _c


def make_consts(k):
    c = {}
    it = k.sb("iota_i", [128, 128], I32)
    k.op("pool", lambda g: g.iota(it[:], pattern=[[1, 128]], base=0, channel_multiplier=-1), (), [it])
    d = k.sb("iota_f", [128, 128], F32)
    k.copy("dve", d[:], it[:], [it], [d])
    tri = k.sb("tri", [128, 128], F32)
    k.ts("dve", tri[:], d[:], 0.0, None, ALU.is_ge, None, [d], [tri])
    idf = k.sb("idf", [128, 128], F32)
    k.ts("dve", idf[:], d[:], 0.0, None, ALU.is_equal, None, [d], [idf])
    ident = k.sb("ident", [128, 128], BF16)
    k.copy("dve", ident[:], idf[:], [idf], [ident])
    trib = k.sb("trib", [128, 128], BF16)
    k.copy("dve", trib[:], tri[:], [tri], [trib])
    triblk = k.sb("triblk", [128, 128], F32)
    k.copy("dve", triblk[:], tri[:], [tri], [triblk])
    k.memset("dve", triblk[0:64, 64:128], 0.0, [triblk])
    onesblk = k.sb("onesblk", [128, 128], F32)
    k.memset("dve", onesblk[:], 0.0, [onesblk])
    k.memset("dve", onesblk[0:64, 0:64], 1.0, [onesblk])
    k.memset("dve", onesblk[64:128, 64:128], 1.0, [onesblk])
    tris = k.sb("tris", [128, 128], F32)
    k.ts("dve", tris[:], triblk[:], -1.0 / 16, None, ALU.mult, None, [triblk], [tris])
    dmat = k.sb("dmat", [128, 128], F32)
    k.tt("dve", dmat[:], onesblk[:], triblk[:], ALU.subtract, [onesblk, triblk], [dmat])
    k.ts("dve", dmat[:], dmat[:], -1.0 / 16, None, ALU.mult, None, [dmat], [dmat])
    triblkb = k.sb("triblkb", [128, 128], BF16)
    k.copy("dve", triblkb[:], triblk[:], [triblk], [triblkb])
    ones = k.sb("ones", [128, 128], BF16)
    k.memset("dve", ones[:], 1.0, [ones])
    rm0 = k.sb("rm0", [128, 1], F32)
    rm1 = k.sb("rm1", [128, 1], F32)
    k.memset("dve", rm0[:], 0.0, [rm0]); k.memset("dve", rm0[0:64, :], 1.0, [rm0])
    k.memset("dve", rm1[:], 0.0, [rm1]); k.memset("dve", rm1[64:128, :], 1.0, [rm1])
    c.update(tri=tri, ident=ident, trib=trib, triblk=triblk, tris=tris, dmat=dmat, triblkb=triblkb, ones=ones,
             rm0=rm0, rm1=rm1, idf=idf)
    return c


def ln_fm(k, cst, y, ps1, ps2, gcol, bcol, cols, o32, obf, W, tmp):
    ybf, ysq, mean, rstd = tmp["ybf"], tmp["ysq"], tmp["mean"], tmp["rstd"]
    for ch in range(8):
        k.copy("act", ybf[:, ch, :], y[:, ch, :], [y], [ybf])
        k.tt("pool", ysq[:, ch, :], y[:, ch, :], y[:, ch, :], ALU.mult, [y], [ysq])
    for ch in range(8):
        k.mm(ps1[:, 0:W], cst["ones"][:], ybf[:, ch, :], ch == 0, ch == 7, [ybf, cst["ones"]], [ps1])
    for ch in range(8):
        k.mm(ps2[:, 0:W], cst["ones"][:], ysq[:, ch, :], ch == 0, ch == 7, [ysq, cst["ones"]], [ps2])
    k.ts("dve", mean[:], ps1[:, 0:W], 1.0 / 1024, None, ALU.mult, None, [ps1], [mean])
    msq = tmp["msq"]
    k.tt("pool", msq[:], mean[:], mean[:], ALU.mult, [mean], [msq])
    k.stt("dve", rstd[:], ps2[:, 0:W], 1.0 / 1024, msq[:], ALU.mult, ALU.subtract, [ps2, msq], [rstd])
    k.rsqrt(rstd[:], rstd[:], 1.0, EPS, [rstd], [rstd])
    g0 = COLS[gcol][0]
    b0 = COLS[bcol][0]
    for ch in range(8):
        e = "dve" if ch % 2 == 0 else "pool"
        k.tt(e, o32[:, ch, :], y[:, ch, :], mean[:], ALU.subtract, [y, mean], [o32])
        k.tt(e, o32[:, ch, :], o32[:, ch, :], rstd[:], ALU.mult, [o32, rstd], [o32])
        k.ts("dve", o32[:, ch, :], o32[:, ch, :], cols[:, g0 + ch:g0 + ch + 1], cols[:, b0 + ch:b0 + ch + 1],
             ALU.mult, ALU.add, [o32, cols], [o32])
        if obf is not None:
            k.copy("act", obf[:, ch, :], o32[:, ch, :], [o32], [obf])


def psum_banks(k):
    banks = [k.psum(f"pb{i}", [128, 512], F32) for i in range(8)]
    bbufs = [k.em.buf(f"psbank{i}") for i in range(8)]

    def reg(b, c0, c1, parts=128, name=None):
        return T(banks[b][0:parts, c0:c1], bbufs[b], excl=True)
    return banks, reg


def load_w(k, q, wt, dram, r0, r1, c0, c1, kcn):
    src = dram[r0:r1, c0:c1].rearrange("(kc p) n -> p kc n", p=128)
    k.dma(q, wt[:, 0:kcn, 0:c1 - c0], src, wt)


def mixer_phase(k, I, cst, cols, banks, reg, xT, xpT, s_xln1):
    w_in, wsT_d, alng, alnb, wup_d, w_out0 = I["w_in"], I["wsT"], I["alng"], I["alnb"], I["wup"], I["w_out0"]
    B0, B1, B2, B4 = reg(0, 0, 512), reg(1, 0, 512), reg(2, 0, 512), reg(4, 0, 512)
    B2bf = T(banks[2][:, :].bitcast(BF16), B2.b, excl=True)
    Rk, Rz = reg(3, 0, 256), reg(3, 256, 512)
    Rd, Rgl = reg(5, 0, 256), reg(5, 256, 384, 16)
    RbT = [reg(2 * h, 0, 128, 64) for h in range(4)]; RqT = [reg(2 * h, 128, 256, 64) for h in range(4)]
    RkT = [reg(2 * h, 256, 384, 64) for h in range(4)]; Rsc = [reg(2 * h, 384, 512) for h in range(4)]
    Rkv1 = RbT
    RoT = [reg(2 * h + 1, 0, 128) for h in range(4)]; Rss = [reg(2 * h + 1, 128, 256) for h in range(4)]
    RrT = [reg(2 * h + 1, 256, 384) for h in range(4)]; Rkv0 = [reg(2 * h + 1, 384, 512, 64) for h in range(4)]

    k.phase()
    win = k.sb("win", [128, 8, 2576], BF16)
    k.dma("pool", win[:], w_in.rearrange("(kc p) n -> p kc n", p=128), win)
    wout = k.sb("wout", [128, 8, 1024], BF16); load_w(k, "pool", wout, w_out0, 0, 1024, 0, 1024, 8)
    wsT = k.sb("wsT", [128, 8, 128], BF16)
    k.dma("pool", wsT[:], wsT_d.rearrange("g s t -> s g t"), wsT)
    for g in range(8):
        k.tt("dve", wsT[:, g, :], wsT[:, g, :], cst["trib"][:], ALU.mult, [wsT, cst["trib"]], [wsT])
    glng = k.sb("glng", [128, 512]); k.dma("sp", glng[:], alng, glng)
    glnb = k.sb("glnb", [128, 512]); k.dma("sp", glnb[:], alnb, glnb)
    wup = k.sb("wup", [32, 256], BF16); k.dma("pool", wup[:], wup_d, wup)
    glT = k.sb("glT", [32, 128], BF16); k.memset("dve", glT[:], 1.0, [glT])
    S32 = [[k.sb(f"S32_{h}_{i}", [64, 128]) for i in range(2)] for h in range(4)]
    Sbf = [[k.sb(f"Sbf_{h}_{i}", [64, 128], BF16) for i in range(2)] for h in range(4)]
    for h in range(4):
        k.memset("dve", S32[h][0][:], 0.0, [S32[h][0]])
    xb = [k.sb(f"xb{i}", [128, 8, 512], BF16) for i in range(2)]
    x32 = k.sb("x32", [128, 8, 512])
    mixT = k.sb("mixT", [128, 8, 512], BF16)
    u_sb = k.sb("u_sb", [128, 512], BF16); v_sb = k.sb("v_sb", [128, 512]); vq = k.sb("vq", [128, 512])
    vln = k.sb("vln", [128, 512], BF16); ya = k.sb("ya", [128, 512], BF16); vb_sb = k.sb("vb_sb", [128, 512], BF16)
    st = k.sb("st", [128, 8]); la = k.sb("la", [128, 256]); ekl = k.sb("ekl", [128, 256])
    klf = k.sb("klf", [128, 256]); kl0 = k.sb("kl0", [128, 256], BF16); kl1 = k.sb("kl1", [128, 256], BF16)
    k.memset("dve", kl0[:], 0.0, [kl0]); k.memset("dve", kl1[:], 0.0, [kl1])
    H4 = range(4)
    bT_sb = [k.sb(f"bT_sb{h}", [64, 128]) for h in H4]; nref = [k.sb(f"nref{h}", [64, 2]) for h in H4]
    eA = [k.sb(f"eA{h}", [64, 128]) for h in H4]; eB = [k.sb(f"eB{h}", [64, 128]) for h in H4]
    eI = [k.sb(f"eI{h}", [64, 128]) for h in H4]; qd = [k.sb(f"qd{h}", [64, 128], BF16) for h in H4]
    kd = [k.sb(f"kd{h}", [64, 128], BF16) for h in H4]; qi = [k.sb(f"qi{h}", [64, 128], BF16) for h in H4]
    sc_sb = [k.sb(f"sc_sb{h}", [128, 128], BF16) for h in H4]; osq = [k.sb(f"osq{h}", [128, 128], BF16) for h in H4]
    rs_sb = [k.sb(f"rs_sb{h}", [128, 128]) for h in H4]; on = [k.sb(f"on{h}", [128, 128]) for h in H4]
    sr = [k.sb(f"sr{h}", [128, 128]) for h in H4]
    y = k.sb("y", [128, 8, 512])
    tmp = dict(ybf=k.sb("ybf", [128, 8, 512], BF16), ysq=k.sb("ysq", [128, 8, 512], BF16), mean=k.sb("mean", [128, 512]),
               rstd=k.sb("rstd", [128, 512]), msq=k.sb("msq", [128, 512]))
    bn0 = COLS["b_norm_g"][0]; bs0 = COLS["bsT"][0]; fl0 = COLS["flag"][0]

    def gla_tok(xbt, c0):
        gla_tok_a(xbt, c0); gla_tok_b(); gla_tok_c()

    def gla_tok_a(xbt, c0):
        for kc in range(8):
            k.mm(Rk[:], xbt[:, kc, c0:c0 + 128], win[:, kc, 1280:1536], kc == 0, kc == 7, [xbt, win], [Rk])
        for kc in range(8):
            k.mm(B4[:], xbt[:, kc, c0:c0 + 128], win[:, kc, 1536:2048], kc == 0, kc == 7, [xbt, win], [B4])
        for kc in range(8):
            k.mm(Rgl[:], win[:, kc, 2560:2576], xbt[:, kc, c0:c0 + 128], kc == 0, kc == 7, [xbt, win], [Rgl])

    def gla_tok_b():
        k.copy("dve", glT[0:16, :], Rgl[:], [Rgl], [glT])
        k.copy("act", vb_sb[:], B4[:], [B4], [vb_sb])
        k.mm(Rz[:], glT[:], wup[:], True, True, [glT, wup], [Rz])
        k.act(la[:], Rz[:], AF.Exp, [Rz], [la], scale=-1.0)
        k.act(la[:], la[:], AF.Ln, [la], [la], bias=1.0)
        k.mm(Rd[:], cst["dmat"][:], la[:], True, True, [la, cst["dmat"]], [Rd])
        k.act(ekl[:], Rd[:], AF.Exp, [Rd], [ekl])

    def gla_tok_c():
        k.tt("dve", kl0[0:64, :], Rk[0:64, :], ekl[0:64, :], ALU.mult, [Rk, ekl], [kl0])
        k.tt("dve", kl1[64:128, :], Rk[64:128, :], ekl[64:128, :], ALU.mult, [Rk, ekl], [kl1])

    def gla_head_decay(h):
        k.mm(RbT[h][:], la[:, h * 64:(h + 1) * 64], cst["tris"][:], True, True, [la, cst["tris"]], [RbT[h]])
        k.copy("dve", bT_sb[h][:], RbT[h][:], [RbT[h]], [bT_sb[h]])
        k.act(eI[h][:], bT_sb[h][:], AF.Exp, [bT_sb[h]], [eI[h]])

    def gla_state(h):
        k.mm(Rkv0[h][:], kl0[:, h * 64:(h + 1) * 64], vb_sb[:, h * 128:(h + 1) * 128], True, True, [kl0, vb_sb], [Rkv0[h]])
        k.mm(Rkv1[h][:], kl1[:, h * 64:(h + 1) * 64], vb_sb[:, h * 128:(h + 1) * 128], True, True, [kl1, vb_sb], [Rkv1[h]])
        k.stt("dve", S32[h][1][:], S32[h][0][:], eI[h][:, 63:64], Rkv0[h][:], ALU.mult, ALU.add,
              [S32[h][0], eI[h], Rkv0[h]], [S32[h][1]])
        k.copy("act", Sbf[h][1][:], S32[h][1][:], [S32[h][1]], [Sbf[h][1]])
        k.stt("dve", S32[h][0][:], S32[h][1][:], eI[h][:, 127:128], Rkv1[h][:], ALU.mult, ALU.add,
              [S32[h][1], eI[h], Rkv1[h]], [S32[h][0]])

    for g in range(4 if xpT is not None else 0):
        xbt = xb[g % 2]
        k.dma("pool", xbt[:], xpT[:, g * 512:(g + 1) * 512].rearrange("(kc p) t -> p kc t", p=128), xbt)
        for tt in range(4):
            gla_tok(xbt, tt * 128)
            for h in range(4):
                gla_head_decay(h)
                gla_state(h)
    for h in range(4):
        k.ts("dve", S32[h][0][:], S32[h][0][:], cols[0:64, fl0:fl0 + 1], None, ALU.mult, None, [S32[h][0], cols], [S32[h][0]])
        k.copy("act", Sbf[h][0][:], S32[h][0][:], [S32[h][0]], [Sbf[h][0]])

    for g in range(4):
        xbt = xb[g % 2]
        gs = slice(g * 512, (g + 1) * 512)
        k.dma("pool", xbt[:], xT[:, gs].rearrange("(kc p) t -> p kc t", p=128), xbt)
        k.dma("sp", x32[:], xT[:, gs].rearrange("(kc p) t -> p kc t", p=128), x32)
        for tt in range(4):
            c0 = tt * 128
            cs = slice(c0, c0 + 128)
            for kc in range(8):
                k.mm(B0[:], xbt[:, kc, cs], win[:, kc, 0:512], kc == 0, kc == 7, [xbt, win], [B0])
            for kc in range(8):
                k.mm(B1[:], xbt[:, kc, cs], win[:, kc, 512:1024], kc == 0, kc == 7, [xbt, win], [B1])
            gla_tok_a(xbt, c0)
            k.act(u_sb[:], B0[:], AF.Gelu_apprx_tanh, [B0], [u_sb])
            k.act(v_sb[:], B1[:], AF.Gelu_apprx_tanh, [B1], [v_sb])
            gla_tok_b()
            k.op("dve", lambda e: e.reduce_sum(out=st[:, 0:1], in_=v_sb[:], axis=AX.X), [v_sb], [st])
            k.tt("dve", vq[:], v_sb[:], v_sb[:], ALU.mult, [v_sb], [vq])
            k.op("dve", lambda e: e.reduce_sum(out=st[:, 1:2], in_=vq[:], axis=AX.X), [vq], [st])
            k.ts("dve", st[:, 2:3], st[:, 0:1], 1.0 / 512, None, ALU.mult, None, [st], [st])
            k.tt("dve", st[:, 3:4], st[:, 2:3], st[:, 2:3], ALU.mult, [st], [st])
            k.stt("dve", st[:, 4:5], st[:, 1:2], 1.0 / 512, st[:, 3:4], ALU.mult, ALU.subtract, [st], [st])
            k.rsqrt(st[:, 5:6], st[:, 4:5], 1.0, EPS, [st], [st])
            k.stt("dve", st[:, 6:7], st[:, 2:3], -1.0, st[:, 5:6], ALU.mult, ALU.mult, [st], [st])
            k.ts("dve", vq[:], v_sb[:], st[:, 5:6], st[:, 6:7], ALU.mult, ALU.add, [v_sb, st], [vq])
            k.tt("dve", vq[:], vq[:], glng[:], ALU.mult, [vq, glng], [vq])
            k.tt("dve", vln[:], vq[:], glnb[:], ALU.add, [vq, glnb], [vln])
            gla_tok_c()
            for h in H4:
                gla_head_decay(h)
            for gg in range(8):
                fs = slice(gg * 64, (gg + 1) * 64)
                k.mm(B2[:, fs], wsT[:, gg, :], vln[:, fs], True, True, [wsT, vln], [B2])
            for gg in range(8):
                fs = slice(gg * 64, (gg + 1) * 64)
                k.stt("dve", ya[:, fs], B2[:, fs], cols[:, bs0 + gg:bs0 + gg + 1], u_sb[:, fs], ALU.add, ALU.mult,
                      [B2, cols, u_sb], [ya])
            for i in range(4):
                k.op("pe", lambda e: e.transpose(B2bf[:, i * 128:(i + 1) * 128], ya[:, i * 128:(i + 1) * 128], cst["ident"][:]),
                     [ya, cst["ident"]], [B2bf])
            k.copy("act", mixT[:, 0:4, cs], B2bf[:, 0:512].rearrange("p (a b) -> p a b", a=4), [B2bf], [mixT])
            for h in H4:
                k.ts("dve", nref[h][:, 0:1], bT_sb[h][:, 31:32], -1.0, None, ALU.mult, None, [bT_sb[h]], [nref[h]])
                k.ts("dve", nref[h][:, 1:2], bT_sb[h][:, 95:96], -1.0, None, ALU.mult, None, [bT_sb[h]], [nref[h]])
            for h in H4:
                for c in range(2):
                    hs = slice(c * 64, (c + 1) * 64)
                    k.act(eA[h][:, hs], bT_sb[h][:, hs], AF.Exp, [bT_sb[h], nref[h]], [eA[h]], bias=nref[h][:, c:c + 1], scale=1.0)
                    k.act(eB[h][:, hs], bT_sb[h][:, hs], AF.Exp, [bT_sb[h]], [eB[h]],
                          bias=bT_sb[h][:, 31 + 64 * c:32 + 64 * c], scale=-1.0)
            for h in H4:
                for kc in range(8):
                    k.mm(RqT[h][:], win[:, kc, 1024 + h * 64:1088 + h * 64], xbt[:, kc, cs], kc == 0, kc == 7, [xbt, win], [RqT[h]])
                for kc in range(8):
                    k.mm(RkT[h][:], win[:, kc, 1280 + h * 64:1344 + h * 64], xbt[:, kc, cs], kc == 0, kc == 7, [xbt, win], [RkT[h]])
            for h in H4:
                k.stt("dve", qd[h][:], RqT[h][:], 0.125, eA[h][:], ALU.mult, ALU.mult, [RqT[h], eA[h]], [qd[h]])
                k.tt("dve", kd[h][:], RkT[h][:], eB[h][:], ALU.mult, [RkT[h], eB[h]], [kd[h]])
                k.stt("dve", qi[h][:], RqT[h][:], 0.125, eI[h][:], ALU.mult, ALU.mult, [RqT[h], eI[h]], [qi[h]])
            for h in H4:
                k.mm(Rsc[h][:], kd[h][:], qd[h][:], True, True, [kd[h], qd[h]], [Rsc[h]])
            for h in H4:
                k.tt("dve", sc_sb[h][:], Rsc[h][:], cst["triblk"][:], ALU.mult, [Rsc[h], cst["triblk"]], [sc_sb[h]])
            for h in H4:
                gla_state(h)
            for h in H4:
                k.mm(RoT[h][:], vb_sb[:, h * 128:(h + 1) * 128], sc_sb[h][:], True, False, [vb_sb, sc_sb[h]], [RoT[h]])
                k.mm(RoT[h][:, 0:64], Sbf[h][0][:], qi[h][:, 0:64], False, False, [Sbf[h][0], qi[h]], [RoT[h]])
                k.mm(RoT[h][:, 64:128], Sbf[h][1][:], qi[h][:, 64:128], False, True, [Sbf[h][1], qi[h]], [RoT[h]])
            for h in H4:
                k.copy("act", Sbf[h][0][:], S32[h][0][:], [S32[h][0]], [Sbf[h][0]])
                k.act(osq[h][:], RoT[h][:], AF.Square, [RoT[h]], [osq[h]])
            for h in H4:
                k.mm(Rss[h][:], cst["ones"][:], osq[h][:], True, True, [osq[h], cst["ones"]], [Rss[h]])
            for h in H4:
                k.op("act", lambda e: e.activation(out=rs_sb[h][:], in_=Rss[h][:], func=AF.Sqrt, bias=EPS, scale=1.0 / 128),
                     [Rss[h]], [rs_sb[h]])
            for h in H4:
                k.op("dve", lambda e: e.reciprocal(out=rs_sb[h][:], in_=rs_sb[h][:]), [rs_sb[h]], [rs_sb[h]])
                k.tt("dve", on[h][:], RoT[h][:], rs_sb[h][:], ALU.mult, [RoT[h], rs_sb[h]], [on[h]])
            for h in H4:
                for kc in range(8):
                    k.mm(RrT[h][:], win[:, kc, 2048 + h * 128:2176 + h * 128], xbt[:, kc, cs], kc == 0, kc == 7, [xbt, win], [RrT[h]])
            for h in H4:
                k.act(sr[h][:], RrT[h][:], AF.Silu, [RrT[h]], [sr[h]])
            for h in H4:
                k.stt("dve", mixT[:, 4 + h, cs], on[h][:], cols[:, bn0:bn0 + 1], sr[h][:], ALU.mult, ALU.mult,
                      [on[h], cols, sr[h]], [mixT])
        for nb in range(8):
            pb = B0 if nb % 2 == 0 else B1
            for ch in range(8):
                k.mm(pb[:], wout[:, ch, nb * 128:(nb + 1) * 128], mixT[:, ch, :], ch == 0, ch == 7, [wout, mixT], [pb])
            k.stt("dve", y[:, nb, :], x32[:, nb, :], ALPHA, pb[:], ALU.mult, ALU.add, [x32, pb], [y])
        ln_fm(k, cst, y, B0, B1, "e_ln1_g", "e_ln1_b", cols, y, None, 512, tmp)
        k.dma("sp", s_xln1[:, gs].rearrange("(kc p) t -> p kc t", p=128), y[:], s_xln1, [y])
    k.end_phase()


def ffn_phase(k, cst, cols, B0, B1, B2, B3, src, dst, wg_d, wu_d, wd_d, NJ, lng, lnb, wp_d, wgate_d, pT_d, pbias):
    k.phase()
    TG = 1024
    xb = k.sb("f_xb", [128, 8, TG], BF16); x32 = k.sb("f_x32", [128, 8, TG])
    hT = k.sb("f_hT", [128, NJ, TG], BF16)
    wgt = [k.sb(f"f_wg{i}", [128, 8, 256], BF16) for i in range(4)]
    wut = [k.sb(f"f_wu{i}", [128, 8, 256], BF16) for i in range(4)]
    wdt = [k.sb(f"f_wd{i}", [128, NJ, 256], BF16) for i in range(2)]
    sg = [k.sb(f"f_sg{i}", [128, 512], BF16) for i in range(2)]
    wgate = k.sb("f_wgate", [128, 8, 1024], BF16); wproj = k.sb("f_wproj", [128, 2, 1024], BF16)
    load_w(k, "pool", wgate, wgate_d, 0, 1024, 0, 1024, 8)
    load_w(k, "pool", wproj, wp_d, 0, 256, 0, 1024, 2)
    pTb = k.sb("f_pTb", [128, 2, 512], BF16); x2bf = k.sb("f_x2bf", [128, 8, 512], BF16)
    gt = k.sb("f_gt", [128, 512]);
    tmp = dict(ybf=k.sb("ybf", [128, 8, 512], BF16), ysq=k.sb("ysq", [128, 8, 512], BF16), mean=k.sb("mean", [128, 512]),
               rstd=k.sb("rstd", [128, 512]), msq=k.sb("msq", [128, 512]))
    pb0 = COLS[pbias][0]
    NJB = (NJ + 1) // 2
    for G in range(NT // TG):
        gs = slice(G * TG, (G + 1) * TG)
        k.dma("pool", xb[:], src[:, gs].rearrange("(kc p) t -> p kc t", p=128), xb, [src])
        k.dma("sp", x32[:], src[:, gs].rearrange("(kc p) t -> p kc t", p=128), x32, [src])
        for jb in range(NJB):
            nj = min(2, NJ - jb * 2)
            a, b = wgt[jb % 4], wut[jb % 4]
            load_w(k, "pool", a, wg_d, 0, 1024, jb * 256, jb * 256 + nj * 128, 8)
            load_w(k, "pool", b, wu_d, 0, 1024, jb * 256, jb * 256 + nj * 128, 8)
            for jj in range(nj):
                j = jb * 2 + jj
                for tg in range(TG // 512):
                    ts_ = slice(tg * 512, (tg + 1) * 512)
                    pg, pu = (B0, B1) if (j * 2 + tg) % 2 == 0 else (B2, B3)
                    s_ = sg[(j * 2 + tg) % 2]
                    for kc in range(8):
                        k.mm(pg[:], a[:, kc, jj * 128:(jj + 1) * 128], xb[:, kc, ts_], kc == 0, kc == 7, [a, xb], [pg])
                    for kc in range(8):
                        k.mm(pu[:], b[:, kc, jj * 128:(jj + 1) * 128], xb[:, kc, ts_], kc == 0, kc == 7, [b, xb], [pu])
                    k.act(s_[:], pg[:], AF.Silu, [pg], [s_])
                    k.tt("dve", hT[:, j, ts_], pu[:], s_[:], ALU.mult, [pu, s_], [hT])
        for nbb in range(4):
            wd_ = wdt[nbb % 2]
            k.dma("pool", wd_[:], wd_d[:, nbb * 256:(nbb + 1) * 256].rearrange("(j p) n -> p j n", p=128), wd_)
            for nn in range(2):
                nb = nbb * 2 + nn
                for tg in range(TG // 512):
                    ts_ = slice(tg * 512, (tg + 1) * 512)
                    pb = B0 if (nb * 2 + tg) % 2 == 0 else B1
                    for j in range(NJ):
                        k.mm(pb[:], wd_[:, j, nn * 128:(nn + 1) * 128], hT[:, j, ts_], j == 0, j == NJ - 1, [wd_, hT], [pb])
                    k.stt("dve", x32[:, nb, ts_], x32[:, nb, ts_], ALPHA, pb[:], ALU.mult, ALU.add, [x32, pb], [x32])
        for tg in range(TG // 512):
            ts_ = slice(tg * 512, (tg + 1) * 512)
            yv = T(x32.h[:, :, ts_], x32.b)
            ln_fm(k, cst, yv, B0, B1, lng, lnb, cols, yv, x2bf, 512, tmp)
            t0 = G * TG + tg * 512
            k.dma("pool", pTb[:], pT_d[:, t0:t0 + 512].rearrange("(kc p) t -> p kc t", p=128), pTb)
            for nb in range(8):
                ns = slice(nb * 128, (nb + 1) * 128)
                pg, pp = (B0, B1) if nb % 2 == 0 else (B2, B3)
                for kc in range(8):
                    k.mm(pg[:], wgate[:, kc, ns], x2bf[:, kc, :], kc == 0, kc == 7, [wgate, x2bf], [pg])
                for kc in range(2):
                    k.mm(pp[:], wproj[:, kc, ns], pTb[:, kc, :], kc == 0, kc == 1, [wproj, pTb], [pp])
                k.act(gt[:], pg[:], AF.Sigmoid, [pg, cols], [gt], bias=cols[:, pb0 + nb:pb0 + nb + 1], scale=1.0)
                k.tt("dve", gt[:], gt[:], pp[:], ALU.mult, [gt, pp], [gt])
                k.tt("pool", yv[:, nb, :], yv[:, nb, :], gt[:], ALU.add, [yv, gt], [yv])
        k.dma("sp", dst[:, gs].rearrange("(kc p) t -> p kc t", p=128), x32[:], dst, [x32])
    k.end_phase()


def qkv_phase(k, B, src, w_qkv, qT_o, kT_o, v_o, koff):
    k.phase()
    wq = k.sb("wqkv", [128, 8, 3072], BF16)
    k.dma("pool", wq[:], w_qkv.rearrange("(kc p) n -> p kc n", p=128), wq)
    xb = [k.sb(f"q_xb{i}", [128, 8, 512], BF16) for i in range(2)]
    qs = k.sb("q_qs", [64, 16, 512], BF16); ks = k.sb("q_ks", [64, 16, 512], BF16); vs = k.sb("q_vs", [128, 4, 1024], BF16)
    n = 0
    for g in range(4):
        gs = slice(g * 512, (g + 1) * 512)
        xbt = xb[g % 2]
        k.dma("pool", xbt[:], src[:, gs].rearrange("(kc p) t -> p kc t", p=128), xbt, [src])
        for which, dstt in ((0, qs), (1, ks)):
            if which == 0 and qT_o is None:
                continue
            for hc in range(16):
                pb = B[n % 8]; n += 1
                c0 = which * 1024 + hc * 64
                for kc in range(8):
                    k.mm(pb[0:64, :], wq[:, kc, c0:c0 + 64], xbt[:, kc, :], kc == 0, kc == 7, [wq, xbt], [pb])
                k.copy("act" if n % 2 else "dve", dstt[:, hc, :], pb[0:64, :], [pb], [dstt])
        for tt in range(4):
            for hf in range(2):
                pb = B[n % 8]; n += 1
                for kc in range(8):
                    k.mm(pb[:], xbt[:, kc, tt * 128:(tt + 1) * 128], wq[:, kc, 2048 + hf * 512:2560 + hf * 512], kc == 0, kc == 7,
                         [wq, xbt], [pb])
                k.copy("act" if n % 2 else "dve", vs[:, tt, hf * 512:(hf + 1) * 512], pb[:], [pb], [vs])
        ko = slice(koff + g * 512, koff + (g + 1) * 512)
        if qT_o is not None:
            k.dma("sp", qT_o[:, :, gs].rearrange("a d t -> d a t"), qs[:], qT_o, [qs])
        k.dma("sp", kT_o[:, :, ko].rearrange("a d t -> d a t"), ks[:], kT_o, [ks])
        k.dma("sp", v_o[ko, :].rearrange("(tt p) f -> p tt f", p=128), vs[:], v_o, [vs])
    k.end_phase()


def _colslab(v):
    v = np.asarray(v, np.float32).reshape(-1, 128)
    return np.ascontiguousarray(v.T)


def make_cols(inp, h):
    cols = np.zeros((128, NCOLS), np.float32)

    def put(name, arr):
        c0, n = COLS[name]
        cols[:, c0:c0 + n] = arr
    put("e_ln1_g", _colslab(inp["e_ln1_g"][0])); put("e_ln1_b", _colslab(inp["e_ln1_b"][0]))
    put("e_ln2_g", _colslab(inp["e_ln2_g"][0])); put("e_ln2_b", _colslab(inp["e_ln2_b"][0]))
    put("o_ln1_g", _colslab(inp["o_ln1_g"][0])); put("o_ln1_b", _colslab(inp["o_ln1_b"][0]))
    put("o_ln2_g", _colslab(inp["o_ln2_g"][0])); put("o_ln2_b", _colslab(inp["o_ln2_b"][0]))
    put("ple_b0", _colslab(inp["ple_b_gate"][0])); put("ple_b1", _colslab(inp["ple_b_gate"][1]))
    put("b_norm_g", _colslab(inp["e_b_norm_g"][0]))
    put("bsT", np.ascontiguousarray(np.asarray(inp["e_a_bs"][0], np.float32).T))
    put("gsubc", _colslab(inp["o_subln_g"][0]))
    put("flag", np.full((128, 1), float(h), np.float32))
    put("nbias", np.full((128, 1), 0.0 if h == 1 else -30000.0, np.float32))
    return cols


def l1_inputs(inp, c):
    b, h = c // 2, c % 2
    x = np.asarray(inp["x"], np.float32)
    p = np.asarray(inp["p"], np.float32)
    sl = slice(h * NT, (h + 1) * NT)
    wup = np.zeros((32, 256), np.float32)
    wup[0:16] = inp["e_b_w_gate_up"][0]
    wup[16] = inp["e_b_b_gate"][0]
    return {
        "xT": np.ascontiguousarray(x[b, sl].T), "xpT": np.ascontiguousarray(x[b, 0:NT].T),
        "p0T": np.ascontiguousarray(p[0, b, sl].T), "cols": make_cols(inp, h),
        "w_in": np.asarray(inp["e_w_in"][0], np.float32),
        "wsT": np.ascontiguousarray(np.asarray(inp["e_a_ws"][0], np.float32).transpose(0, 2, 1)),
        "alng": np.ascontiguousarray(np.broadcast_to(np.asarray(inp["e_a_ln_g"][0], np.float32), (128, 512))),
        "alnb": np.ascontiguousarray(np.broadcast_to(np.asarray(inp["e_a_ln_b"][0], np.float32), (128, 512))),
        "wup": wup, "w_out0": np.asarray(inp["e_w_out"][0], np.float32),
        "ffn_wg": np.asarray(inp["e_ffn_wg"][0], np.float32), "ffn_wu": np.asarray(inp["e_ffn_wu"][0], np.float32),
        "ffn_wd": np.asarray(inp["e_ffn_wd"][0], np.float32),
        "wp0": np.asarray(inp["ple_w_proj"][0], np.float32), "wg0": np.asarray(inp["ple_w_gate"][0], np.float32),
        "w_qkv": np.asarray(inp["o_w_qkv"][0], np.float32),
    }


LAMBDA_INIT = 0.8 - 0.6 * float(np.exp(-0.3 * 1))
CAP = 256


def l2_phases(k, I, cst, cols, banks, reg, s_x1, s_q, s_kT, s_v, outT):
    lam_d, gsub_d, wo_d, wr_d = I["lamv"], I["gsub"], I["w_out1"], I["w_router"]
    ewg, ewu, ewd, p1T, wp1, wg1 = I["exp_wg"], I["exp_wu"], I["exp_wd"], I["p1T"], I["wp1"], I["wg1"]
    s_oT = k.dscr("s_oT", [1024, NT], BF16); s_xl = k.dscr("s_xl", [1024, NT])
    s_xg = k.dscr("s_xg", [8, 1024, 4 * CAP], BF16); s_yg = k.dscr("s_yg", [8, 4 * CAP, 1024], BF16)
    s_PT = k.dscr("s_PT", [4, 128, 8 * 2 * 512], BF16)
    B = [reg(i, 0, 512) for i in range(8)]
    gsl = k.sb("gsl", [128, 8, 8])
    nb0 = COLS["nbias"][0]

    k.phase()
    lamv = k.sb("lamv", [128, 4, 64]); k.dma("sp", lamv[:], lam_d, lamv)
    lt = k.sb("lt", [128, 8]); lp = k.sb("lp", [128, 64])
    for i in range(2):
        k.tt("dve", lp[:], lamv[:, 2 * i, :], lamv[:, 2 * i + 1, :], ALU.mult, [lamv], [lp])
        k.op("dve", lambda e: e.reduce_sum(out=lt[:, i:i + 1], in_=lp[:], axis=AX.X), [lp], [lt])
    k.act(lt[:, 2:4], lt[:, 0:2], AF.Exp, [lt], [lt])
    k.tt("dve", lt[:, 4:5], lt[:, 3:4], lt[:, 2:3], ALU.subtract, [lt], [lt])
    k.ts("dve", lt[:, 5:6], lt[:, 4:5], -LAMBDA_INIT, None, ALU.add, None, [lt], [lt])
    Kt = [k.sb(f"Kt{i}", [128, 2 * NT], BF16) for i in range(2)]
    Vt = [k.sb(f"Vt{i}", [128, 32, 128], BF16) for i in range(2)]
    Qz = [[k.sb(f"Qz{i}_{c}", [128, NT], BF16) for c in range(2)] for i in range(2)]
    for i in range(2):
        for c in range(2):
            k.memset("dve", Qz[i][c][:], 0.0, [Qz[i][c]])
    Et = [k.sb(f"Et{i}", [128, 512], BF16) for i in range(6)]
    rs = [k.sb(f"rs{c}", [128, 512]) for c in range(2)]; Oc = [k.sb(f"Oc{c}", [128, 512]) for c in range(2)]
    t1 = k.sb("t1", [128, 512]); od = k.sb("od", [128, 512])
    osq = k.sb("a_osq", [128, 512], BF16); rstd = k.sb("a_rstd", [128, 512]); onb = k.sb("onb", [128, 512], BF16)
    gc0 = COLS["gsubc"][0]
    k.ts("dve", lt[:, 6:7], cols[:, gc0:gc0 + 1], 1.0 - LAMBDA_INIT, None, ALU.mult, None, [cols], [lt])
    Oa = [B[2], B[3]]; Sm = [B[4], B[5]]
    Bs = B[7]; Sbanks = [B[0], B[1], B[6]]
    for h in range(8):
        Kh, Vh, Qh = Kt[h % 2], Vt[h % 2], Qz[h % 2]
        k.dma("sp", Kh[:], s_kT[2 * h:2 * h + 2].rearrange("c d t -> (c d) t"), Kh, [s_kT])
        k.dma("sp", Vh[:], s_v[:, h * 128:(h + 1) * 128].rearrange("(tt p) f -> p tt f", p=128), Vh, [s_v])
        k.dma("sp", Qh[0][0:64, :], s_q[2 * h], Qh[0], [s_q])
        k.dma("sp", Qh[1][64:128, :], s_q[2 * h + 1], Qh[1], [s_q])
        blocks = [(g, kb, c) for g in range(4) for kb in range(16 + 4 * (g + 1)) for c in range(2)]

        def emit_S(i):
            g, kb, c = blocks[i]
            m = kb - (16 + 4 * g)
            n0 = 128 * m if m > 0 else 0
            Sb = Sbanks[i % 3]
            k.mm(Sb[:, n0:512], Kh[:, kb * 128:(kb + 1) * 128], Qh[c][:, g * 512 + n0:(g + 1) * 512], True, True,
                 [Kh, Qh[c]], [Sb])

        for j in range(2):
            emit_S(j)
        for i, (g, kb, c) in enumerate(blocks):
            if i + 2 < len(blocks):
                emit_S(i + 2)
            m = kb - (16 + 4 * g)
            n0 = 128 * m if m > 0 else 0
            last = 16 + 4 * g + 3
            Sb = Sbanks[i % 3]; E = Et[i % 6]
            if kb < 16:
                k.act(E[:, n0:512], Sb[:, n0:512], AF.Exp, [Sb, cols], [E], bias=cols[:, nb0:nb0 + 1], scale=0.125)
            else:
                k.act(E[:, n0:512], Sb[:, n0:512], AF.Exp, [Sb], [E], scale=0.125)
            if m >= 0:
                k.tt("dve", E[:, n0:n0 + 128], E[:, n0:n0 + 128], cst["trib"][:], ALU.mult, [E, cst["trib"]], [E])
            k.mm(Oa[c][:, n0:512], Vh[:, kb, :], E[:, n0:512], kb == 0, kb == last, [Vh, E], [Oa[c]])
            k.mm(Sm[c][:, n0:512], cst["ones"][:], E[:, n0:512], kb == 0, kb == last, [cst["ones"], E], [Sm[c]])
            if kb == last and c == 1:
                for cc in range(2):
                    k.op("dve", lambda e: e.reciprocal(out=rs[cc][:], in_=Sm[cc][:]), [Sm[cc]], [rs[cc]])
                    k.copy("act", Oc[cc][:], Oa[cc][:], [Oa[cc]], [Oc[cc]])
                k.tt("pool", t1[:], Oc[0][:], rs[0][:], ALU.mult, [Oc[0], rs[0]], [t1])
                k.tt("dve", od[:], Oc[1][:], rs[1][:], ALU.mult, [Oc[1], rs[1]], [od])
                k.stt("dve", od[:], od[:], lt[:, 5:6], t1[:], ALU.mult, ALU.add, [od, lt, t1], [od])
                k.tt("pool", osq[:], od[:], od[:], ALU.mult, [od], [osq])
                k.mm(Bs[:], cst["ones"][:], osq[:], True, True, [cst["ones"], osq], [Bs])
                k.rsqrt(rstd[:], Bs[:], 1.0 / 128, EPS, [Bs], [rstd])
                k.stt("dve", onb[:], od[:], lt[:, 6:7], rstd[:], ALU.mult, ALU.mult, [od, lt, rstd], [onb])
                k.dma("sp", s_oT[h * 128:(h + 1) * 128, g * 512:(g + 1) * 512], onb[:], s_oT, [onb])
    k.end_phase()

    k.phase()
    wo = k.sb("wo", [128, 8, 1024], BF16); load_w(k, "pool", wo, wo_d, 0, 1024, 0, 1024, 8)
    wr = k.sb("wr", [128, 8, 8]); k.dma("sp", wr[:], wr_d.rearrange("(kc p) e -> p kc e", p=128), wr)
    oTs = k.sb("oTs", [128, 8, 512], BF16); x32 = k.sb("r_x32", [128, 8, 512]); xbf = k.sb("r_xbf", [128, 8, 512], BF16)
    tmp = dict(ybf=k.sb("ybf", [128, 8, 512], BF16), ysq=k.sb("ysq", [128, 8, 512], BF16), mean=k.sb("mean", [128, 512]),
               rstd=k.sb("rstd", [128, 512]), msq=k.sb("msq", [128, 512]))
    xtok = k.sb("xtok", [128, 4, 1024], BF16)
    lg = k.sb("lg", [128, 4, 8]); m8 = k.sb("m8", [128, 4, 8]); sel = k.sb("sel", [128, 4, 8]); is1 = k.sb("is1", [128, 4, 8])
    selb = k.sb("selb", [128, 32], BF16); gw = k.sb("gw", [128, 4, 8]); gq = k.sb("gq", [128, 8])
    off = k.sb("off", [128, 4, 8]); d1 = k.sb("d1", [128, 4, 8]); ghl = k.sb("ghl", [128, 4, 8, 2], BF16)
    ghi32 = k.sb("ghi32", [128, 4, 8]); g2t = k.sb("g2t", [128, 2])
    io_i = k.sb("io_i", [128, CAP], I32); io1 = k.sb("io1", [128, CAP])
    k.op("pool", lambda e: e.iota(io_i[:], pattern=[[1, CAP]], base=1, channel_multiplier=0), (), [io_i])
    k.copy("dve", io1[:], io_i[:], [io_i], [io1])
    tris_bf = k.sb("tris_bf", [128, 128], BF16)
    k.tt("dve", tris_bf[:], cst["tri"][:], cst["idf"][:], ALU.subtract, [cst["tri"], cst["idf"]], [tris_bf])
    P = k.sb("P", [128, 4, 8, CAP], BF16)
    xg = k.sb("xg", [128, 8, CAP], BF16); PTs = k.sb("PTs", [128, 8, 2, 512], BF16)
    B7bf = T(banks[7][:, :].bitcast(BF16), B[7].b, excl=True)
    B6bf = T(banks[6][:, :].bitcast(BF16), B[6].b, excl=True)
    for g in range(4):
        gs = slice(g * 512, (g + 1) * 512)
        k.dma("sp", oTs[:], s_oT[:, gs].rearrange("(kc p) t -> p kc t", p=128), oTs, [s_oT])
        k.dma("sp", x32[:], s_x1[:, gs].rearrange("(kc p) t -> p kc t", p=128), x32, [s_x1])
        for nb in range(8):
            pb = B[nb % 2]
            for ch in range(8):
                k.mm(pb[:], wo[:, ch, nb * 128:(nb + 1) * 128], oTs[:, ch, :], ch == 0, ch == 7, [wo, oTs], [pb])
            k.stt("dve", x32[:, nb, :], x32[:, nb, :], ALPHA, pb[:], ALU.mult, ALU.add, [x32, pb], [x32])
        ln_fm(k, cst, x32, B[0], B[1], "o_ln1_g", "o_ln1_b", cols, x32, xbf, 512, tmp)
        k.dma("sp", s_xl[:, gs].rearrange("(kc p) t -> p kc t", p=128), x32[:], s_xl, [x32])
        for tt in range(4):
            cs = slice(tt * 128, (tt + 1) * 128)
            for half in range(2):
                pbf = B6bf if half == 0 else B7bf
                for i in range(4):
                    ch = half * 4 + i
                    k.op("pe", lambda e: e.transpose(pbf[:, i * 128:(i + 1) * 128], xbf[:, ch, cs], cst["ident"][:]),
                         [xbf, cst["ident"]], [pbf])
                k.copy("act" if half else "dve", xtok[:, tt, half * 512:(half + 1) * 512], pbf[:, 0:512], [pbf], [xtok])
            for ch in range(8):
                k.mm(B[2][:, tt * 8:(tt + 1) * 8], x32[:, ch, cs], wr[:, ch, :], ch == 0, ch == 7, [x32, wr], [B[2]])
        k.copy("dve", lg[:].rearrange("p a b -> p (a b)"), B[2][:, 0:32], [B[2]], [lg])
        for tt in range(4):
            k.op("dve", lambda e: e.max(out=m8[:, tt, :], in_=lg[:, tt, :]), [lg], [m8])
            k.ts("dve", sel[:, tt, :], lg[:, tt, :], m8[:, tt, 1:2], None, ALU.is_ge, None, [lg, m8], [sel])
            k.ts("dve", is1[:, tt, :], lg[:, tt, :], m8[:, tt, 0:1], None, ALU.is_ge, None, [lg, m8], [is1])
        for tt in range(4):
            k.tt("dve", gq[:, tt:tt + 1], m8[:, tt, 1:2], m8[:, tt, 0:1], ALU.subtract, [m8], [gq])
        k.act(gq[:, 0:4], gq[:, 0:4], AF.Exp, [gq], [gq])
        k.ts("dve", gq[:, 4:8], gq[:, 0:4], 1.0, None, ALU.add, None, [gq], [gq])
        k.op("dve", lambda e: e.reciprocal(out=gq[:, 4:8], in_=gq[:, 4:8]), [gq], [gq])
        k.tt("dve", gq[:, 0:4], gq[:, 0:4], gq[:, 4:8], ALU.mult, [gq], [gq])
        for tt in range(4):
            k.ts("dve", gw[:, tt, :], sel[:, tt, :], gq[:, tt:tt + 1], None, ALU.mult, None, [sel, gq], [gw])
            k.tt("dve", off[:, 0, 0:1], gq[:, 4 + tt:5 + tt], gq[:, tt:tt + 1], ALU.subtract, [gq], [off])
            k.stt("dve", gw[:, tt, :], is1[:, tt, :], off[:, 0, 0:1], gw[:, tt, :], ALU.mult, ALU.add, [is1, off, gw], [gw])
        k.copy("dve", ghl[:, :, :, 0], gw[:], [gw], [ghl])
        k.copy("dve", ghi32[:], ghl[:, :, :, 0], [ghl], [ghi32])
        k.tt("dve", ghi32[:], gw[:], ghi32[:], ALU.subtract, [gw, ghi32], [ghi32])
        k.copy("dve", ghl[:, :, :, 1], ghi32[:], [ghi32], [ghl])
        k.copy("dve", selb[:], sel[:].rearrange("p a b -> p (a b)"), [sel], [selb])
        k.mm(B[2][:, 0:32], tris_bf[:], selb[:], True, True, [tris_bf, selb], [B[2]])
        k.mm(B[3][:, 0:32], cst["ones"][:], selb[:], True, True, [cst["ones"], selb], [B[3]])
        k.memset("dve", off[:, 0, :], 0.0, [off])
        for tt in range(1, 4):
            k.tt("dve", off[:, tt, :], off[:, tt - 1, :], B[3][:, (tt - 1) * 8:tt * 8], ALU.add, [off, B[3]], [off])
        k.tt("dve", d1[:].rearrange("p a b -> p (a b)"), off[:].rearrange("p a b -> p (a b)"), B[2][:, 0:32], ALU.add,
             [off, B[2]], [d1])
        k.stt("dve", d1[:], d1[:], 1.0, sel[:], ALU.add, ALU.mult, [d1, sel], [d1])
        for tt in range(4):
            for e in range(8):
                k.ts("dve", P[:, tt, e, :], io1[:], d1[:, tt, e:e + 1], None, ALU.is_equal, None, [io1, d1], [P])
        for e in range(8):
            for ch in range(8):
                pb = B[ch % 2]
                for tt in range(4):
                    k.mm(pb[:, 0:CAP], xtok[:, tt, ch * 128:(ch + 1) * 128], P[:, tt, e, :], tt == 0, tt == 3, [xtok, P], [pb])
                k.copy("act" if ch % 2 else "dve", xg[:, ch, :], pb[:, 0:CAP], [pb], [xg])
            k.dma("sp", s_xg[e, :, g * CAP:(g + 1) * CAP].rearrange("(kc p) s -> p kc s", p=128), xg[:], s_xg, [xg])
            for st in range(2):
                pbf = B6bf if st == 0 else B7bf
                for tt in range(4):
                    k.op("pe", lambda e_: e_.transpose(pbf[:, tt * 128:(tt + 1) * 128], P[:, tt, e, st * 128:(st + 1) * 128],
                                                       cst["ident"][:]), [P, cst["ident"]], [pbf])
                k.copy("act" if st else "dve", PTs[:, e, st, :], pbf[:, 0:512], [pbf], [PTs])
                for tt in range(4):
                    k.mm(B[4][:, 0:2], P[:, tt, e, st * 128:(st + 1) * 128], ghl[:, tt, e, :], tt == 0, tt == 3, [P, ghl], [B[4]])
                k.copy("dve", g2t[:], B[4][:, 0:2], [B[4]], [g2t])
                k.tt("dve", gsl[:, e, g * 2 + st:g * 2 + st + 1], g2t[:, 0:1], g2t[:, 1:2], ALU.add, [g2t], [gsl])
        k.dma("sp", s_PT[g].rearrange("p (a b c) -> p a b c", a=8, b=2), PTs[:], s_PT, [PTs])
    k.end_phase()
    expert_phase(k, B, s_xg, s_yg, ewg, ewu, ewd, gsl)
    combine_phase(k, cst, cols, B, s_yg, s_PT, s_xl, outT, wp1, wg1, p1T)
    k.em.wait_all("sp", _bufs([outT]))


def build_fused(k):
    names = [("xT", [1024, NT]), ("xpT", [1024, NT]), ("p0T", [256, NT]), ("p0pT", [256, NT]), ("p1T", [256, NT]),
             ("cols", [128, NCOLS]), ("w_in", [1024, 2576]), ("wsT", [8, 128, 128]), ("alng", [128, 512]),
             ("alnb", [128, 512]), ("wup", [32, 256]), ("w_out0", [1024, 1024]), ("ffn_wg", [1024, 2816]),
             ("ffn_wu", [1024, 2816]), ("ffn_wd", [2816, 1024]), ("wp0", [256, 1024]), ("wg0", [1024, 1024]),
             ("w_qkv", [1024, 3072]), ("lamv", [128, 4, 64]), ("gsub", [128, 128]), ("w_out1", [1024, 1024]),
             ("w_router", [1024, 8]), ("exp_wg", [8, 1024, 3584]), ("exp_wu", [8, 1024, 3584]),
             ("exp_wd", [8, 3584, 1024]), ("wp1", [256, 1024]), ("wg1", [1024, 1024])]
    I = {n: k.din(n, shp) for n, shp in names}
    outT = k.dout("outT", [1024, NT])
    s_xln1 = k.dscr("s_xln1", [1024, NT]); s_x1p = k.dscr("s_x1p", [1024, NT]); s_x1 = k.dscr("s_x1", [1024, NT])
    s_q = k.dscr("s_q", [16, 64, NT], BF16); s_kT = k.dscr("s_kT", [16, 64, 2 * NT], BF16)
    s_v = k.dscr("s_v", [2 * NT, 1024], BF16)
    cst = make_consts(k)
    banks, reg = psum_banks(k)
    cols = k.sb("cols", [128, NCOLS]); k.dma("sp", cols[:], I["cols"], cols)
    B = [reg(i, 0, 512) for i in range(8)]
    for (src, psrc, pref, x1dst, qdst, koff) in ((I["xpT"], I["p0pT"], None, s_x1p, None, 0),
                                                 (I["xT"], I["p0T"], I["xpT"], s_x1, s_q, NT)):
        mixer_phase(k, I, cst, cols, banks, reg, src, pref, s_xln1)
        ffn_phase(k, cst, cols, B[0], B[1], B[2], B[3], s_xln1, x1dst, I["ffn_wg"], I["ffn_wu"], I["ffn_wd"], 22,
                  "e_ln2_g", "e_ln2_b", I["wp0"], I["wg0"], psrc, "ple_b0")
        qkv_phase(k, B, x1dst, I["w_qkv"], qdst, s_kT, s_v, koff)
    l2_phases(k, I, cst, cols, banks, reg, s_x1, s_q, s_kT, s_v, outT)


def expert_phase(k, B, s_xg, s_yg, ewg, ewu, ewd, gsl):
    k.phase()
    NS = 4 * CAP
    NJ = 28
    xg = [k.sb(f"e_xg{i}", [128, 8, NS], BF16) for i in range(2)]
    hT = k.sb("e_hT", [128, NJ, NS], BF16)
    wgt = [k.sb(f"e_wg{i}", [128, 8, 256], BF16) for i in range(4)]
    wut = [k.sb(f"e_wu{i}", [128, 8, 256], BF16) for i in range(4)]
    wdt = [k.sb(f"e_wd{i}", [128, NJ, 256], BF16) for i in range(2)]
    sg = [k.sb(f"e_sg{i}", [128, 512], BF16) for i in range(2)]
    yg = k.sb("e_yg", [128, NS // 128, 1024], BF16)
    n = 0
    for e in range(8):
        xe = xg[e % 2]
        k.dma("sp", xe[:], s_xg[e].rearrange("(kc p) s -> p kc s", p=128), xe, [s_xg])
        for jb in range(NJ // 2):
            a, b = wgt[jb % 4], wut[jb % 4]
            load_w(k, "pool", a, ewg[e], 0, 1024, jb * 256, jb * 256 + 256, 8)
            load_w(k, "pool", b, ewu[e], 0, 1024, jb * 256, jb * 256 + 256, 8)
            for jj in range(2):
                j = jb * 2 + jj
                for tg in range(NS // 512):
                    ts_ = slice(tg * 512, (tg + 1) * 512)
                    pg, pu = (B[0], B[1]) if n % 2 == 0 else (B[2], B[3])
                    s_ = sg[n % 2]; n += 1
                    for kc in range(8):
                        k.mm(pg[:], a[:, kc, jj * 128:(jj + 1) * 128], xe[:, kc, ts_], kc == 0, kc == 7, [a, xe], [pg])
                    for kc in range(8):
                        k.mm(pu[:], b[:, kc, jj * 128:(jj + 1) * 128], xe[:, kc, ts_], kc == 0, kc == 7, [b, xe], [pu])
                    k.act(s_[:], pg[:], AF.Silu, [pg], [s_])
                    k.tt("dve", hT[:, j, ts_], pu[:], s_[:], ALU.mult, [pu, s_], [hT])
        for nbb in range(4):
            wd_ = wdt[nbb % 2]
            k.dma("pool", wd_[:], ewd[e][:, nbb * 256:(nbb + 1) * 256].rearrange("(j p) n -> p j n", p=128), wd_)
            for st in range(NS // 128):
                pb = B[4 + (st % 4)]
                for j in range(NJ):
                    k.mm(pb[:, 0:256], hT[:, j, st * 128:(st + 1) * 128], wd_[:, j, :], j == 0, j == NJ - 1, [hT, wd_], [pb])
                k.ts("dve", yg[:, st, nbb * 256:(nbb + 1) * 256], pb[:, 0:256], gsl[:, e, st:st + 1], None, ALU.mult, None,
                     [pb, gsl], [yg])
        k.dma("sp", s_yg[e].rearrange("(st p) f -> p st f", p=128), yg[:], s_yg, [yg])
    k.end_phase()


def combine_phase(k, cst, cols, B, s_yg, s_PT, s_xl, outT, wp_d, wgate_d, pT_d):
    k.phase()
    ygl = k.sb("c_ygl", [128, 8, 2, 1024], BF16); PT = k.sb("c_PT", [128, 8, 2, 512], BF16)
    x32 = k.sb("c_x32", [128, 8, 512]); x2bf = k.sb("c_x2bf", [128, 8, 512], BF16)
    wgate = k.sb("c_wgate", [128, 8, 1024], BF16); wproj = k.sb("c_wproj", [128, 2, 1024], BF16)
    load_w(k, "pool", wgate, wgate_d, 0, 1024, 0, 1024, 8)
    load_w(k, "pool", wproj, wp_d, 0, 256, 0, 1024, 2)
    pTb = k.sb("c_pTb", [128, 2, 512], BF16); gt = k.sb("c_gt", [128, 512])
    tmp = dict(ybf=k.sb("ybf", [128, 8, 512], BF16), ysq=k.sb("ysq", [128, 8, 512], BF16), mean=k.sb("mean", [128, 512]),
               rstd=k.sb("rstd", [128, 512]), msq=k.sb("msq", [128, 512]))
    pb0 = COLS["ple_b1"][0]
    for g in range(4):
        gs = slice(g * 512, (g + 1) * 512)
        for e in range(8):
            k.dma("sp", ygl[:, e, :, :], s_yg[e, g * CAP:(g + 1) * CAP, :].rearrange("(st p) f -> p st f", p=128), ygl, [s_yg])
        k.dma("sp", PT[:], s_PT[g].rearrange("p (a b c) -> p a b c", a=8, b=2), PT, [s_PT])
        k.dma("sp", x32[:], s_xl[:, gs].rearrange("(kc p) t -> p kc t", p=128), x32, [s_xl])
        for nb in range(8):
            pb = B[nb % 2]
            i = 0
            for e in range(8):
                for st in range(2):
                    k.mm(pb[:], ygl[:, e, st, nb * 128:(nb + 1) * 128], PT[:, e, st, :], i == 0, i == 15, [ygl, PT], [pb])
                    i += 1
            k.stt("dve", x32[:, nb, :], x32[:, nb, :], ALPHA, pb[:], ALU.mult, ALU.add, [x32, pb], [x32])
        ln_fm(k, cst, x32, B[0], B[1], "o_ln2_g", "o_ln2_b", cols, x32, x2bf, 512, tmp)
        k.dma("pool", pTb[:], pT_d[:, gs].rearrange("(kc p) t -> p kc t", p=128), pTb)
        for nb in range(8):
            ns = slice(nb * 128, (nb + 1) * 128)
            pg, pp = (B[0], B[1]) if nb % 2 == 0 else (B[2], B[3])
            for kc in range(8):
                k.mm(pg[:], wgate[:, kc, ns], x2bf[:, kc, :], kc == 0, kc == 7, [wgate, x2bf], [pg])
            for kc in range(2):
                k.mm(pp[:], wproj[:, kc, ns], pTb[:, kc, :], kc == 0, kc == 1, [wproj, pTb], [pp])
            k.act(gt[:], pg[:], AF.Sigmoid, [pg, cols], [gt], bias=cols[:, pb0 + nb:pb0 + nb + 1], scale=1.0)
            k.tt("dve", gt[:], gt[:], pp[:], ALU.mult, [gt, pp], [gt])
            k.tt("pool", x32[:, nb, :], x32[:, nb, :], gt[:], ALU.add, [x32, gt], [x32])
        k.dma("sp", outT[:, gs].rearrange("(kc p) t -> p kc t", p=128), x32[:], outT, [x32])
    k.end_phase()


def fused_inputs(inp, c):
    b, h = c // 2, c % 2
    d = l1_inputs(inp, c)
    p = np.asarray(inp["p"], np.float32)
    sl = slice(h * NT, (h + 1) * NT)
    lamv = np.stack([inp["o_lam_q1"][0], inp["o_lam_k1"][0], inp["o_lam_q2"][0], inp["o_lam_k2"][0]]).astype(np.float32)
    d.update({
        "p0pT": np.ascontiguousarray(p[0, b, 0:NT].T),
        "p1T": np.ascontiguousarray(p[1, b, sl].T),
        "lamv": np.ascontiguousarray(np.broadcast_to(lamv, (128, 4, 64))),
        "gsub": np.ascontiguousarray(np.broadcast_to(np.asarray(inp["o_subln_g"][0], np.float32), (128, 128))),
        "w_out1": np.asarray(inp["o_w_out"][0], np.float32), "w_router": np.asarray(inp["o_router"][0], np.float32),
        "exp_wg": np.asarray(inp["o_exp_wg"][0], np.float32), "exp_wu": np.asarray(inp["o_exp_wu"][0], np.float32),
        "exp_wd": np.asarray(inp["o_exp_wd"][0], np.float32),
        "wp1": np.asarray(inp["ple_w_proj"][1], np.float32), "wg1": np.asarray(inp["ple_w_gate"][1], np.float32),
    })
    return d


_CACHE = {}


def kernel(**inputs):
    inp = {k_: np.asarray(v) for k_, v in inputs.items()}
    if "k" not in _CACHE:
        kb = KB(); build_fused(kb); _CACHE["k"] = kb
    kb = _CACHE["k"]
    res = run_bass_kernel_spmd(kb.nc, [fused_inputs(inp, c) for c in range(8)], core_ids=list(range(8))).results
    out = np.empty((4, 2 * NT, 1024), np.float32)
    for c in range(8):
        b, h = c // 2, c % 2
        out[b, h * NT:(h + 1) * NT] = res[c]["outT"].T
    return out
```

```python
import numpy as np
import ml_dtypes
import concourse.bass as bass
import concourse.mybir as mybir
from concourse.bass_utils import run_bass_kernel_spmd

AF = mybir.ActivationFunctionType
ALU = mybir.AluOpType
AX = mybir.AxisListType
F32 = mybir.dt.float32
BF16 = mybir.dt.bfloat16
I32 = mybir.dt.int32


class Buf:
    __slots__ = ("name", "w", "r", "dsem", "dcount", "genw", "uid")
    _n = [0]

    def __init__(self, name):
        self.name = name
        self.w = None
        self.r = {}
        self.dsem = None
        self.dcount = 0
        self.genw = None
        Buf._n[0] += 1
        self.uid = Buf._n[0]


class Emitter:
    def __init__(self, nc):
        self.nc = nc
        self.eng = {"pe": nc.tensor, "dve": nc.vector, "act": nc.scalar, "pool": nc.gpsimd, "sp": nc.sync}
        self.ep = {e: 0 for e in self.eng}
        self.free_dsems = []
        self.sem = {e: nc.alloc_semaphore("sem_" + e) for e in self.eng}
        self.cnt = {e: 0 for e in self.eng}
        self.seen = {e: {} for e in self.eng}
        self.semobj = {}
        for e, s in self.sem.items():
            self.semobj[("e", e, 0)] = s
        self.nbuf = 0
        self.dbufs = []

    def buf(self, name=None):
        self.nbuf += 1
        return Buf(name or f"b{self.nbuf}")

    def _need(self, e, tok, out):
        if tok is None:
            return
        key, val = tok
        if key[0] == "e" and key[2] < self.ep[key[1]]:
            return
        if key == ("e", e, self.ep[e]):
            if e == "pe":
                return
        if self.seen[e].get(key, 0) >= val:
            return
        if out.get(key, 0) < val:
            out[key] = val

    def _collect(self, e, reads, writes):
        need = {}
        for b in reads:
            self._need(e, b.w, need)
        for b in writes:
            self._need(e, b.w, need)
            for k, v in b.r.items():
                self._need(e, (k, v), need)
        return need

    def _dowaits(self, e, need):
        for key, val in need.items():
            self.eng[e].wait_ge(self.semobj[key], val)
            self.seen[e][key] = val

    def op(self, e, fn, reads=(), writes=()):
        need = self._collect(e, reads, writes)
        self._dowaits(e, need)
        ins = fn(self.eng[e])
        ins.then_inc(self.sem[e], 1)
        self.cnt[e] += 1
        tok = (("e", e, self.ep[e]), self.cnt[e])
        for b in writes:
            b.w = tok
            b.r = {}
            b.genw = None
        for b in reads:
            if b.r.get(tok[0], 0) < tok[1]:
                b.r[tok[0]] = tok[1]
        return tok

    def dma(self, q, out, in_, dst, srcs=(), **kw):
        if dst.dsem is None:
            if self.free_dsems and (q != "pool" or self.nc.free_len() < 2):
                dst.dsem, dst.dcount = self.free_dsems.pop()
            else:
                self.nds = getattr(self, "nds", 0) + 1
                dst.dsem = self.nc.alloc_semaphore(f"dsem_{self.nds}")
            self.semobj[("d", dst.uid)] = dst.dsem
            self.dbufs.append(dst)
        dkey = ("d", dst.uid)
        same_gen = dst.w is not None and dst.w[0] == dkey and not dst.r and dst.genw is not None
        need = {}
        for b in srcs:
            self._need(q, b.w, need)
        if same_gen:
            for key, val in dst.genw.items():
                self._need(q, (key, val), need)
        else:
            gen = {}
            if dst.w is not None:
                gen[dst.w[0]] = dst.w[1]
            for k, v in dst.r.items():
                gen[k] = max(gen.get(k, 0), v)
            for key, val in gen.items():
                self._need(q, (key, val), need)
            dst.genw = gen
        self._dowaits(q, need)
        ins = self.eng[q].dma_start(out=out, in_=in_, **kw)
        dst.dcount += 16
        ins.then_inc(dst.dsem, 16)
        tok = (dkey, dst.dcount)
        dst.w = tok
        dst.r = {}
        for b in srcs:
            if b.r.get(dkey, 0) < dst.dcount:
                b.r[dkey] = dst.dcount
        return tok

    def barrier(self):
        for e in self.eng:
            need = {}
            for f in self.eng:
                if f != e and self.cnt[f] > 0:
                    self._need(e, (("e", f, self.ep[f]), self.cnt[f]), need)
            for b in self.dbufs:
                self._need(e, (("d", b.uid), b.dcount), need)
            self._dowaits(e, need)
        for e in self.eng:
            if self.cnt[e] > 12000:
                self.ep[e] += 1
                self.sem[e] = self.nc.alloc_semaphore(f"sem_{e}_{self.ep[e]}")
                self.semobj[("e", e, self.ep[e])] = self.sem[e]
                self.cnt[e] = 0

    def retire(self, bufs):
        for b in bufs:
            if b.dsem is not None:
                self.free_dsems.append((b.dsem, b.dcount))
                self.dbufs = [x for x in self.dbufs if x is not b]
                b.dsem = None

    def wait_all(self, e, bufs):
        need = {}
        for b in bufs:
            self._need(e, b.w, need)
        self._dowaits(e, need)


class T:
    def __init__(self, h, b, excl=False):
        self.h = h
        self.b = b
        self.excl = excl

    def __getitem__(self, idx):
        return self.h[idx]


def _bufs(xs):
    return [getattr(x, "b", x) for x in xs]


class KB:
    def __init__(self):
        import contextlib
        self.nc = bass.Bass("TRN2", target_bir_lowering=False)
        self.em = Emitter(self.nc)
        self.n = 0
        self.gstack = contextlib.ExitStack()
        self.stack = self.gstack

    def phase(self):
        import contextlib
        self.stack = contextlib.ExitStack()
        self.pbufs = []
        return self.stack

    def end_phase(self):
        self.em.barrier()
        self.em.retire(self.pbufs)
        self.pbufs = []
        self.stack.close()
        self.stack = self.gstack

    def din(self, name, shape, dt=F32):
        return self.nc.dram_tensor(name, list(shape), dt, kind="ExternalInput").ap()

    def dout(self, name, shape, dt=F32):
        return T(self.nc.dram_tensor(name, list(shape), dt, kind="ExternalOutput").ap(), self.em.buf(name))

    def dscr(self, name, shape, dt=F32):
        return T(self.nc.dram_tensor(name, list(shape), dt, kind="Internal").ap(), self.em.buf(name))

    def sb(self, name, shape, dt=F32):
        self.n += 1
        h = self.stack.enter_context(self.nc.sbuf_tensor(f"{name}_{self.n}", list(shape), dt))
        t = T(h, self.em.buf(name))
        if self.stack is not self.gstack:
            self.pbufs.append(t.b)
        return t

    def psum(self, name, shape, dt=F32):
        return self.gstack.enter_context(self.nc.psum_tensor(name, list(shape), dt))

    def view(self, ap, name="v"):
        return T(ap, self.em.buf(name))

    def op(self, e, fn, reads=(), writes=()):
        r, w = [], _bufs(writes)
        for x in reads:
            if getattr(x, "excl", False):
                w.append(x.b)
            else:
                r.append(getattr(x, "b", x))
        return self.em.op(e, fn, r, w)

    def dma(self, q, out, in_, dst, srcs=(), **kw):
        return self.em.dma(q, out, in_, getattr(dst, "b", dst), _bufs(srcs), **kw)

    def mm(self, out, lhsT, rhs, start, stop, reads, writes, skip=False):
        if skip:
            self.op("pe", lambda e: e.matmul(out, lhsT, rhs, start=start, stop=stop, skip_group_check=True), reads, writes)
        else:
            self.op("pe", lambda e: e.matmul(out, lhsT, rhs, start=start, stop=stop), reads, writes)

    def act(self, out, in_, func, reads, writes, bias=None, scale=None, eng="act"):
        kw = {}
        if bias is not None:
            kw["bias"] = bias
        if scale is not None:
            kw["scale"] = scale
        self.op("act", lambda e: e.activation(out=out, in_=in_, func=func, **kw), reads, writes)

    def tt(self, e, out, a, b, op, reads, writes):
        self.op(e, lambda g: g.tensor_tensor(out=out, in0=a, in1=b, op=op), reads, writes)

    def ts(self, e, out, a, s1, s2, op0, op1, reads, writes):
        if s2 is None:
            self.op(e, lambda g: g.tensor_scalar(out=out, in0=a, scalar1=s1, scalar2=None, op0=op0), reads, writes)
        else:
            self.op(e, lambda g: g.tensor_scalar(out=out, in0=a, scalar1=s1, scalar2=s2, op0=op0, op1=op1), reads, writes)

    def stt(self, e, out, a, s, b, op0, op1, reads, writes):
        self.op(e, lambda g: g.scalar_tensor_tensor(out=out, in0=a, scalar=s, in1=b, op0=op0, op1=op1), reads, writes)

    def copy(self, e, out, in_, reads, writes):
        if e == "act":
            self.op(e, lambda g: g.copy(out=out, in_=in_), reads, writes)
        else:
            self.op(e, lambda g: g.tensor_copy(out=out, in_=in_), reads, writes)

    def rsqrt(self, out, in_, scale, eps, reads, writes):
        self.op("act", lambda e: e.activation(out=out, in_=in_, func=AF.Sqrt, bias=eps, scale=scale), reads, writes)
        self.op("dve", lambda e: e.reciprocal(out=out, in_=out), writes, writes)

    def memset(self, e, ap, val, writes):
        self.op(e, lambda g: g.memset(ap, val), (), writes)


ALPHA = (2.0 * 2) ** 0.25
EPS = 1e-5
NT = 2048

COLS = {}
_c = 0
for _nm, _n in [("e_ln1_g", 8), ("e_ln1_b", 8), ("e_ln2_g", 8), ("e_ln2_b", 8), ("o_ln1_g", 8), ("o_ln1_b", 8),
                ("o_ln2_g", 8), ("o_ln2_b", 8), ("ple_b0", 8), ("ple_b1", 8), ("b_norm_g", 1), ("bsT", 8),
                ("flag", 1), ("nbias", 1), ("gsubc", 1)]:
    COLS[_nm] = (_c, _n)
    _c += _n
NCOLS = _c


def make_consts(k):
    c = {}
    it = k.sb("iota_i", [128, 128], I32)
    k.op("pool", lambda g: g.iota(it[:], pattern=[[1, 128]], base=0, channel_multiplier=-1), (), [it])
    d = k.sb("iota_f", [128, 128], F32)
    k.copy("dve", d[:], it[:], [it], [d])
    tri = k.sb("tri", [128, 128], F32)
    k.ts("dve", tri[:], d[:], 0.0, None, ALU.is_ge, None, [d], [tri])
    idf = k.sb("idf", [128, 128], F32)
    k.ts("dve", idf[:], d[:], 0.0, None, ALU.is_equal, None, [d], [idf])
    ident = k.sb("ident", [128, 128], BF16)
    k.copy("dve", ident[:], idf[:], [idf], [ident])
    trib = k.sb("trib", [128, 128], BF16)
    k.copy("dve", trib[:], tri[:], [tri], [trib])
    triblk = k.sb("triblk", [128, 128], F32)
    k.copy("dve", triblk[:], tri[:], [tri], [triblk])
    k.memset("dve", triblk[0:64, 64:128], 0.0, [triblk])
    onesblk = k.sb("onesblk", [128, 128], F32)
    k.memset("dve", onesblk[:], 0.0, [onesblk])
    k.memset("dve", onesblk[0:64, 0:64], 1.0, [onesblk])
    k.memset("dve", onesblk[64:128, 64:128], 1.0, [onesblk])
    tris = k.sb("tris", [128, 128], F32)
    k.ts("dve", tris[:], triblk[:], -1.0 / 16, None, ALU.mult, None, [triblk], [tris])
    dmat = k.sb("dmat", [128, 128], F32)
    k.tt("dve", dmat[:], onesblk[:], triblk[:], ALU.subtract, [onesblk, triblk], [dmat])
    k.ts("dve", dmat[:], dmat[:], -1.0 / 16, None, ALU.mult, None, [dmat], [dmat])
    triblkb = k.sb("triblkb", [128, 128], BF16)
    k.copy("dve", triblkb[:], triblk[:], [triblk], [triblkb])
    ones = k.sb("ones", [128, 128], BF16)
    k.memset("dve", ones[:], 1.0, [ones])
    rm0 = k.sb("rm0", [128, 1], F32)
    rm1 = k.sb("rm1", [128, 1], F32)
    k.memset("dve", rm0[:], 0.0, [rm0]); k.memset("dve", rm0[0:64, :], 1.0, [rm0])
    k.memset("dve", rm1[:], 0.0, [rm1]); k.memset("dve", rm1[64:128, :], 1.0, [rm1])
    c.update(tri=tri, ident=ident, trib=trib, triblk=triblk, tris=tris, dmat=dmat, triblkb=triblkb, ones=ones,
             rm0=rm0, rm1=rm1, idf=idf)
    return c


def ln_fm(k, cst, y, ps1, ps2, gcol, bcol, cols, o32, obf, W, tmp):
    ybf, ysq, mean, rstd = tmp["ybf"], tmp["ysq"], tmp["mean"], tmp["rstd"]
    for ch in range(8):
        k.copy("dve", ybf[:, ch, :], y[:, ch, :], [y], [ybf])
        k.act(ysq[:, ch, :], y[:, ch, :], AF.Square, [y], [ysq])
    for ch in range(8):
        k.mm(ps1[:, 0:W], cst["ones"][:], ybf[:, ch, :], ch == 0, ch == 7, [ybf, cst["ones"]], [ps1])
    for ch in range(8):
        k.mm(ps2[:, 0:W], cst["ones"][:], ysq[:, ch, :], ch == 0, ch == 7, [ysq, cst["ones"]], [ps2])
    k.ts("dve", mean[:], ps1[:, 0:W], 1.0 / 1024, None, ALU.mult, None, [ps1], [mean])
    msq = tmp["msq"]
    k.tt("dve", msq[:], mean[:], mean[:], ALU.mult, [mean], [msq])
    k.stt("dve", rstd[:], ps2[:, 0:W], 1.0 / 1024, msq[:], ALU.mult, ALU.subtract, [ps2, msq], [rstd])
    k.rsqrt(rstd[:], rstd[:], 1.0, EPS, [rstd], [rstd])
    g0 = COLS[gcol][0]
    b0 = COLS[bcol][0]
    for ch in range(8):
        e = "dve"
        k.tt(e, o32[:, ch, :], y[:, ch, :], mean[:], ALU.subtract, [y, mean], [o32])
        k.tt(e, o32[:, ch, :], o32[:, ch, :], rstd[:], ALU.mult, [o32, rstd], [o32])
        k.ts("dve", o32[:, ch, :], o32[:, ch, :], cols[:, g0 + ch:g0 + ch + 1], cols[:, b0 + ch:b0 + ch + 1],
             ALU.mult, ALU.add, [o32, cols], [o32])
        if obf is not None:
            k.copy("act", obf[:, ch, :], o32[:, ch, :], [o32], [obf])


def psum_banks(k):
    banks = [k.psum(f"pb{i}", [128, 512], F32) for i in range(8)]
    bbufs = [k.em.buf(f"psbank{i}") for i in range(8)]

    def reg(b, c0, c1, parts=128, name=None):
        return T(banks[b][0:parts, c0:c1], bbufs[b], excl=True)
    return banks, reg


def load_w(k, q, wt, dram, r0, r1, c0, c1, kcn):
    src = dram[r0:r1, c0:c1].rearrange("(kc p) n -> p kc n", p=128)
    k.dma(q, wt[:, 0:kcn, 0:c1 - c0], src, wt)


def mixer_phase(k, I, cst, cols, banks, reg, xT, xpT, s_xln1):
    w_in, wsT_d, alng, alnb, wup_d, w_out0 = I["w_in"], I["wsT"], I["alng"], I["alnb"], I["wup"], I["w_out0"]
    B0, B1, B2, B4 = reg(0, 0, 512), reg(1, 0, 512), reg(2, 0, 512), reg(4, 0, 512)
    B2bf = T(banks[2][:, :].bitcast(BF16), B2.b, excl=True)
    Rk, Rz = reg(3, 0, 256), reg(3, 256, 512)
    Rd, Rgl = reg(5, 0, 256), reg(5, 256, 384, 16)
    RbT = [reg(2 * h, 0, 128, 64) for h in range(4)]; RqT = [reg(2 * h, 128, 256, 64) for h in range(4)]
    RkT = [reg(2 * h, 256, 384, 64) for h in range(4)]; Rsc = [reg(2 * h, 384, 512) for h in range(4)]
    Rkv1 = RbT
    RoT = [reg(2 * h + 1, 0, 128) for h in range(4)]; Rss = [reg(2 * h + 1, 128, 256) for h in range(4)]
    RrT = [reg(2 * h + 1, 256, 384) for h in range(4)]; Rkv0 = [reg(2 * h + 1, 384, 512, 64) for h in range(4)]

    k.phase()
    win = k.sb("win", [128, 8, 2576], BF16)
    k.dma("pool", win[:], w_in.rearrange("(kc p) n -> p kc n", p=128), win)
    wout = k.sb("wout", [128, 8, 1024], BF16); load_w(k, "pool", wout, w_out0, 0, 1024, 0, 1024, 8)
    wsT = k.sb("wsT", [128, 8, 128], BF16)
    k.dma("pool", wsT[:], wsT_d.rearrange("g s t -> s g t"), wsT)
    for g in range(8):
        k.tt("dve", wsT[:, g, :], wsT[:, g, :], cst["trib"][:], ALU.mult, [wsT, cst["trib"]], [wsT])
    glng = k.sb("glng", [128, 512]); k.dma("sp", glng[:], alng, glng)
    glnb = k.sb("glnb", [128, 512]); k.dma("sp", glnb[:], alnb, glnb)
    wup = k.sb("wup", [32, 256], BF16); k.dma("pool", wup[:], wup_d, wup)
    glT = k.sb("glT", [32, 128], BF16); k.memset("dve", glT[:], 1.0, [glT])
    S32 = [[k.sb(f"S32_{h}_{i}", [64, 128]) for i in range(2)] for h in range(4)]
    Sbf = [[k.sb(f"Sbf_{h}_{i}", [64, 128], BF16) for i in range(2)] for h in range(4)]
    for h in range(4):
        k.memset("dve", S32[h][0][:], 0.0, [S32[h][0]])
    xb = [k.sb(f"xb{i}", [128, 8, 512], BF16) for i in range(2)]
    x32 = k.sb("x32", [128, 8, 512])
    mixT = k.sb("mixT", [128, 8, 512], BF16)
    u_sb = k.sb("u_sb", [128, 512], BF16); v_sb = k.sb("v_sb", [128, 512]); vq = k.sb("vq", [128, 512])
    vln = k.sb("vln", [128, 512], BF16); ya = k.sb("ya", [128, 512], BF16); vb_sb = k.sb("vb_sb", [128, 512], BF16)
    st = k.sb("st", [128, 8]); la = k.sb("la", [128, 256]); ekl = k.sb("ekl", [128, 256])
    klf = k.sb("klf", [128, 256]); kl0 = k.sb("kl0", [128, 256], BF16); kl1 = k.sb("kl1", [128, 256], BF16)
    k.memset("dve", kl0[:], 0.0, [kl0]); k.memset("dve", kl1[:], 0.0, [kl1])
    H4 = range(4)
    bT_sb = [k.sb(f"bT_sb{h}", [64, 128]) for h in H4]; nref = [k.sb(f"nref{h}", [64, 2]) for h in H4]
    eA = [k.sb(f"eA{h}", [64, 128]) for h in H4]; eB = [k.sb(f"eB{h}", [64, 128]) for h in H4]
    eI = [k.sb(f"eI{h}", [64, 128]) for h in H4]; qd = [k.sb(f"qd{h}", [64, 128], BF16) for h in H4]
    kd = [k.sb(f"kd{h}", [64, 128], BF16) for h in H4]; qi = [k.sb(f"qi{h}", [64, 128], BF16) for h in H4]
    sc_sb = [k.sb(f"sc_sb{h}", [128, 128], BF16) for h in H4]; osq = [k.sb(f"osq{h}", [128, 128], BF16) for h in H4]
    rs_sb = [k.sb(f"rs_sb{h}", [128, 128]) for h in H4]; on = [k.sb(f"on{h}", [128, 128]) for h in H4]
    sr = [k.sb(f"sr{h}", [128, 128]) for h in H4]
    y = k.sb("y", [128, 8, 512])
    tmp = dict(ybf=k.sb("ybf", [128, 8, 512], BF16), ysq=k.sb("ysq", [128, 8, 512], BF16), mean=k.sb("mean", [128, 512]),
               rstd=k.sb("rstd", [128, 512]), msq=k.sb("msq", [128, 512]))
    bn0 = COLS["b_norm_g"][0]; bs0 = COLS["bsT"][0]; fl0 = COLS["flag"][0]

    def gla_tok(xbt, c0):
        gla_tok_a(xbt, c0); gla_tok_b(); gla_tok_c()

    def gla_tok_a(xbt, c0):
        for kc in range(8):
            k.mm(Rk[:], xbt[:, kc, c0:c0 + 128], win[:, kc, 1280:1536], kc == 0, kc == 7, [xbt, win], [Rk])
        for kc in range(8):
            k.mm(B4[:], xbt[:, kc, c0:c0 + 128], win[:, kc, 1536:2048], kc == 0, kc == 7, [xbt, win], [B4])
        for kc in range(8):
            k.mm(Rgl[:], win[:, kc, 2560:2576], xbt[:, kc, c0:c0 + 128], kc == 0, kc == 7, [xbt, win], [Rgl])

    def gla_tok_b():
        k.copy("dve", glT[0:16, :], Rgl[:], [Rgl], [glT])
        k.copy("act", vb_sb[:], B4[:], [B4], [vb_sb])
        k.mm(Rz[:], glT[:], wup[:], True, True, [glT, wup], [Rz])
        k.act(la[:], Rz[:], AF.Exp, [Rz], [la], scale=-1.0)
        k.act(la[:], la[:], AF.Ln, [la], [la], bias=1.0)
        k.mm(Rd[:], cst["dmat"][:], la[:], True, True, [la, cst["dmat"]], [Rd])
        k.act(ekl[:], Rd[:], AF.Exp, [Rd], [ekl])

    def gla_tok_c():
        k.tt("dve", kl0[0:64, :], Rk[0:64, :], ekl[0:64, :], ALU.mult, [Rk, ekl], [kl0])
        k.tt("dve", kl1[64:128, :], Rk[64:128, :], ekl[64:128, :], ALU.mult, [Rk, ekl], [kl1])

    def gla_head_decay(h):
        k.mm(RbT[h][:], la[:, h * 64:(h + 1) * 64], cst["tris"][:], True, True, [la, cst["tris"]], [RbT[h]])
        k.copy("dve", bT_sb[h][:], RbT[h][:], [RbT[h]], [bT_sb[h]])
        k.act(eI[h][:], bT_sb[h][:], AF.Exp, [bT_sb[h]], [eI[h]])

    def gla_state(h):
        k.mm(Rkv0[h][:], kl0[:, h * 64:(h + 1) * 64], vb_sb[:, h * 128:(h + 1) * 128], True, True, [kl0, vb_sb], [Rkv0[h]])
        k.mm(Rkv1[h][:], kl1[:, h * 64:(h + 1) * 64], vb_sb[:, h * 128:(h + 1) * 128], True, True, [kl1, vb_sb], [Rkv1[h]])
        k.stt("dve", S32[h][1][:], S32[h][0][:], eI[h][:, 63:64], Rkv0[h][:], ALU.mult, ALU.add,
              [S32[h][0], eI[h], Rkv0[h]], [S32[h][1]])
        k.copy("act", Sbf[h][1][:], S32[h][1][:], [S32[h][1]], [Sbf[h][1]])
        k.stt("dve", S32[h][0][:], S32[h][1][:], eI[h][:, 127:128], Rkv1[h][:], ALU.mult, ALU.add,
              [S32[h][1], eI[h], Rkv1[h]], [S32[h][0]])

    for g in range(4 if xpT is not None else 0):
        xbt = xb[g % 2]
        k.dma("pool", xbt[:], xpT[:, g * 512:(g + 1) * 512].rearrange("(kc p) t -> p kc t", p=128), xbt)
        for tt in range(4):
            gla_tok(xbt, tt * 128)
            for h in range(4):
                gla_head_decay(h)
                gla_state(h)
    for h in range(4):
        k.ts("dve", S32[h][0][:], S32[h][0][:], cols[0:64, fl0:fl0 + 1], None, ALU.mult, None, [S32[h][0], cols], [S32[h][0]])
        k.copy("act", Sbf[h][0][:], S32[h][0][:], [S32[h][0]], [Sbf[h][0]])

    for g in range(4):
        xbt = xb[g % 2]
        gs = slice(g * 512, (g + 1) * 512)
        k.dma("pool", xbt[:], xT[:, gs].rearrange("(kc p) t -> p kc t", p=128), xbt)
        k.dma("sp", x32[:], xT[:, gs].rearrange("(kc p) t -> p kc t", p=128), x32)
        for tt in range(4):
            c0 = tt * 128
            cs = slice(c0, c0 + 128)
            for kc in range(8):
                k.mm(B0[:], xbt[:, kc, cs], win[:, kc, 0:512], kc == 0, kc == 7, [xbt, win], [B0])
            for kc in range(8):
                k.mm(B1[:], xbt[:, kc, cs], win[:, kc, 512:1024], kc == 0, kc == 7, [xbt, win], [B1])
            gla_tok_a(xbt, c0)
            k.act(u_sb[:], B0[:], AF.Gelu_apprx_tanh, [B0], [u_sb])
            k.act(v_sb[:], B1[:], AF.Gelu_apprx_tanh, [B1], [v_sb])
            gla_tok_b()
            k.op("dve", lambda e: e.reduce_sum(out=st[:, 0:1], in_=v_sb[:], axis=AX.X), [v_sb], [st])
            k.tt("dve", vq[:], v_sb[:], v_sb[:], ALU.mult, [v_sb], [vq])
            k.op("dve", lambda e: e.reduce_sum(out=st[:, 1:2], in_=vq[:], axis=AX.X), [vq], [st])
            k.ts("dve", st[:, 2:3], st[:, 0:1], 1.0 / 512, None, ALU.mult, None, [st], [st])
            k.tt("dve", st[:, 3:4], st[:, 2:3], st[:, 2:3], ALU.mult, [st], [st])
            k.stt("dve", st[:, 4:5], st[:, 1:2], 1.0 / 512, st[:, 3:4], ALU.mult, ALU.subtract, [st], [st])
            k.rsqrt(st[:, 5:6], st[:, 4:5], 1.0, EPS, [st], [st])
            k.stt("dve", st[:, 6:7], st[:, 2:3], -1.0, st[:, 5:6], ALU.mult, ALU.mult, [st], [st])
            k.ts("dve", vq[:], v_sb[:], st[:, 5:6], st[:, 6:7], ALU.mult, ALU.add, [v_sb, st], [vq])
            k.tt("dve", vq[:], vq[:], glng[:], ALU.mult, [vq, glng], [vq])
            k.tt("dve", vln[:], vq[:], glnb[:], ALU.add, [vq, glnb], [vln])
            gla_tok_c()
            for h in H4:
                gla_head_decay(h)
            for gg in range(8):
                fs = slice(gg * 64, (gg + 1) * 64)
                k.mm(B2[:, fs], wsT[:, gg, :], vln[:, fs], True, True, [wsT, vln], [B2])
            for gg in range(8):
                fs = slice(gg * 64, (gg + 1) * 64)
                k.stt("dve", ya[:, fs], B2[:, fs], cols[:, bs0 + gg:bs0 + gg + 1], u_sb[:, fs], ALU.add, ALU.mult,
                      [B2, cols, u_sb], [ya])
            for i in range(4):
                k.op("pe", lambda e: e.transpose(B2bf[:, i * 128:(i + 1) * 128], ya[:, i * 128:(i + 1) * 128], cst["ident"][:]),
                     [ya, cst["ident"]], [B2bf])
            k.copy("act", mixT[:, 0:4, cs], B2bf[:, 0:512].rearrange("p (a b) -> p a b", a=4), [B2bf], [mixT])
            for h in H4:
                k.ts("dve", nref[h][:, 0:1], bT_sb[h][:, 31:32], -1.0, None, ALU.mult, None, [bT_sb[h]], [nref[h]])
                k.ts("dve", nref[h][:, 1:2], bT_sb[h][:, 95:96], -1.0, None, ALU.mult, None, [bT_sb[h]], [nref[h]])
            for h in H4:
                for c in range(2):
                    hs = slice(c * 64, (c + 1) * 64)
                    k.act(eA[h][:, hs], bT_sb[h][:, hs], AF.Exp, [bT_sb[h], nref[h]], [eA[h]], bias=nref[h][:, c:c + 1], scale=1.0)
                    k.act(eB[h][:, hs], bT_sb[h][:, hs], AF.Exp, [bT_sb[h]], [eB[h]],
                          bias=bT_sb[h][:, 31 + 64 * c:32 + 64 * c], scale=-1.0)
            for h in H4:
                for kc in range(8):
                    k.mm(RqT[h][:], win[:, kc, 1024 + h * 64:1088 + h * 64], xbt[:, kc, cs], kc == 0, kc == 7, [xbt, win], [RqT[h]])
                for kc in range(8):
                    k.mm(RkT[h][:], win[:, kc, 1280 + h * 64:1344 + h * 64], xbt[:, kc, cs], kc == 0, kc == 7, [xbt, win], [RkT[h]])
            for h in H4:
                k.stt("dve", qd[h][:], RqT[h][:], 0.125, eA[h][:], ALU.mult, ALU.mult, [RqT[h], eA[h]], [qd[h]])
                k.tt("dve", kd[h][:], RkT[h][:], eB[h][:], ALU.mult, [RkT[h], eB[h]], [kd[h]])
                k.stt("dve", qi[h][:], RqT[h][:], 0.125, eI[h][:], ALU.mult, ALU.mult, [RqT[h], eI[h]], [qi[h]])
            for h in H4:
                k.mm(Rsc[h][:], kd[h][:], qd[h][:], True, True, [kd[h], qd[h]], [Rsc[h]])
            for h in H4:
                k.tt("dve", sc_sb[h][:], Rsc[h][:], cst["triblk"][:], ALU.mult, [Rsc[h], cst["triblk"]], [sc_sb[h]])
            for h in H4:
                gla_state(h)
            for h in H4:
                k.mm(RoT[h][:], vb_sb[:, h * 128:(h + 1) * 128], sc_sb[h][:], True, False, [vb_sb, sc_sb[h]], [RoT[h]])
                k.mm(RoT[h][:, 0:64], Sbf[h][0][:], qi[h][:, 0:64], False, False, [Sbf[h][0], qi[h]], [RoT[h]])
                k.mm(RoT[h][:, 64:128], Sbf[h][1][:], qi[h][:, 64:128], False, True, [Sbf[h][1], qi[h]], [RoT[h]])
            for h in H4:
                k.copy("act", Sbf[h][0][:], S32[h][0][:], [S32[h][0]], [Sbf[h][0]])
                k.act(osq[h][:], RoT[h][:], AF.Square, [RoT[h]], [osq[h]])
            for h in H4:
                k.mm(Rss[h][:], cst["ones"][:], osq[h][:], True, True, [osq[h], cst["ones"]], [Rss[h]])
            for h in H4:
                k.op("act", lambda e: e.activation(out=rs_sb[h][:], in_=Rss[h][:], func=AF.Sqrt, bias=EPS, scale=1.0 / 128),
                     [Rss[h]], [rs_sb[h]])
            for h in H4:
                k.op("dve", lambda e: e.reciprocal(out=rs_sb[h][:], in_=rs_sb[h][:]), [rs_sb[h]], [rs_sb[h]])
                k.tt("dve", on[h][:], RoT[h][:], rs_sb[h][:], ALU.mult, [RoT[h], rs_sb[h]], [on[h]])
            for h in H4:
                for kc in range(8):
                    k.mm(RrT[h][:], win[:, kc, 2048 + h * 128:2176 + h * 128], xbt[:, kc, cs], kc == 0, kc == 7, [xbt, win], [RrT[h]])
            for h in H4:
                k.act(sr[h][:], RrT[h][:], AF.Silu, [RrT[h]], [sr[h]])
            for h in H4:
                k.stt("dve", mixT[:, 4 + h, cs], on[h][:], cols[:, bn0:bn0 + 1], sr[h][:], ALU.mult, ALU.mult,
                      [on[h], cols, sr[h]], [mixT])
        for nb in range(8):
            pb = B0 if nb % 2 == 0 else B1
            for ch in range(8):
                k.mm(pb[:], wout[:, ch, nb * 128:(nb + 1) * 128], mixT[:, ch, :], ch == 0, ch == 7, [wout, mixT], [pb])
            k.stt("dve", y[:, nb, :], x32[:, nb, :], ALPHA, pb[:], ALU.mult, ALU.add, [x32, pb], [y])
        ln_fm(k, cst, y, B0, B1, "e_ln1_g", "e_ln1_b", cols, y, None, 512, tmp)
        k.dma("sp", s_xln1[:, gs].rearrange("(kc p) t -> p kc t", p=128), y[:], s_xln1, [y])
    k.end_phase()


def ffn_phase(k, cst, cols, B0, B1, B2, B3, src, dst, wg_d, wu_d, wd_d, NJ, lng, lnb, wp_d, wgate_d, pT_d, pbias):
    k.phase()
    TG = 1024
    xb = k.sb("f_xb", [128, 8, TG], BF16); x32 = k.sb("f_x32", [128, 8, TG])
    hT = k.sb("f_hT", [128, NJ, TG], BF16)
    wgt = [k.sb(f"f_wg{i}", [128, 8, 256], BF16) for i in range(4)]
    wut = [k.sb(f"f_wu{i}", [128, 8, 256], BF16) for i in range(4)]
    wdt = [k.sb(f"f_wd{i}", [128, NJ, 256], BF16) for i in range(2)]
    sg = [k.sb(f"f_sg{i}", [128, 512], BF16) for i in range(2)]
    wgate = k.sb("f_wgate", [128, 8, 1024], BF16); wproj = k.sb("f_wproj", [128, 2, 1024], BF16)
    load_w(k, "pool", wgate, wgate_d, 0, 1024, 0, 1024, 8)
    load_w(k, "pool", wproj, wp_d, 0, 256, 0, 1024, 2)
    pTb = k.sb("f_pTb", [128, 2, 512], BF16); x2bf = k.sb("f_x2bf", [128, 8, 512], BF16)
    gt = k.sb("f_gt", [128, 512]);
    tmp = dict(ybf=k.sb("ybf", [128, 8, 512], BF16), ysq=k.sb("ysq", [128, 8, 512], BF16), mean=k.sb("mean", [128, 512]),
               rstd=k.sb("rstd", [128, 512]), msq=k.sb("msq", [128, 512]))
    pb0 = COLS[pbias][0]
    NJB = (NJ + 1) // 2
    for G in range(NT // TG):
        gs = slice(G * TG, (G + 1) * TG)
        k.dma("pool", xb[:], src[:, gs].rearrange("(kc p) t -> p kc t", p=128), xb, [src])
        k.dma("sp", x32[:], src[:, gs].rearrange("(kc p) t -> p kc t", p=128), x32, [src])
        for jb in range(NJB):
            nj = min(2, NJ - jb * 2)
            a, b = wgt[jb % 4], wut[jb % 4]
            load_w(k, "pool", a, wg_d, 0, 1024, jb * 256, jb * 256 + nj * 128, 8)
            load_w(k, "pool", b, wu_d, 0, 1024, jb * 256, jb * 256 + nj * 128, 8)
            for jj in range(nj):
                j = jb * 2 + jj
                for tg in range(TG // 512):
                    ts_ = slice(tg * 512, (tg + 1) * 512)
                    pg, pu = (B0, B1) if (j * 2 + tg) % 2 == 0 else (B2, B3)
                    s_ = sg[(j * 2 + tg) % 2]
                    for kc in range(8):
                        k.mm(pg[:], a[:, kc, jj * 128:(jj + 1) * 128], xb[:, kc, ts_], kc == 0, kc == 7, [a, xb], [pg])
                    for kc in range(8):
                        k.mm(pu[:], b[:, kc, jj * 128:(jj + 1) * 128], xb[:, kc, ts_], kc == 0, kc == 7, [b, xb], [pu])
                    k.act(s_[:], pg[:], AF.Silu, [pg], [s_])
                    k.tt("dve", hT[:, j, ts_], pu[:], s_[:], ALU.mult, [pu, s_], [hT])
        for nbb in range(4):
            wd_ = wdt[nbb % 2]
            k.dma("pool", wd_[:], wd_d[:, nbb * 256:(nbb + 1) * 256].rearrange("(j p) n -> p j n", p=128), wd_)
            for nn in range(2):
                nb = nbb * 2 + nn
                for tg in range(TG // 512):
                    ts_ = slice(tg * 512, (tg + 1) * 512)
                    pb = B0 if (nb * 2 + tg) % 2 == 0 else B1
                    for j in range(NJ):
                        k.mm(pb[:], wd_[:, j, nn * 128:(nn + 1) * 128], hT[:, j, ts_], j == 0, j == NJ - 1, [wd_, hT], [pb])
                    k.stt("dve", x32[:, nb, ts_], x32[:, nb, ts_], ALPHA, pb[:], ALU.mult, ALU.add, [x32, pb], [x32])
        for tg in range(TG // 512):
            ts_ = slice(tg * 512, (tg + 1) * 512)
            yv = T(x32.h[:, :, ts_], x32.b)
            ln_fm(k, cst, yv, B0, B1, lng, lnb, cols, yv, x2bf, 512, tmp)
            t0 = G * TG + tg * 512
            k.dma("pool", pTb[:], pT_d[:, t0:t0 + 512].rearrange("(kc p) t -> p kc t", p=128), pTb)
            for nb in range(8):
                ns = slice(nb * 128, (nb + 1) * 128)
                pg, pp = (B0, B1) if nb % 2 == 0 else (B2, B3)
                for kc in range(8):
                    k.mm(pg[:], wgate[:, kc, ns], x2bf[:, kc, :], kc == 0, kc == 7, [wgate, x2bf], [pg])
                for kc in range(2):
                    k.mm(pp[:], wproj[:, kc, ns], pTb[:, kc, :], kc == 0, kc == 1, [wproj, pTb], [pp])
                k.act(gt[:], pg[:], AF.Sigmoid, [pg, cols], [gt], bias=cols[:, pb0 + nb:pb0 + nb + 1], scale=1.0)
                k.tt("dve", gt[:], gt[:], pp[:], ALU.mult, [gt, pp], [gt])
                k.tt("dve", yv[:, nb, :], yv[:, nb, :], gt[:], ALU.add, [yv, gt], [yv])
        k.dma("sp", dst[:, gs].rearrange("(kc p) t -> p kc t", p=128), x32[:], dst, [x32])
    k.end_phase()


def qkv_phase(k, B, src, w_qkv, qT_o, kT_o, v_o, koff):
    k.phase()
    wq = k.sb("wqkv", [128, 8, 3072], BF16)
    k.dma("pool", wq[:], w_qkv.rearrange("(kc p) n -> p kc n", p=128), wq)
    xb = [k.sb(f"q_xb{i}", [128, 8, 512], BF16) for i in range(2)]
    qs = k.sb("q_qs", [64, 16, 512], BF16); ks = k.sb("q_ks", [64, 16, 512], BF16); vs = k.sb("q_vs", [128, 4, 1024], BF16)
    n = 0
    for g in range(4):
        gs = slice(g * 512, (g + 1) * 512)
        xbt = xb[g % 2]
        k.dma("pool", xbt[:], src[:, gs].rearrange("(kc p) t -> p kc t", p=128), xbt, [src])
        for which, dstt in ((0, qs), (1, ks)):
            if which == 0 and qT_o is None:
                continue
            for hc in range(16):
                pb = B[n % 8]; n += 1
                c0 = which * 1024 + hc * 64
                for kc in range(8):
                    k.mm(pb[0:64, :], wq[:, kc, c0:c0 + 64], xbt[:, kc, :], kc == 0, kc == 7, [wq, xbt], [pb])
                k.copy("act" if n % 2 else "dve", dstt[:, hc, :], pb[0:64, :], [pb], [dstt])
        for tt in range(4):
            for hf in range(2):
                pb = B[n % 8]; n += 1
                for kc in range(8):
                    k.mm(pb[:], xbt[:, kc, tt * 128:(tt + 1) * 128], wq[:, kc, 2048 + hf * 512:2560 + hf * 512], kc == 0, kc == 7,
                         [wq, xbt], [pb])
                k.copy("act" if n % 2 else "dve", vs[:, tt, hf * 512:(hf + 1) * 512], pb[:], [pb], [vs])
        ko = slice(koff + g * 512, koff + (g + 1) * 512)
        if qT_o is not None:
            k.dma("sp", qT_o[:, :, gs].rearrange("a d t -> d a t"), qs[:], qT_o, [qs])
        k.dma("sp", kT_o[:, :, ko].rearrange("a d t -> d a t"), ks[:], kT_o, [ks])
        k.dma("sp", v_o[ko, :].rearrange("(tt p) f -> p tt f", p=128), vs[:], v_o, [vs])
    k.end_phase()


def _colslab(v):
    v = np.asarray(v, np.float32).reshape(-1, 128)
    return np.ascontiguousarray(v.T)


def make_cols(inp, h):
    cols = np.zeros((128, NCOLS), np.float32)

    def put(name, arr):
        c0, n = COLS[name]
        cols[:, c0:c0 + n] = arr
    put("e_ln1_g", _colslab(inp["e_ln1_g"][0])); put("e_ln1_b", _colslab(inp["e_ln1_b"][0]))
    put("e_ln2_g", _colslab(inp["e_ln2_g"][0])); put("e_ln2_b", _colslab(inp["e_ln2_b"][0]))
    put("o_ln1_g", _colslab(inp["o_ln1_g"][0])); put("o_ln1_b", _colslab(inp["o_ln1_b"][0]))
    put("o_ln2_g", _colslab(inp["o_ln2_g"][0])); put("o_ln2_b", _colslab(inp["o_ln2_b"][0]))
    put("ple_b0", _colslab(inp["ple_b_gate"][0])); put("ple_b1", _colslab(inp["ple_b_gate"][1]))
    put("b_norm_g", _colslab(inp["e_b_norm_g"][0]))
    put("bsT", np.ascontiguousarray(np.asarray(inp["e_a_bs"][0], np.float32).T))
    put("gsubc", _colslab(inp["o_subln_g"][0]))
    put("flag", np.full((128, 1), float(h), np.float32))
    put("nbias", np.full((128, 1), 0.0 if h == 1 else -30000.0, np.float32))
    return cols


def l1_inputs(inp, c):
    b, h = c // 2, c % 2
    x = np.asarray(inp["x"], np.float32)
    p = np.asarray(inp["p"], np.float32)
    sl = slice(h * NT, (h + 1) * NT)
    wup = np.zeros((32, 256), np.float32)
    wup[0:16] = inp["e_b_w_gate_up"][0]
    wup[16] = inp["e_b_b_gate"][0]
    return {
        "xT": np.ascontiguousarray(x[b, sl].T), "xpT": np.ascontiguousarray(x[b, 0:NT].T),
        "p0T": np.ascontiguousarray(p[0, b, sl].T), "cols": make_cols(inp, h),
        "w_in": np.asarray(inp["e_w_in"][0], np.float32),
        "wsT": np.ascontiguousarray(np.asarray(inp["e_a_ws"][0], np.float32).transpose(0, 2, 1)),
        "alng": np.ascontiguousarray(np.broadcast_to(np.asarray(inp["e_a_ln_g"][0], np.float32), (128, 512))),
        "alnb": np.ascontiguousarray(np.broadcast_to(np.asarray(inp["e_a_ln_b"][0], np.float32), (128, 512))),
        "wup": wup, "w_out0": np.asarray(inp["e_w_out"][0], np.float32),
        "ffn_wg": np.asarray(inp["e_ffn_wg"][0], np.float32), "ffn_wu": np.asarray(inp["e_ffn_wu"][0], np.float32),
        "ffn_wd": np.asarray(inp["e_ffn_wd"][0], np.float32),
        "wp0": np.asarray(inp["ple_w_proj"][0], np.float32), "wg0": np.asarray(inp["ple_w_gate"][0], np.float32),
        "w_qkv": np.asarray(inp["o_w_qkv"][0], np.float32),
    }


LAMBDA_INIT = 0.8 - 0.6 * float(np.exp(-0.3 * 1))
CAP = 256


def l2_phases(k, I, cst, cols, banks, reg, s_x1, s_q, s_kT, s_v, outT):
    lam_d, gsub_d, wo_d, wr_d = I["lamv"], I["gsub"], I["w_out1"], I["w_router"]
    ewg, ewu, ewd, p1T, wp1, wg1 = I["exp_wg"], I["exp_wu"], I["exp_wd"], I["p1T"], I["wp1"], I["wg1"]
    s_oT = k.dscr("s_oT", [1024, NT], BF16); s_xl = k.dscr("s_xl", [1024, NT])
    s_xg = k.dscr("s_xg", [8, 1024, 4 * CAP], BF16); s_yg = k.dscr("s_yg", [8, 4 * CAP, 1024], BF16)
    s_PT = k.dscr("s_PT", [4, 128, 8 * 2 * 512], BF16)
    B = [reg(i, 0, 512) for i in range(8)]
    gsl = k.sb("gsl", [128, 8, 8])
    nb0 = COLS["nbias"][0]

    k.phase()
    lamv = k.sb("lamv", [128, 4, 64]); k.dma("sp", lamv[:], lam_d, lamv)
    lt = k.sb("lt", [128, 8]); lp = k.sb("lp", [128, 64])
    for i in range(2):
        k.tt("dve", lp[:], lamv[:, 2 * i, :], lamv[:, 2 * i + 1, :], ALU.mult, [lamv], [lp])
        k.op("dve", lambda e: e.reduce_sum(out=lt[:, i:i + 1], in_=lp[:], axis=AX.X), [lp], [lt])
    k.act(lt[:, 2:4], lt[:, 0:2], AF.Exp, [lt], [lt])
    k.tt("dve", lt[:, 4:5], lt[:, 3:4], lt[:, 2:3], ALU.subtract, [lt], [lt])
    k.ts("dve", lt[:, 5:6], lt[:, 4:5], -LAMBDA_INIT, None, ALU.add, None, [lt], [lt])
    Kt = [k.sb(f"Kt{i}", [128, 2 * NT], BF16) for i in range(2)]
    Vt = [k.sb(f"Vt{i}", [128, 32, 128], BF16) for i in range(2)]
    Qz = [[k.sb(f"Qz{i}_{c}", [128, NT], BF16) for c in range(2)] for i in range(2)]
    for i in range(2):
        for c in range(2):
            k.memset("dve", Qz[i][c][:], 0.0, [Qz[i][c]])
    Et = [k.sb(f"Et{i}", [128, 512], BF16) for i in range(6)]
    rs = [k.sb(f"rs{c}", [128, 512]) for c in range(2)]; Oc = [k.sb(f"Oc{c}", [128, 512]) for c in range(2)]
    t1 = k.sb("t1", [128, 512]); od = k.sb("od", [128, 512])
    osq = k.sb("a_osq", [128, 512], BF16); rstd = k.sb("a_rstd", [128, 512]); onb = k.sb("onb", [128, 512], BF16)
    gc0 = COLS["gsubc"][0]
    k.ts("dve", lt[:, 6:7], cols[:, gc0:gc0 + 1], 1.0 - LAMBDA_INIT, None, ALU.mult, None, [cols], [lt])
    Oa = [B[2], B[3]]; Sm = [B[4], B[5]]
    Bs = B[7]; Sbanks = [B[0], B[1], B[6]]
    for h in range(8):
        Kh, Vh, Qh = Kt[h % 2], Vt[h % 2], Qz[h % 2]
        k.dma("sp", Kh[:], s_kT[2 * h:2 * h + 2].rearrange("c d t -> (c d) t"), Kh, [s_kT])
        k.dma("sp", Vh[:], s_v[:, h * 128:(h + 1) * 128].rearrange("(tt p) f -> p tt f", p=128), Vh, [s_v])
        k.dma("sp", Qh[0][0:64, :], s_q[2 * h], Qh[0], [s_q])
        k.dma("sp", Qh[1][64:128, :], s_q[2 * h + 1], Qh[1], [s_q])
        blocks = [(g, kb, c) for g in range(4) for kb in range(16 + 4 * (g + 1)) for c in range(2)]

        def emit_S(i):
            g, kb, c = blocks[i]
            m = kb - (16 + 4 * g)
            n0 = 128 * m if m > 0 else 0
            Sb = Sbanks[i % 3]
            k.mm(Sb[:, n0:512], Kh[:, kb * 128:(kb + 1) * 128], Qh[c][:, g * 512 + n0:(g + 1) * 512], True, True,
                 [Kh, Qh[c]], [Sb])

        for j in range(2):
            emit_S(j)
        for i, (g, kb, c) in enumerate(blocks):
            if i + 2 < len(blocks):
                emit_S(i + 2)
            m = kb - (16 + 4 * g)
            n0 = 128 * m if m > 0 else 0
            last = 16 + 4 * g + 3
            Sb = Sbanks[i % 3]; E = Et[i % 6]
            if kb < 16:
                k.act(E[:, n0:512], Sb[:, n0:512], AF.Exp, [Sb, cols], [E], bias=cols[:, nb0:nb0 + 1], scale=0.125)
            else:
                k.act(E[:, n0:512], Sb[:, n0:512], AF.Exp, [Sb], [E], scale=0.125)
            if m >= 0:
                k.tt("dve", E[:, n0:n0 + 128], E[:, n0:n0 + 128], cst["trib"][:], ALU.mult, [E, cst["trib"]], [E])
            k.mm(Oa[c][:, n0:512], Vh[:, kb, :], E[:, n0:512], kb == 0, kb == last, [Vh, E], [Oa[c]])
            k.mm(Sm[c][:, n0:512], cst["ones"][:], E[:, n0:512], kb == 0, kb == last, [cst["ones"], E], [Sm[c]])
            if kb == last and c == 1:
                for cc in range(2):
                    k.op("dve", lambda e: e.reciprocal(out=rs[cc][:], in_=Sm[cc][:]), [Sm[cc]], [rs[cc]])
                    k.copy("act", Oc[cc][:], Oa[cc][:], [Oa[cc]], [Oc[cc]])
                k.tt("pool", t1[:], Oc[0][:], rs[0][:], ALU.mult, [Oc[0], rs[0]], [t1])
                k.tt("dve", od[:], Oc[1][:], rs[1][:], ALU.mult, [Oc[1], rs[1]], [od])
                k.stt("dve", od[:], od[:], lt[:, 5:6], t1[:], ALU.mult, ALU.add, [od, lt, t1], [od])
                k.tt("pool", osq[:], od[:], od[:], ALU.mult, [od], [osq])
                k.mm(Bs[:], cst["ones"][:], osq[:], True, True, [cst["ones"], osq], [Bs])
                k.rsqrt(rstd[:], Bs[:], 1.0 / 128, EPS, [Bs], [rstd])
                k.stt("dve", onb[:], od[:], lt[:, 6:7], rstd[:], ALU.mult, ALU.mult, [od, lt, rstd], [onb])
                k.dma("sp", s_oT[h * 128:(h + 1) * 128, g * 512:(g + 1) * 512], onb[:], s_oT, [onb])
    k.end_phase()

    k.phase()
    wo = k.sb("wo", [128, 8, 1024], BF16); load_w(k, "pool", wo, wo_d, 0, 1024, 0, 1024, 8)
    wr = k.sb("wr", [128, 8, 8]); k.dma("sp", wr[:], wr_d.rearrange("(kc p) e -> p kc e", p=128), wr)
    oTs = k.sb("oTs", [128, 8, 512], BF16); x32 = k.sb("r_x32", [128, 8, 512]); xbf = k.sb("r_xbf", [128, 8, 512], BF16)
    tmp = dict(ybf=k.sb("ybf", [128, 8, 512], BF16), ysq=k.sb("ysq", [128, 8, 512], BF16), mean=k.sb("mean", [128, 512]),
               rstd=k.sb("rstd", [128, 512]), msq=k.sb("msq", [128, 512]))
    xtok = k.sb("xtok", [128, 4, 1024], BF16)
    lg = k.sb("lg", [128, 4, 8]); m8 = k.sb("m8", [128, 4, 8]); sel = k.sb("sel", [128, 4, 8]); is1 = k.sb("is1", [128, 4, 8])
    selb = k.sb("selb", [128, 32], BF16); gw = k.sb("gw", [128, 4, 8]); gq = k.sb("gq", [128, 8])
    off = k.sb("off", [128, 4, 8]); d1 = k.sb("d1", [128, 4, 8]); ghl = k.sb("ghl", [128, 4, 8, 2], BF16)
    ghi32 = k.sb("ghi32", [128, 4, 8]); g2t = k.sb("g2t", [128, 2])
    io_i = k.sb("io_i", [128, CAP], I32); io1 = k.sb("io1", [128, CAP])
    k.op("pool", lambda e: e.iota(io_i[:], pattern=[[1, CAP]], base=1, channel_multiplier=0), (), [io_i])
    k.copy("dve", io1[:], io_i[:], [io_i], [io1])
    tris_bf = k.sb("tris_bf", [128, 128], BF16)
    k.tt("dve", tris_bf[:], cst["tri"][:], cst["idf"][:], ALU.subtract, [cst["tri"], cst["idf"]], [tris_bf])
    P = k.sb("P", [128, 4, 8, CAP], BF16)
    xg = k.sb("xg", [128, 8, CAP], BF16); PTs = k.sb("PTs", [128, 8, 2, 512], BF16)
    B7bf = T(banks[7][:, :].bitcast(BF16), B[7].b, excl=True)
    B6bf = T(banks[6][:, :].bitcast(BF16), B[6].b, excl=True)
    for g in range(4):
        gs = slice(g * 512, (g + 1) * 512)
        k.dma("sp", oTs[:], s_oT[:, gs].rearrange("(kc p) t -> p kc t", p=128), oTs, [s_oT])
        k.dma("sp", x32[:], s_x1[:, gs].rearrange("(kc p) t -> p kc t", p=128), x32, [s_x1])
        for nb in range(8):
            pb = B[nb % 2]
            for ch in range(8):
                k.mm(pb[:], wo[:, ch, nb * 128:(nb + 1) * 128], oTs[:, ch, :], ch == 0, ch == 7, [wo, oTs], [pb])
            k.stt("dve", x32[:, nb, :], x32[:, nb, :], ALPHA, pb[:], ALU.mult, ALU.add, [x32, pb], [x32])
        ln_fm(k, cst, x32, B[0], B[1], "o_ln1_g", "o_ln1_b", cols, x32, xbf, 512, tmp)
        k.dma("sp", s_xl[:, gs].rearrange("(kc p) t -> p kc t", p=128), x32[:], s_xl, [x32])
        for tt in range(4):
            cs = slice(tt * 128, (tt + 1) * 128)
            for half in range(2):
                pbf = B6bf if half == 0 else B7bf
                for i in range(4):
                    ch = half * 4 + i
                    k.op("pe", lambda e: e.transpose(pbf[:, i * 128:(i + 1) * 128], xbf[:, ch, cs], cst["ident"][:]),
                         [xbf, cst["ident"]], [pbf])
                k.copy("act" if half else "dve", xtok[:, tt, half * 512:(half + 1) * 512], pbf[:, 0:512], [pbf], [xtok])
            for ch in range(8):
                k.mm(B[2][:, tt * 8:(tt + 1) * 8], x32[:, ch, cs], wr[:, ch, :], ch == 0, ch == 7, [x32, wr], [B[2]])
        k.copy("dve", lg[:].rearrange("p a b -> p (a b)"), B[2][:, 0:32], [B[2]], [lg])
        for tt in range(4):
            k.op("dve", lambda e: e.max(out=m8[:, tt, :], in_=lg[:, tt, :]), [lg], [m8])
            k.ts("dve", sel[:, tt, :], lg[:, tt, :], m8[:, tt, 1:2], None, ALU.is_ge, None, [lg, m8], [sel])
            k.ts("dve", is1[:, tt, :], lg[:, tt, :], m8[:, tt, 0:1], None, ALU.is_ge, None, [lg, m8], [is1])
        for tt in range(4):
            k.tt("dve", gq[:, tt:tt + 1], m8[:, tt, 1:2], m8[:, tt, 0:1], ALU.subtract, [m8], [gq])
        k.act(gq[:, 0:4], gq[:, 0:4], AF.Exp, [gq], [gq])
        k.ts("dve", gq[:, 4:8], gq[:, 0:4], 1.0, None, ALU.add, None, [gq], [gq])
        k.op("dve", lambda e: e.reciprocal(out=gq[:, 4:8], in_=gq[:, 4:8]), [gq], [gq])
        k.tt("dve", gq[:, 0:4], gq[:, 0:4], gq[:, 4:8], ALU.mult, [gq], [gq])
        for tt in range(4):
            k.ts("dve", gw[:, tt, :], sel[:, tt, :], gq[:, tt:tt + 1], None, ALU.mult, None, [sel, gq], [gw])
            k.tt("dve", off[:, 0, 0:1], gq[:, 4 + tt:5 + tt], gq[:, tt:tt + 1], ALU.subtract, [gq], [off])
            k.stt("dve", gw[:, tt, :], is1[:, tt, :], off[:, 0, 0:1], gw[:, tt, :], ALU.mult, ALU.add, [is1, off, gw], [gw])
        k.copy("dve", ghl[:, :, :, 0], gw[:], [gw], [ghl])
        k.copy("dve", ghi32[:], ghl[:, :, :, 0], [ghl], [ghi32])
        k.tt("dve", ghi32[:], gw[:], ghi32[:], ALU.subtract, [gw, ghi32], [ghi32])
        k.copy("dve", ghl[:, :, :, 1], ghi32[:], [ghi32], [ghl])
        k.copy("dve", selb[:], sel[:].rearrange("p a b -> p (a b)"), [sel], [selb])
        k.mm(B[2][:, 0:32], tris_bf[:], selb[:], True, True, [tris_bf, selb], [B[2]])
        k.mm(B[3][:, 0:32], cst["ones"][:], selb[:], True, True, [cst["ones"], selb], [B[3]])
        k.memset("dve", off[:, 0, :], 0.0, [off])
        for tt in range(1, 4):
            k.tt("dve", off[:, tt, :], off[:, tt - 1, :], B[3][:, (tt - 1) * 8:tt * 8], ALU.add, [off, B[3]], [off])
        k.tt("dve", d1[:].rearrange("p a b -> p (a b)"), off[:].rearrange("p a b -> p (a b)"), B[2][:, 0:32], ALU.add,
             [off, B[2]], [d1])
        k.stt("dve", d1[:], d1[:], 1.0, sel[:], ALU.add, ALU.mult, [d1, sel], [d1])
        for tt in range(4):
            for e in range(8):
                k.ts("dve", P[:, tt, e, :], io1[:], d1[:, tt, e:e + 1], None, ALU.is_equal, None, [io1, d1], [P])
        for e in range(8):
            for ch in range(8):
                pb = B[ch % 2]
                for tt in range(4):
                    k.mm(pb[:, 0:CAP], xtok[:, tt, ch * 128:(ch + 1) * 128], P[:, tt, e, :], tt == 0, tt == 3, [xtok, P], [pb])
                k.copy("act" if ch % 2 else "dve", xg[:, ch, :], pb[:, 0:CAP], [pb], [xg])
            k.dma("sp", s_xg[e, :, g * CAP:(g + 1) * CAP].rearrange("(kc p) s -> p kc s", p=128), xg[:], s_xg, [xg])
            for st in range(2):
                pbf = B6bf if st == 0 else B7bf
                for tt in range(4):
                    k.op("pe", lambda e_: e_.transpose(pbf[:, tt * 128:(tt + 1) * 128], P[:, tt, e, st * 128:(st + 1) * 128],
                                                       cst["ident"][:]), [P, cst["ident"]], [pbf])
                k.copy("act" if st else "dve", PTs[:, e, st, :], pbf[:, 0:512], [pbf], [PTs])
                for tt in range(4):
                    k.mm(B[4][:, 0:2], P[:, tt, e, st * 128:(st + 1) * 128], ghl[:, tt, e, :], tt == 0, tt == 3, [P, ghl], [B[4]])
                k.copy("dve", g2t[:], B[4][:, 0:2], [B[4]], [g2t])
                k.tt("dve", gsl[:, e, g * 2 + st:g * 2 + st + 1], g2t[:, 0:1], g2t[:, 1:2], ALU.add, [g2t], [gsl])
        k.dma("sp", s_PT[g].rearrange("p (a b c) -> p a b c", a=8, b=2), PTs[:], s_PT, [PTs])
    k.end_phase()
    expert_phase(k, B, s_xg, s_yg, ewg, ewu, ewd, gsl)
    combine_phase(k, cst, cols, B, s_yg, s_PT, s_xl, outT, wp1, wg1, p1T)
    k.em.wait_all("sp", _bufs([outT]))


def build_fused(k):
    names = [("xT", [1024, NT]), ("xpT", [1024, NT]), ("p0T", [256, NT]), ("p0pT", [256, NT]), ("p1T", [256, NT]),
             ("cols", [128, NCOLS]), ("w_in", [1024, 2576]), ("wsT", [8, 128, 128]), ("alng", [128, 512]),
             ("alnb", [128, 512]), ("wup", [32, 256]), ("w_out0", [1024, 1024]), ("ffn_wg", [1024, 2816]),
             ("ffn_wu", [1024, 2816]), ("ffn_wd", [2816, 1024]), ("wp0", [256, 1024]), ("wg0", [1024, 1024]),
             ("w_qkv", [1024, 3072]), ("lamv", [128, 4, 64]), ("gsub", [128, 128]), ("w_out1", [1024, 1024]),
             ("w_router", [1024, 8]), ("exp_wg", [8, 1024, 3584]), ("exp_wu", [8, 1024, 3584]),
             ("exp_wd", [8, 3584, 1024]), ("wp1", [256, 1024]), ("wg1", [1024, 1024])]
    I = {n: k.din(n, shp) for n, shp in names}
    outT = k.dout("outT", [1024, NT])
    s_xln1 = k.dscr("s_xln1", [1024, NT]); s_x1p = k.dscr("s_x1p", [1024, NT]); s_x1 = k.dscr("s_x1", [1024, NT])
    s_q = k.dscr("s_q", [16, 64, NT], BF16); s_kT = k.dscr("s_kT", [16, 64, 2 * NT], BF16)
    s_v = k.dscr("s_v", [2 * NT, 1024], BF16)
    cst = make_consts(k)
    banks, reg = psum_banks(k)
    cols = k.sb("cols", [128, NCOLS]); k.dma("sp", cols[:], I["cols"], cols)
    B = [reg(i, 0, 512) for i in range(8)]
    for (src, psrc, pref, x1dst, qdst, koff) in ((I["xpT"], I["p0pT"], None, s_x1p, None, 0),
                                                 (I["xT"], I["p0T"], I["xpT"], s_x1, s_q, NT)):
        mixer_phase(k, I, cst, cols, banks, reg, src, pref, s_xln1)
        ffn_phase(k, cst, cols, B[0], B[1], B[2], B[3], s_xln1, x1dst, I["ffn_wg"], I["ffn_wu"], I["ffn_wd"], 22,
                  "e_ln2_g", "e_ln2_b", I["wp0"], I["wg0"], psrc, "ple_b0")
        qkv_phase(k, B, x1dst, I["w_qkv"], qdst, s_kT, s_v, koff)
    l2_phases(k, I, cst, cols, banks, reg, s_x1, s_q, s_kT, s_v, outT)


def expert_phase(k, B, s_xg, s_yg, ewg, ewu, ewd, gsl):
    k.phase()
    NS = 4 * CAP
    NJ = 28
    xg = [k.sb(f"e_xg{i}", [128, 8, NS], BF16) for i in range(2)]
    hT = k.sb("e_hT", [128, NJ, NS], BF16)
    wgt = [k.sb(f"e_wg{i}", [128, 8, 256], BF16) for i in range(4)]
    wut = [k.sb(f"e_wu{i}", [128, 8, 256], BF16) for i in range(4)]
    wdt = [k.sb(f"e_wd{i}", [128, NJ, 256], BF16) for i in range(2)]
    sg = [k.sb(f"e_sg{i}", [128, 512], BF16) for i in range(2)]
    yg = k.sb("e_yg", [128, NS // 128, 1024], BF16)
    n = 0
    for e in range(8):
        xe = xg[e % 2]
        k.dma("sp", xe[:], s_xg[e].rearrange("(kc p) s -> p kc s", p=128), xe, [s_xg])
        for jb in range(NJ // 2):
            a, b = wgt[jb % 4], wut[jb % 4]
            load_w(k, "pool", a, ewg[e], 0, 1024, jb * 256, jb * 256 + 256, 8)
            load_w(k, "pool", b, ewu[e], 0, 1024, jb * 256, jb * 256 + 256, 8)
            for jj in range(2):
                j = jb * 2 + jj
                for tg in range(NS // 512):
                    ts_ = slice(tg * 512, (tg + 1) * 512)
                    pg, pu = (B[0], B[1]) if n % 2 == 0 else (B[2], B[3])
                    s_ = sg[n % 2]; n += 1
                    for kc in range(8):
                        k.mm(pg[:], a[:, kc, jj * 128:(jj + 1) * 128], xe[:, kc, ts_], kc == 0, kc == 7, [a, xe], [pg])
                    for kc in range(8):
                        k.mm(pu[:], b[:, kc, jj * 128:(jj + 1) * 128], xe[:, kc, ts_], kc == 0, kc == 7, [b, xe], [pu])
                    k.act(s_[:], pg[:], AF.Silu, [pg], [s_])
                    k.tt("dve", hT[:, j, ts_], pu[:], s_[:], ALU.mult, [pu, s_], [hT])
        for nbb in range(4):
            wd_ = wdt[nbb % 2]
            k.dma("pool", wd_[:], ewd[e][:, nbb * 256:(nbb + 1) * 256].rearrange("(j p) n -> p j n", p=128), wd_)
            for st in range(NS // 128):
                pb = B[4 + (st % 4)]
                for j in range(NJ):
                    k.mm(pb[:, 0:256], hT[:, j, st * 128:(st + 1) * 128], wd_[:, j, :], j == 0, j == NJ - 1, [hT, wd_], [pb])
                k.ts("dve", yg[:, st, nbb * 256:(nbb + 1) * 256], pb[:, 0:256], gsl[:, e, st:st + 1], None, ALU.mult, None,
                     [pb, gsl], [yg])
        k.dma("sp", s_yg[e].rearrange("(st p) f -> p st f", p=128), yg[:], s_yg, [yg])
    k.end_phase()


def combine_phase(k, cst, cols, B, s_yg, s_PT, s_xl, outT, wp_d, wgate_d, pT_d):
    k.phase()
    ygl = k.sb("c_ygl", [128, 8, 2, 1024], BF16); PT = k.sb("c_PT", [128, 8, 2, 512], BF16)
    x32 = k.sb("c_x32", [128, 8, 512]); x2bf = k.sb("c_x2bf", [128, 8, 512], BF16)
    wgate = k.sb("c_wgate", [128, 8, 1024], BF16); wproj = k.sb("c_wproj", [128, 2, 1024], BF16)
    load_w(k, "pool", wgate, wgate_d, 0, 1024, 0, 1024, 8)
    load_w(k, "pool", wproj, wp_d, 0, 256, 0, 1024, 2)
    pTb = k.sb("c_pTb", [128, 2, 512], BF16); gt = k.sb("c_gt", [128, 512])
    tmp = dict(ybf=k.sb("ybf", [128, 8, 512], BF16), ysq=k.sb("ysq", [128, 8, 512], BF16), mean=k.sb("mean", [128, 512]),
               rstd=k.sb("rstd", [128, 512]), msq=k.sb("msq", [128, 512]))
    pb0 = COLS["ple_b1"][0]
    for g in range(4):
        gs = slice(g * 512, (g + 1) * 512)
        for e in range(8):
            k.dma("sp", ygl[:, e, :, :], s_yg[e, g * CAP:(g + 1) * CAP, :].rearrange("(st p) f -> p st f", p=128), ygl, [s_yg])
        k.dma("sp", PT[:], s_PT[g].rearrange("p (a b c) -> p a b c", a=8, b=2), PT, [s_PT])
        k.dma("sp", x32[:], s_xl[:, gs].rearrange("(kc p) t -> p kc t", p=128), x32, [s_xl])
        for nb in range(8):
            pb = B[nb % 2]
            i = 0
            for e in range(8):
                for st in range(2):
                    k.mm(pb[:], ygl[:, e, st, nb * 128:(nb + 1) * 128], PT[:, e, st, :], i == 0, i == 15, [ygl, PT], [pb])
                    i += 1
            k.stt("dve", x32[:, nb, :], x32[:, nb, :], ALPHA, pb[:], ALU.mult, ALU.add, [x32, pb], [x32])
        ln_fm(k, cst, x32, B[0], B[1], "o_ln2_g", "o_ln2_b", cols, x32, x2bf, 512, tmp)
        k.dma("pool", pTb[:], pT_d[:, gs].rearrange("(kc p) t -> p kc t", p=128), pTb)
        for nb in range(8):
            ns = slice(nb * 128, (nb + 1) * 128)
            pg, pp = (B[0], B[1]) if nb % 2 == 0 else (B[2], B[3])
            for kc in range(8):
                k.mm(pg[:], wgate[:, kc, ns], x2bf[:, kc, :], kc == 0, kc == 7, [wgate, x2bf], [pg])
            for kc in range(2):
                k.mm(pp[:], wproj[:, kc, ns], pTb[:, kc, :], kc == 0, kc == 1, [wproj, pTb], [pp])
            k.act(gt[:], pg[:], AF.Sigmoid, [pg, cols], [gt], bias=cols[:, pb0 + nb:pb0 + nb + 1], scale=1.0)
            k.tt("dve", gt[:], gt[:], pp[:], ALU.mult, [gt, pp], [gt])
            k.tt("dve", x32[:, nb, :], x32[:, nb, :], gt[:], ALU.add, [x32, gt], [x32])
        k.dma("sp", outT[:, gs].rearrange("(kc p) t -> p kc t", p=128), x32[:], outT, [x32])
    k.end_phase()


def fused_inputs(inp, c):
    b, h = c // 2, c % 2
    d = l1_inputs(inp, c)
    p = np.asarray(inp["p"], np.float32)
    sl = slice(h * NT, (h + 1) * NT)
    lamv = np.stack([inp["o_lam_q1"][0], inp["o_lam_k1"][0], inp["o_lam_q2"][0], inp["o_lam_k2"][0]]).astype(np.float32)
    d.update({
        "p0pT": np.ascontiguousarray(p[0, b, 0:NT].T),
        "p1T": np.ascontiguousarray(p[1, b, sl].T),
        "lamv": np.ascontiguousarray(np.broadcast_to(lamv, (128, 4, 64))),
        "gsub": np.ascontiguousarray(np.broadcast_to(np.asarray(inp["o_subln_g"][0], np.float32), (128, 128))),
        "w_out1": np.asarray(inp["o_w_out"][0], np.float32), "w_router": np.asarray(inp["o_router"][0], np.float32),
        "exp_wg": np.asarray(inp["o_exp_wg"][0], np.float32), "exp_wu": np.asarray(inp["o_exp_wu"][0], np.float32),
        "exp_wd": np.asarray(inp["o_exp_wd"][0], np.float32),
        "wp1": np.asarray(inp["ple_w_proj"][1], np.float32), "wg1": np.asarray(inp["ple_w_gate"][1], np.float32),
    })
    return d


_CACHE = {}


def kernel(**inputs):
    inp = {k_: np.asarray(v) for k_, v in inputs.items()}
    if "k" not in _CACHE:
        kb = KB(); build_fused(kb); _CACHE["k"] = kb
    kb = _CACHE["k"]
    res = run_bass_kernel_spmd(kb.nc, [fused_inputs(inp, c) for c in range(8)], core_ids=list(range(8))).results
    out = np.empty((4, 2 * NT, 1024), np.float32)
    for c in range(8):
        b, h = c // 2, c % 2
        out[b, h * NT:(h + 1) * NT] = res[c]["outT"].T
    return out
```

```python
import numpy as np
import ml_dtypes
import concourse.bass as bass
import concourse.mybir as mybir
from concourse.bass_utils import run_bass_kernel_spmd

AF = mybir.ActivationFunctionType
ALU = mybir.AluOpType
AX = mybir.AxisListType
F32 = mybir.dt.float32
BF16 = mybir.dt.bfloat16
I32 = mybir.dt.int32


class Buf:
    __slots__ = ("name", "w", "r", "dsem", "dcount", "genw", "uid", "swq")
    _n = [0]

    def __init__(self, name):
        self.name = name
        self.w = None
        self.r = {}
        self.dsem = None
        self.dcount = 0
        self.genw = None
        Buf._n[0] += 1
        self.uid = Buf._n[0]
        self.swq = False


class Emitter:
    def __init__(self, nc):
        self.nc = nc
        self.eng = {"pe": nc.tensor, "dve": nc.vector, "act": nc.scalar, "pool": nc.gpsimd, "sp": nc.sync}
        self.ep = {e: 0 for e in self.eng}
        self.free_dsems = []
        self.sem = {e: nc.alloc_semaphore("sem_" + e) for e in self.eng}
        self.cnt = {e: 0 for e in self.eng}
        self.seen = {e: {} for e in self.eng}
        self.semobj = {}
        for e, s in self.sem.items():
            self.semobj[("e", e, 0)] = s
        self.nbuf = 0
        self.dbufs = []

    def buf(self, name=None):
        self.nbuf += 1
        return Buf(name or f"b{self.nbuf}")

    def _need(self, e, tok, out):
        if tok is None:
            return
        key, val = tok
        if key[0] == "e" and key[2] < self.ep[key[1]]:
            return
        if key == ("e", e, self.ep[e]):
            if e == "pe":
                return
        if self.seen[e].get(key, 0) >= val:
            return
        if out.get(key, 0) < val:
            out[key] = val

    def _collect(self, e, reads, writes):
        need = {}
        for b in reads:
            self._need(e, b.w, need)
        for b in writes:
            self._need(e, b.w, need)
            for k, v in b.r.items():
                self._need(e, (k, v), need)
        return need

    def _dowaits(self, e, need):
        for key, val in need.items():
            self.eng[e].wait_ge(self.semobj[key], val)
            self.seen[e][key] = val

    def op(self, e, fn, reads=(), writes=()):
        need = self._collect(e, reads, writes)
        self._dowaits(e, need)
        ins = fn(self.eng[e])
        ins.then_inc(self.sem[e], 1)
        self.cnt[e] += 1
        tok = (("e", e, self.ep[e]), self.cnt[e])
        for b in writes:
            b.w = tok
            b.r = {}
            b.genw = None
        for b in reads:
            if b.r.get(tok[0], 0) < tok[1]:
                b.r[tok[0]] = tok[1]
        return tok

    def dma(self, q, out, in_, dst, srcs=(), **kw):
        if dst.dsem is None:
            if self.free_dsems and q != "pool":
                dst.dsem, dst.dcount = self.free_dsems.pop()
            else:
                self.nds = getattr(self, "nds", 0) + 1
                dst.dsem = self.nc.alloc_semaphore(f"dsem_{self.nds}")
            self.semobj[("d", dst.uid)] = dst.dsem
            self.dbufs.append(dst)
        if q == "pool":
            dst.swq = True
        dkey = ("d", dst.uid)
        same_gen = dst.w is not None and dst.w[0] == dkey and not dst.r and dst.genw is not None
        need = {}
        for b in srcs:
            self._need(q, b.w, need)
        if same_gen:
            for key, val in dst.genw.items():
                self._need(q, (key, val), need)
        else:
            gen = {}
            if dst.w is not None:
                gen[dst.w[0]] = dst.w[1]
            for k, v in dst.r.items():
                gen[k] = max(gen.get(k, 0), v)
            for key, val in gen.items():
                self._need(q, (key, val), need)
            dst.genw = gen
        self._dowaits(q, need)
        ins = self.eng[q].dma_start(out=out, in_=in_, **kw)
        dst.dcount += 16
        ins.then_inc(dst.dsem, 16)
        tok = (dkey, dst.dcount)
        dst.w = tok
        dst.r = {}
        for b in srcs:
            if b.r.get(dkey, 0) < dst.dcount:
                b.r[dkey] = dst.dcount
        return tok

    def barrier(self):
        for e in self.eng:
            need = {}
            for f in self.eng:
                if f != e and self.cnt[f] > 0:
                    self._need(e, (("e", f, self.ep[f]), self.cnt[f]), need)
            for b in self.dbufs:
                self._need(e, (("d", b.uid), b.dcount), need)
            self._dowaits(e, need)
        for e in self.eng:
            if self.cnt[e] > 12000:
                self.ep[e] += 1
                self.sem[e] = self.nc.alloc_semaphore(f"sem_{e}_{self.ep[e]}")
                self.semobj[("e", e, self.ep[e])] = self.sem[e]
                self.cnt[e] = 0

    def retire(self, bufs):
        for b in bufs:
            if b.dsem is not None:
                if not b.swq:
                    self.free_dsems.append((b.dsem, b.dcount))
                self.dbufs = [x for x in self.dbufs if x is not b]
                b.dsem = None

    def wait_all(self, e, bufs):
        need = {}
        for b in bufs:
            self._need(e, b.w, need)
        self._dowaits(e, need)


class T:
    def __init__(self, h, b, excl=False):
        self.h = h
        self.b = b
        self.excl = excl

    def __getitem__(self, idx):
        return self.h[idx]


def _bufs(xs):
    return [getattr(x, "b", x) for x in xs]


class KB:
    def __init__(self):
        import contextlib
        self.nc = bass.Bass("TRN2", target_bir_lowering=False)
        self.em = Emitter(self.nc)
        self.n = 0
        self.gstack = contextlib.ExitStack()
        self.stack = self.gstack

    def phase(self):
        import contextlib
        self.stack = contextlib.ExitStack()
        self.pbufs = []
        return self.stack

    def end_phase(self):
        self.em.barrier()
        self.em.retire(self.pbufs)
        self.pbufs = []
        self.stack.close()
        self.stack = self.gstack

    def din(self, name, shape, dt=F32):
        return self.nc.dram_tensor(name, list(shape), dt, kind="ExternalInput").ap()

    def dout(self, name, shape, dt=F32):
        return T(self.nc.dram_tensor(name, list(shape), dt, kind="ExternalOutput").ap(), self.em.buf(name))

    def dscr(self, name, shape, dt=F32):
        return T(self.nc.dram_tensor(name, list(shape), dt, kind="Internal").ap(), self.em.buf(name))

    def sb(self, name, shape, dt=F32):
        self.n += 1
        h = self.stack.enter_context(self.nc.sbuf_tensor(f"{name}_{self.n}", list(shape), dt))
        t = T(h, self.em.buf(name))
        if self.stack is not self.gstack:
            self.pbufs.append(t.b)
        return t

    def psum(self, name, shape, dt=F32):
        return self.gstack.enter_context(self.nc.psum_tensor(name, list(shape), dt))

    def view(self, ap, name="v"):
        return T(ap, self.em.buf(name))

    def op(self, e, fn, reads=(), writes=()):
        r, w = [], _bufs(writes)
        for x in reads:
            if getattr(x, "excl", False):
                w.append(x.b)
            else:
                r.append(getattr(x, "b", x))
        return self.em.op(e, fn, r, w)

    def dma(self, q, out, in_, dst, srcs=(), **kw):
        return self.em.dma(q, out, in_, getattr(dst, "b", dst), _bufs(srcs), **kw)

    def mm(self, out, lhsT, rhs, start, stop, reads, writes, skip=False):
        if skip:
            self.op("pe", lambda e: e.matmul(out, lhsT, rhs, start=start, stop=stop, skip_group_check=True), reads, writes)
        else:
            self.op("pe", lambda e: e.matmul(out, lhsT, rhs, start=start, stop=stop), reads, writes)

    def act(self, out, in_, func, reads, writes, bias=None, scale=None, eng="act"):
        kw = {}
        if bias is not None:
            kw["bias"] = bias
        if scale is not None:
            kw["scale"] = scale
        self.op("act", lambda e: e.activation(out=out, in_=in_, func=func, **kw), reads, writes)

    def tt(self, e, out, a, b, op, reads, writes):
        self.op(e, lambda g: g.tensor_tensor(out=out, in0=a, in1=b, op=op), reads, writes)

    def ts(self, e, out, a, s1, s2, op0, op1, reads, writes):
        if s2 is None:
            self.op(e, lambda g: g.tensor_scalar(out=out, in0=a, scalar1=s1, scalar2=None, op0=op0), reads, writes)
        else:
            self.op(e, lambda g: g.tensor_scalar(out=out, in0=a, scalar1=s1, scalar2=s2, op0=op0, op1=op1), reads, writes)

    def stt(self, e, out, a, s, b, op0, op1, reads, writes):
        self.op(e, lambda g: g.scalar_tensor_tensor(out=out, in0=a, scalar=s, in1=b, op0=op0, op1=op1), reads, writes)

    def copy(self, e, out, in_, reads, writes):
        if e == "act":
            self.op(e, lambda g: g.copy(out=out, in_=in_), reads, writes)
        else:
            self.op(e, lambda g: g.tensor_copy(out=out, in_=in_), reads, writes)

    def rsqrt(self, out, in_, scale, eps, reads, writes):
        self.op("act", lambda e: e.activation(out=out, in_=in_, func=AF.Sqrt, bias=eps, scale=scale), reads, writes)
        self.op("dve", lambda e: e.reciprocal(out=out, in_=out), writes, writes)

    def memset(self, e, ap, val, writes):
        self.op(e, lambda g: g.memset(ap, val), (), writes)


ALPHA = (2.0 * 2) ** 0.25
EPS = 1e-5
NT = 2048

COLS = {}
_c = 0
for _nm, _n in [("e_ln1_g", 8), ("e_ln1_b", 8), ("e_ln2_g", 8), ("e_ln2_b", 8), ("o_ln1_g", 8), ("o_ln1_b", 8),
                ("o_ln2_g", 8), ("o_ln2_b", 8), ("ple_b0", 8), ("ple_b1", 8), ("b_norm_g", 1), ("bsT", 8),
                ("flag", 1), ("nbias", 1), ("gsubc", 1)]:
    COLS[_nm] = (_c, _n)
    _c += _n
NCOLS = _c


def make_consts(k):
    c = {}
    it = k.sb("iota_i", [128, 128], I32)
    k.op("pool", lambda g: g.iota(it[:], pattern=[[1, 128]], base=0, channel_multiplier=-1), (), [it])
    d = k.sb("iota_f", [128, 128], F32)
    k.copy("dve", d[:], it[:], [it], [d])
    tri = k.sb("tri", [128, 128], F32)
    k.ts("dve", tri[:], d[:], 0.0, None, ALU.is_ge, None, [d], [tri])
    idf = k.sb("idf", [128, 128], F32)
    k.ts("dve", idf[:], d[:], 0.0, None, ALU.is_equal, None, [d], [idf])
    ident = k.sb("ident", [128, 128], BF16)
    k.copy("dve", ident[:], idf[:], [idf], [ident])
    trib = k.sb("trib", [128, 128], BF16)
    k.copy("dve", trib[:], tri[:], [tri], [trib])
    triblk = k.sb("triblk", [128, 128], F32)
    k.copy("dve", triblk[:], tri[:], [tri], [triblk])
    k.memset("dve", triblk[0:64, 64:128], 0.0, [triblk])
    onesblk = k.sb("onesblk", [128, 128], F32)
    k.memset("dve", onesblk[:], 0.0, [onesblk])
    k.memset("dve", onesblk[0:64, 0:64], 1.0, [onesblk])
    k.memset("dve", onesblk[64:128, 64:128], 1.0, [onesblk])
    tris = k.sb("tris", [128, 128], F32)
    k.ts("dve", tris[:], triblk[:], -1.0 / 16, None, ALU.mult, None, [triblk], [tris])
    dmat = k.sb("dmat", [128, 128], F32)
    k.tt("dve", dmat[:], onesblk[:], triblk[:], ALU.subtract, [onesblk, triblk], [dmat])
    k.ts("dve", dmat[:], dmat[:], -1.0 / 16, None, ALU.mult, None, [dmat], [dmat])
    triblkb = k.sb("triblkb", [128, 128], BF16)
    k.copy("dve", triblkb[:], triblk[:], [triblk], [triblkb])
    ones = k.sb("ones", [128, 128], BF16)
    k.memset("dve", ones[:], 1.0, [ones])
    rm0 = k.sb("rm0", [128, 1], F32)
    rm1 = k.sb("rm1", [128, 1], F32)
    k.memset("dve", rm0[:], 0.0, [rm0]); k.memset("dve", rm0[0:64, :], 1.0, [rm0])
    k.memset("dve", rm1[:], 0.0, [rm1]); k.memset("dve", rm1[64:128, :], 1.0, [rm1])
    c.update(tri=tri, ident=ident, trib=trib, triblk=triblk, tris=tris, dmat=dmat, triblkb=triblkb, ones=ones,
             rm0=rm0, rm1=rm1, idf=idf)
    return c


def ln_fm(k, cst, y, ps1, ps2, gcol, bcol, cols, o32, obf, W, tmp):
    ybf, ysq, mean, rstd = tmp["ybf"], tmp["ysq"], tmp["mean"], tmp["rstd"]
    for ch in range(8):
        k.copy("dve", ybf[:, ch, :], y[:, ch, :], [y], [ybf])
        k.act(ysq[:, ch, :], y[:, ch, :], AF.Square, [y], [ysq])
    for ch in range(8):
        k.mm(ps1[:, 0:W], cst["ones"][:], ybf[:, ch, :], ch == 0, ch == 7, [ybf, cst["ones"]], [ps1])
    for ch in range(8):
        k.mm(ps2[:, 0:W], cst["ones"][:], ysq[:, ch, :], ch == 0, ch == 7, [ysq, cst["ones"]], [ps2])
    k.ts("dve", mean[:], ps1[:, 0:W], 1.0 / 1024, None, ALU.mult, None, [ps1], [mean])
    msq = tmp["msq"]
    k.tt("dve", msq[:], mean[:], mean[:], ALU.mult, [mean], [msq])
    k.stt("dve", rstd[:], ps2[:, 0:W], 1.0 / 1024, msq[:], ALU.mult, ALU.subtract, [ps2, msq], [rstd])
    k.rsqrt(rstd[:], rstd[:], 1.0, EPS, [rstd], [rstd])
    g0 = COLS[gcol][0]
    b0 = COLS[bcol][0]
    for ch in range(8):
        e = "dve"
        k.tt(e, o32[:, ch, :], y[:, ch, :], mean[:], ALU.subtract, [y, mean], [o32])
        k.tt(e, o32[:, ch, :], o32[:, ch, :], rstd[:], ALU.mult, [o32, rstd], [o32])
        k.ts("dve", o32[:, ch, :], o32[:, ch, :], cols[:, g0 + ch:g0 + ch + 1], cols[:, b0 + ch:b0 + ch + 1],
             ALU.mult, ALU.add, [o32, cols], [o32])
        if obf is not None:
            k.copy("act", obf[:, ch, :], o32[:, ch, :], [o32], [obf])


def psum_banks(k):
    banks = [k.psum(f"pb{i}", [128, 512], F32) for i in range(8)]
    bbufs = [k.em.buf(f"psbank{i}") for i in range(8)]

    def reg(b, c0, c1, parts=128, name=None):
        return T(banks[b][0:parts, c0:c1], bbufs[b], excl=True)
    return banks, reg


def load_w(k, q, wt, dram, r0, r1, c0, c1, kcn):
    src = dram[r0:r1, c0:c1].rearrange("(kc p) n -> p kc n", p=128)
    k.dma(q, wt[:, 0:kcn, 0:c1 - c0], src, wt)


def mixer_phase(k, I, cst, cols, banks, reg, xT, xpT, s_xln1):
    w_in, wsT_d, alng, alnb, wup_d, w_out0 = I["w_in"], I["wsT"], I["alng"], I["alnb"], I["wup"], I["w_out0"]
    B0, B1, B2, B4 = reg(0, 0, 512), reg(1, 0, 512), reg(2, 0, 512), reg(4, 0, 512)
    B2bf = T(banks[2][:, :].bitcast(BF16), B2.b, excl=True)
    Rk, Rz = reg(3, 0, 256), reg(3, 256, 512)
    Rd, Rgl = reg(5, 0, 256), reg(5, 256, 384, 16)
    RbT = [reg(2 * h, 0, 128, 64) for h in range(4)]; RqT = [reg(2 * h, 128, 256, 64) for h in range(4)]
    RkT = [reg(2 * h, 256, 384, 64) for h in range(4)]; Rsc = [reg(2 * h, 384, 512) for h in range(4)]
    Rkv1 = RbT
    RoT = [reg(2 * h + 1, 0, 128) for h in range(4)]; Rss = [reg(2 * h + 1, 128, 256) for h in range(4)]
    RrT = [reg(2 * h + 1, 256, 384) for h in range(4)]; Rkv0 = [reg(2 * h + 1, 384, 512, 64) for h in range(4)]

    k.phase()
    win = k.sb("win", [128, 8, 2576], BF16)
    k.dma("pool", win[:], w_in.rearrange("(kc p) n -> p kc n", p=128), win)
    wout = k.sb("wout", [128, 8, 1024], BF16); load_w(k, "pool", wout, w_out0, 0, 1024, 0, 1024, 8)
    wsT = k.sb("wsT", [128, 8, 128], BF16)
    k.dma("pool", wsT[:], wsT_d.rearrange("g s t -> s g t"), wsT)
    for g in range(8):
        k.tt("dve", wsT[:, g, :], wsT[:, g, :], cst["trib"][:], ALU.mult, [wsT, cst["trib"]], [wsT])
    glng = k.sb("glng", [128, 512]); k.dma("sp", glng[:], alng, glng)
    glnb = k.sb("glnb", [128, 512]); k.dma("sp", glnb[:], alnb, glnb)
    wup = k.sb("wup", [32, 256], BF16); k.dma("pool", wup[:], wup_d, wup)
    glT = k.sb("glT", [32, 128], BF16); k.memset("dve", glT[:], 1.0, [glT])
    S32 = [[k.sb(f"S32_{h}_{i}", [64, 128]) for i in range(2)] for h in range(4)]
    Sbf = [[k.sb(f"Sbf_{h}_{i}", [64, 128], BF16) for i in range(2)] for h in range(4)]
    for h in range(4):
        k.memset("dve", S32[h][0][:], 0.0, [S32[h][0]])
    xb = [k.sb(f"xb{i}", [128, 8, 512], BF16) for i in range(2)]
    x32 = k.sb("x32", [128, 8, 512])
    mixT = k.sb("mixT", [128, 8, 512], BF16)
    u_sb = k.sb("u_sb", [128, 512], BF16); v_sb = k.sb("v_sb", [128, 512]); vq = k.sb("vq", [128, 512])
    vln = k.sb("vln", [128, 512], BF16); ya = k.sb("ya", [128, 512], BF16); vb_sb = k.sb("vb_sb", [128, 512], BF16)
    st = k.sb("st", [128, 8]); la = k.sb("la", [128, 256]); ekl = k.sb("ekl", [128, 256])
    klf = k.sb("klf", [128, 256]); kl0 = k.sb("kl0", [128, 256], BF16); kl1 = k.sb("kl1", [128, 256], BF16)
    k.memset("dve", kl0[:], 0.0, [kl0]); k.memset("dve", kl1[:], 0.0, [kl1])
    H4 = range(4)
    bT_sb = [k.sb(f"bT_sb{h}", [64, 128]) for h in H4]; nref = [k.sb(f"nref{h}", [64, 2]) for h in H4]
    eA = [k.sb(f"eA{h}", [64, 128]) for h in H4]; eB = [k.sb(f"eB{h}", [64, 128]) for h in H4]
    eI = [k.sb(f"eI{h}", [64, 128]) for h in H4]; qd = [k.sb(f"qd{h}", [64, 128], BF16) for h in H4]
    kd = [k.sb(f"kd{h}", [64, 128], BF16) for h in H4]; qi = [k.sb(f"qi{h}", [64, 128], BF16) for h in H4]
    sc_sb = [k.sb(f"sc_sb{h}", [128, 128], BF16) for h in H4]; osq = [k.sb(f"osq{h}", [128, 128], BF16) for h in H4]
    rs_sb = [k.sb(f"rs_sb{h}", [128, 128]) for h in H4]; on = [k.sb(f"on{h}", [128, 128]) for h in H4]
    sr = [k.sb(f"sr{h}", [128, 128]) for h in H4]
    y = k.sb("y", [128, 8, 512])
    tmp = dict(ybf=k.sb("ybf", [128, 8, 512], BF16), ysq=k.sb("ysq", [128, 8, 512], BF16), mean=k.sb("mean", [128, 512]),
               rstd=k.sb("rstd", [128, 512]), msq=k.sb("msq", [128, 512]))
    bn0 = COLS["b_norm_g"][0]; bs0 = COLS["bsT"][0]; fl0 = COLS["flag"][0]

    def gla_tok(xbt, c0):
        gla_tok_a(xbt, c0); gla_tok_b(); gla_tok_c()

    def gla_tok_a(xbt, c0):
        for kc in range(8):
            k.mm(Rk[:], xbt[:, kc, c0:c0 + 128], win[:, kc, 1280:1536], kc == 0, kc == 7, [xbt, win], [Rk])
        for kc in range(8):
            k.mm(B4[:], xbt[:, kc, c0:c0 + 128], win[:, kc, 1536:2048], kc == 0, kc == 7, [xbt, win], [B4])
        for kc in range(8):
            k.mm(Rgl[:], win[:, kc, 2560:2576], xbt[:, kc, c0:c0 + 128], kc == 0, kc == 7, [xbt, win], [Rgl])

    def gla_tok_b():
        k.copy("dve", glT[0:16, :], Rgl[:], [Rgl], [glT])
        k.copy("act", vb_sb[:], B4[:], [B4], [vb_sb])
        k.mm(Rz[:], glT[:], wup[:], True, True, [glT, wup], [Rz])
        k.act(la[:], Rz[:], AF.Exp, [Rz], [la], scale=-1.0)
        k.act(la[:], la[:], AF.Ln, [la], [la], bias=1.0)
        k.mm(Rd[:], cst["dmat"][:], la[:], True, True, [la, cst["dmat"]], [Rd])
        k.act(ekl[:], Rd[:], AF.Exp, [Rd], [ekl])

    def gla_tok_c():
        k.tt("dve", kl0[0:64, :], Rk[0:64, :], ekl[0:64, :], ALU.mult, [Rk, ekl], [kl0])
        k.tt("dve", kl1[64:128, :], Rk[64:128, :], ekl[64:128, :], ALU.mult, [Rk, ekl], [kl1])

    def gla_head_decay(h):
        k.mm(RbT[h][:], la[:, h * 64:(h + 1) * 64], cst["tris"][:], True, True, [la, cst["tris"]], [RbT[h]])
        k.copy("dve", bT_sb[h][:], RbT[h][:], [RbT[h]], [bT_sb[h]])
        k.act(eI[h][:], bT_sb[h][:], AF.Exp, [bT_sb[h]], [eI[h]])

    def gla_state(h):
        k.mm(Rkv0[h][:], kl0[:, h * 64:(h + 1) * 64], vb_sb[:, h * 128:(h + 1) * 128], True, True, [kl0, vb_sb], [Rkv0[h]])
        k.mm(Rkv1[h][:], kl1[:, h * 64:(h + 1) * 64], vb_sb[:, h * 128:(h + 1) * 128], True, True, [kl1, vb_sb], [Rkv1[h]])
        k.stt("dve", S32[h][1][:], S32[h][0][:], eI[h][:, 63:64], Rkv0[h][:], ALU.mult, ALU.add,
              [S32[h][0], eI[h], Rkv0[h]], [S32[h][1]])
        k.copy("act", Sbf[h][1][:], S32[h][1][:], [S32[h][1]], [Sbf[h][1]])
        k.stt("dve", S32[h][0][:], S32[h][1][:], eI[h][:, 127:128], Rkv1[h][:], ALU.mult, ALU.add,
              [S32[h][1], eI[h], Rkv1[h]], [S32[h][0]])

    for g in range(4 if xpT is not None else 0):
        xbt = xb[g % 2]
        k.dma("pool", xbt[:], xpT[:, g * 512:(g + 1) * 512].rearrange("(kc p) t -> p kc t", p=128), xbt)
        for tt in range(4):
            gla_tok(xbt, tt * 128)
            for h in range(4):
                gla_head_decay(h)
                gla_state(h)
    for h in range(4):
        k.ts("dve", S32[h][0][:], S32[h][0][:], cols[0:64, fl0:fl0 + 1], None, ALU.mult, None, [S32[h][0], cols], [S32[h][0]])
        k.copy("act", Sbf[h][0][:], S32[h][0][:], [S32[h][0]], [Sbf[h][0]])

    for g in range(4):
        xbt = xb[g % 2]
        gs = slice(g * 512, (g + 1) * 512)
        k.dma("pool", xbt[:], xT[:, gs].rearrange("(kc p) t -> p kc t", p=128), xbt)
        k.dma("sp", x32[:], xT[:, gs].rearrange("(kc p) t -> p kc t", p=128), x32)
        for tt in range(4):
            c0 = tt * 128
            cs = slice(c0, c0 + 128)
            for kc in range(8):
                k.mm(B0[:], xbt[:, kc, cs], win[:, kc, 0:512], kc == 0, kc == 7, [xbt, win], [B0])
            for kc in range(8):
                k.mm(B1[:], xbt[:, kc, cs], win[:, kc, 512:1024], kc == 0, kc == 7, [xbt, win], [B1])
            gla_tok_a(xbt, c0)
            k.act(u_sb[:], B0[:], AF.Gelu_apprx_tanh, [B0], [u_sb])
            k.act(v_sb[:], B1[:], AF.Gelu_apprx_tanh, [B1], [v_sb])
            gla_tok_b()
            k.op("dve", lambda e: e.reduce_sum(out=st[:, 0:1], in_=v_sb[:], axis=AX.X), [v_sb], [st])
            k.tt("dve", vq[:], v_sb[:], v_sb[:], ALU.mult, [v_sb], [vq])
            k.op("dve", lambda e: e.reduce_sum(out=st[:, 1:2], in_=vq[:], axis=AX.X), [vq], [st])
            k.ts("dve", st[:, 2:3], st[:, 0:1], 1.0 / 512, None, ALU.mult, None, [st], [st])
            k.tt("dve", st[:, 3:4], st[:, 2:3], st[:, 2:3], ALU.mult, [st], [st])
            k.stt("dve", st[:, 4:5], st[:, 1:2], 1.0 / 512, st[:, 3:4], ALU.mult, ALU.subtract, [st], [st])
            k.rsqrt(st[:, 5:6], st[:, 4:5], 1.0, EPS, [st], [st])
            k.stt("dve", st[:, 6:7], st[:, 2:3], -1.0, st[:, 5:6], ALU.mult, ALU.mult, [st], [st])
            k.ts("dve", vq[:], v_sb[:], st[:, 5:6], st[:, 6:7], ALU.mult, ALU.add, [v_sb, st], [vq])
            k.tt("dve", vq[:], vq[:], glng[:], ALU.mult, [vq, glng], [vq])
            k.tt("dve", vln[:], vq[:], glnb[:], ALU.add, [vq, glnb], [vln])
            gla_tok_c()
            for h in H4:
                gla_head_decay(h)
            for gg in range(8):
                fs = slice(gg * 64, (gg + 1) * 64)
                k.mm(B2[:, fs], wsT[:, gg, :], vln[:, fs], True, True, [wsT, vln], [B2])
            for gg in range(8):
                fs = slice(gg * 64, (gg + 1) * 64)
                k.stt("dve", ya[:, fs], B2[:, fs], cols[:, bs0 + gg:bs0 + gg + 1], u_sb[:, fs], ALU.add, ALU.mult,
                      [B2, cols, u_sb], [ya])
            for i in range(4):
                k.op("pe", lambda e: e.transpose(B2bf[:, i * 128:(i + 1) * 128], ya[:, i * 128:(i + 1) * 128], cst["ident"][:]),
                     [ya, cst["ident"]], [B2bf])
            k.copy("act", mixT[:, 0:4, cs], B2bf[:, 0:512].rearrange("p (a b) -> p a b", a=4), [B2bf], [mixT])
            for h in H4:
                k.ts("dve", nref[h][:, 0:1], bT_sb[h][:, 31:32], -1.0, None, ALU.mult, None, [bT_sb[h]], [nref[h]])
                k.ts("dve", nref[h][:, 1:2], bT_sb[h][:, 95:96], -1.0, None, ALU.mult, None, [bT_sb[h]], [nref[h]])
            for h in H4:
                for c in range(2):
                    hs = slice(c * 64, (c + 1) * 64)
                    k.act(eA[h][:, hs], bT_sb[h][:, hs], AF.Exp, [bT_sb[h], nref[h]], [eA[h]], bias=nref[h][:, c:c + 1], scale=1.0)
                    k.act(eB[h][:, hs], bT_sb[h][:, hs], AF.Exp, [bT_sb[h]], [eB[h]],
                          bias=bT_sb[h][:, 31 + 64 * c:32 + 64 * c], scale=-1.0)
            for h in H4:
                for kc in range(8):
                    k.mm(RqT[h][:], win[:, kc, 1024 + h * 64:1088 + h * 64], xbt[:, kc, cs], kc == 0, kc == 7, [xbt, win], [RqT[h]])
                for kc in range(8):
                    k.mm(RkT[h][:], win[:, kc, 1280 + h * 64:1344 + h * 64], xbt[:, kc, cs], kc == 0, kc == 7, [xbt, win], [RkT[h]])
            for h in H4:
                k.stt("dve", qd[h][:], RqT[h][:], 0.125, eA[h][:], ALU.mult, ALU.mult, [RqT[h], eA[h]], [qd[h]])
                k.tt("dve", kd[h][:], RkT[h][:], eB[h][:], ALU.mult, [RkT[h], eB[h]], [kd[h]])
                k.stt("dve", qi[h][:], RqT[h][:], 0.125, eI[h][:], ALU.mult, ALU.mult, [RqT[h], eI[h]], [qi[h]])
            for h in H4:
                k.mm(Rsc[h][:], kd[h][:], qd[h][:], True, True, [kd[h], qd[h]], [Rsc[h]])
            for h in H4:
                k.tt("dve", sc_sb[h][:], Rsc[h][:], cst["triblk"][:], ALU.mult, [Rsc[h], cst["triblk"]], [sc_sb[h]])
            for h in H4:
                gla_state(h)
            for h in H4:
                k.mm(RoT[h][:], vb_sb[:, h * 128:(h + 1) * 128], sc_sb[h][:], True, False, [vb_sb, sc_sb[h]], [RoT[h]])
                k.mm(RoT[h][:, 0:64], Sbf[h][0][:], qi[h][:, 0:64], False, False, [Sbf[h][0], qi[h]], [RoT[h]])
                k.mm(RoT[h][:, 64:128], Sbf[h][1][:], qi[h][:, 64:128], False, True, [Sbf[h][1], qi[h]], [RoT[h]])
            for h in H4:
                k.copy("act", Sbf[h][0][:], S32[h][0][:], [S32[h][0]], [Sbf[h][0]])
                k.act(osq[h][:], RoT[h][:], AF.Square, [RoT[h]], [osq[h]])
            for h in H4:
                k.mm(Rss[h][:], cst["ones"][:], osq[h][:], True, True, [osq[h], cst["ones"]], [Rss[h]])
            for h in H4:
                k.op("act", lambda e: e.activation(out=rs_sb[h][:], in_=Rss[h][:], func=AF.Sqrt, bias=EPS, scale=1.0 / 128),
                     [Rss[h]], [rs_sb[h]])
            for h in H4:
                k.op("dve", lambda e: e.reciprocal(out=rs_sb[h][:], in_=rs_sb[h][:]), [rs_sb[h]], [rs_sb[h]])
                k.tt("dve", on[h][:], RoT[h][:], rs_sb[h][:], ALU.mult, [RoT[h], rs_sb[h]], [on[h]])
            for h in H4:
                for kc in range(8):
                    k.mm(RrT[h][:], win[:, kc, 2048 + h * 128:2176 + h * 128], xbt[:, kc, cs], kc == 0, kc == 7, [xbt, win], [RrT[h]])
            for h in H4:
                k.act(sr[h][:], RrT[h][:], AF.Silu, [RrT[h]], [sr[h]])
            for h in H4:
                k.stt("dve", mixT[:, 4 + h, cs], on[h][:], cols[:, bn0:bn0 + 1], sr[h][:], ALU.mult, ALU.mult,
                      [on[h], cols, sr[h]], [mixT])
        for nb in range(8):
            pb = B0 if nb % 2 == 0 else B1
            for ch in range(8):
                k.mm(pb[:], wout[:, ch, nb * 128:(nb + 1) * 128], mixT[:, ch, :], ch == 0, ch == 7, [wout, mixT], [pb])
            k.stt("dve", y[:, nb, :], x32[:, nb, :], ALPHA, pb[:], ALU.mult, ALU.add, [x32, pb], [y])
        ln_fm(k, cst, y, B0, B1, "e_ln1_g", "e_ln1_b", cols, y, None, 512, tmp)
        k.dma("sp", s_xln1[:, gs].rearrange("(kc p) t -> p kc t", p=128), y[:], s_xln1, [y])
    k.end_phase()


def ffn_phase(k, cst, cols, B0, B1, B2, B3, src, dst, wg_d, wu_d, wd_d, NJ, lng, lnb, wp_d, wgate_d, pT_d, pbias):
    k.phase()
    TG = 1024
    xb = k.sb("f_xb", [128, 8, TG], BF16); x32 = k.sb("f_x32", [128, 8, TG])
    hT = k.sb("f_hT", [128, NJ, TG], BF16)
    wgt = [k.sb(f"f_wg{i}", [128, 8, 256], BF16) for i in range(4)]
    wut = [k.sb(f"f_wu{i}", [128, 8, 256], BF16) for i in range(4)]
    wdt = [k.sb(f"f_wd{i}", [128, NJ, 256], BF16) for i in range(2)]
    sg = [k.sb(f"f_sg{i}", [128, 512], BF16) for i in range(2)]
    wgate = k.sb("f_wgate", [128, 8, 1024], BF16); wproj = k.sb("f_wproj", [128, 2, 1024], BF16)
    load_w(k, "pool", wgate, wgate_d, 0, 1024, 0, 1024, 8)
    load_w(k, "pool", wproj, wp_d, 0, 256, 0, 1024, 2)
    pTb = k.sb("f_pTb", [128, 2, 512], BF16); x2bf = k.sb("f_x2bf", [128, 8, 512], BF16)
    gt = k.sb("f_gt", [128, 512]);
    tmp = dict(ybf=k.sb("ybf", [128, 8, 512], BF16), ysq=k.sb("ysq", [128, 8, 512], BF16), mean=k.sb("mean", [128, 512]),
               rstd=k.sb("rstd", [128, 512]), msq=k.sb("msq", [128, 512]))
    pb0 = COLS[pbias][0]
    NJB = (NJ + 1) // 2
    for G in range(NT // TG):
        gs = slice(G * TG, (G + 1) * TG)
        k.dma("pool", xb[:], src[:, gs].rearrange("(kc p) t -> p kc t", p=128), xb, [src])
        k.dma("sp", x32[:], src[:, gs].rearrange("(kc p) t -> p kc t", p=128), x32, [src])
        for jb in range(NJB):
            nj = min(2, NJ - jb * 2)
            a, b = wgt[jb % 4], wut[jb % 4]
            load_w(k, "pool", a, wg_d, 0, 1024, jb * 256, jb * 256 + nj * 128, 8)
            load_w(k, "pool", b, wu_d, 0, 1024, jb * 256, jb * 256 + nj * 128, 8)
            for jj in range(nj):
                j = jb * 2 + jj
                for tg in range(TG // 512):
                    ts_ = slice(tg * 512, (tg + 1) * 512)
                    pg, pu = (B0, B1) if (j * 2 + tg) % 2 == 0 else (B2, B3)
                    s_ = sg[(j * 2 + tg) % 2]
                    for kc in range(8):
                        k.mm(pg[:], a[:, kc, jj * 128:(jj + 1) * 128], xb[:, kc, ts_], kc == 0, kc == 7, [a, xb], [pg])
                    for kc in range(8):
                        k.mm(pu[:], b[:, kc, jj * 128:(jj + 1) * 128], xb[:, kc, ts_], kc == 0, kc == 7, [b, xb], [pu])
                    k.act(s_[:], pg[:], AF.Silu, [pg], [s_])
                    k.tt("dve", hT[:, j, ts_], pu[:], s_[:], ALU.mult, [pu, s_], [hT])
        for nbb in range(4):
            wd_ = wdt[nbb % 2]
            k.dma("pool", wd_[:], wd_d[:, nbb * 256:(nbb + 1) * 256].rearrange("(j p) n -> p j n", p=128), wd_)
            for nn in range(2):
                nb = nbb * 2 + nn
                for tg in range(TG // 512):
                    ts_ = slice(tg * 512, (tg + 1) * 512)
                    pb = B0 if (nb * 2 + tg) % 2 == 0 else B1
                    for j in range(NJ):
                        k.mm(pb[:], wd_[:, j, nn * 128:(nn + 1) * 128], hT[:, j, ts_], j == 0, j == NJ - 1, [wd_, hT], [pb])
                    k.stt("dve", x32[:, nb, ts_], x32[:, nb, ts_], ALPHA, pb[:], ALU.mult, ALU.add, [x32, pb], [x32])
        for tg in range(TG // 512):
            ts_ = slice(tg * 512, (tg + 1) * 512)
            yv = T(x32.h[:, :, ts_], x32.b)
            ln_fm(k, cst, yv, B0, B1, lng, lnb, cols, yv, x2bf, 512, tmp)
            t0 = G * TG + tg * 512
            k.dma("pool", pTb[:], pT_d[:, t0:t0 + 512].rearrange("(kc p) t -> p kc t", p=128), pTb)
            for nb in range(8):
                ns = slice(nb * 128, (nb + 1) * 128)
                pg, pp = (B0, B1) if nb % 2 == 0 else (B2, B3)
                for kc in range(8):
                    k.mm(pg[:], wgate[:, kc, ns], x2bf[:, kc, :], kc == 0, kc == 7, [wgate, x2bf], [pg])
                for kc in range(2):
                    k.mm(pp[:], wproj[:, kc, ns], pTb[:, kc, :], kc == 0, kc == 1, [wproj, pTb], [pp])
                k.act(gt[:], pg[:], AF.Sigmoid, [pg, cols], [gt], bias=cols[:, pb0 + nb:pb0 + nb + 1], scale=1.0)
                k.tt("dve", gt[:], gt[:], pp[:], ALU.mult, [gt, pp], [gt])
                k.tt("dve", yv[:, nb, :], yv[:, nb, :], gt[:], ALU.add, [yv, gt], [yv])
        k.dma("sp", dst[:, gs].rearrange("(kc p) t -> p kc t", p=128), x32[:], dst, [x32])
    k.end_phase()


def qkv_phase(k, B, src, w_qkv, qT_o, kT_o, v_o, koff):
    k.phase()
    wq = k.sb("wqkv", [128, 8, 3072], BF16)
    k.dma("pool", wq[:], w_qkv.rearrange("(kc p) n -> p kc n", p=128), wq)
    xb = [k.sb(f"q_xb{i}", [128, 8, 512], BF16) for i in range(2)]
    qs = k.sb("q_qs", [64, 16, 512], BF16); ks = k.sb("q_ks", [64, 16, 512], BF16); vs = k.sb("q_vs", [128, 4, 1024], BF16)
    n = 0
    for g in range(4):
        gs = slice(g * 512, (g + 1) * 512)
        xbt = xb[g % 2]
        k.dma("pool", xbt[:], src[:, gs].rearrange("(kc p) t -> p kc t", p=128), xbt, [src])
        for which, dstt in ((0, qs), (1, ks)):
            if which == 0 and qT_o is None:
                continue
            for hc in range(16):
                pb = B[n % 8]; n += 1
                c0 = which * 1024 + hc * 64
                for kc in range(8):
                    k.mm(pb[0:64, :], wq[:, kc, c0:c0 + 64], xbt[:, kc, :], kc == 0, kc == 7, [wq, xbt], [pb])
                k.copy("act" if n % 2 else "dve", dstt[:, hc, :], pb[0:64, :], [pb], [dstt])
        for tt in range(4):
            for hf in range(2):
                pb = B[n % 8]; n += 1
                for kc in range(8):
                    k.mm(pb[:], xbt[:, kc, tt * 128:(tt + 1) * 128], wq[:, kc, 2048 + hf * 512:2560 + hf * 512], kc == 0, kc == 7,
                         [wq, xbt], [pb])
                k.copy("act" if n % 2 else "dve", vs[:, tt, hf * 512:(hf + 1) * 512], pb[:], [pb], [vs])
        ko = slice(koff + g * 512, koff + (g + 1) * 512)
        if qT_o is not None:
            k.dma("sp", qT_o[:, :, gs].rearrange("a d t -> d a t"), qs[:], qT_o, [qs])
        k.dma("sp", kT_o[:, :, ko].rearrange("a d t -> d a t"), ks[:], kT_o, [ks])
        k.dma("sp", v_o[ko, :].rearrange("(tt p) f -> p tt f", p=128), vs[:], v_o, [vs])
    k.end_phase()


def _colslab(v):
    v = np.asarray(v, np.float32).reshape(-1, 128)
    return np.ascontiguousarray(v.T)


def make_cols(inp, h):
    cols = np.zeros((128, NCOLS), np.float32)

    def put(name, arr):
        c0, n = COLS[name]
        cols[:, c0:c0 + n] = arr
    put("e_ln1_g", _colslab(inp["e_ln1_g"][0])); put("e_ln1_b", _colslab(inp["e_ln1_b"][0]))
    put("e_ln2_g", _colslab(inp["e_ln2_g"][0])); put("e_ln2_b", _colslab(inp["e_ln2_b"][0]))
    put("o_ln1_g", _colslab(inp["o_ln1_g"][0])); put("o_ln1_b", _colslab(inp["o_ln1_b"][0]))
    put("o_ln2_g", _colslab(inp["o_ln2_g"][0])); put("o_ln2_b", _colslab(inp["o_ln2_b"][0]))
    put("ple_b0", _colslab(inp["ple_b_gate"][0])); put("ple_b1", _colslab(inp["ple_b_gate"][1]))
    put("b_norm_g", _colslab(inp["e_b_norm_g"][0]))
    put("bsT", np.ascontiguousarray(np.asarray(inp["e_a_bs"][0], np.float32).T))
    put("gsubc", _colslab(inp["o_subln_g"][0]))
    put("flag", np.full((128, 1), float(h), np.float32))
    put("nbias", np.full((128, 1), 0.0 if h == 1 else -30000.0, np.float32))
    return cols


def l1_inputs(inp, c):
    b, h = c // 2, c % 2
    x = np.asarray(inp["x"], np.float32)
    p = np.asarray(inp["p"], np.float32)
    sl = slice(h * NT, (h + 1) * NT)
    wup = np.zeros((32, 256), np.float32)
    wup[0:16] = inp["e_b_w_gate_up"][0]
    wup[16] = inp["e_b_b_gate"][0]
    return {
        "xT": np.ascontiguousarray(x[b, sl].T), "xpT": np.ascontiguousarray(x[b, 0:NT].T),
        "p0T": np.ascontiguousarray(p[0, b, sl].T), "cols": make_cols(inp, h),
        "w_in": np.asarray(inp["e_w_in"][0], np.float32),
        "wsT": np.ascontiguousarray(np.asarray(inp["e_a_ws"][0], np.float32).transpose(0, 2, 1)),
        "alng": np.ascontiguousarray(np.broadcast_to(np.asarray(inp["e_a_ln_g"][0], np.float32), (128, 512))),
        "alnb": np.ascontiguousarray(np.broadcast_to(np.asarray(inp["e_a_ln_b"][0], np.float32), (128, 512))),
        "wup": wup, "w_out0": np.asarray(inp["e_w_out"][0], np.float32),
        "ffn_wg": np.asarray(inp["e_ffn_wg"][0], np.float32), "ffn_wu": np.asarray(inp["e_ffn_wu"][0], np.float32),
        "ffn_wd": np.asarray(inp["e_ffn_wd"][0], np.float32),
        "wp0": np.asarray(inp["ple_w_proj"][0], np.float32), "wg0": np.asarray(inp["ple_w_gate"][0], np.float32),
        "w_qkv": np.asarray(inp["o_w_qkv"][0], np.float32),
    }


LAMBDA_INIT = 0.8 - 0.6 * float(np.exp(-0.3 * 1))
CAP = 256


def l2_phases(k, I, cst, cols, banks, reg, s_x1, s_q, s_kT, s_v, outT):
    lam_d, gsub_d, wo_d, wr_d = I["lamv"], I["gsub"], I["w_out1"], I["w_router"]
    ewg, ewu, ewd, p1T, wp1, wg1 = I["exp_wg"], I["exp_wu"], I["exp_wd"], I["p1T"], I["wp1"], I["wg1"]
    s_oT = k.dscr("s_oT", [1024, NT], BF16); s_xl = k.dscr("s_xl", [1024, NT])
    s_xg = k.dscr("s_xg", [8, 1024, 4 * CAP], BF16); s_yg = k.dscr("s_yg", [8, 4 * CAP, 1024], BF16)
    s_PT = k.dscr("s_PT", [4, 128, 8 * 2 * 512], BF16)
    B = [reg(i, 0, 512) for i in range(8)]
    gsl = k.sb("gsl", [128, 8, 8])
    nb0 = COLS["nbias"][0]

    k.phase()
    lamv = k.sb("lamv", [128, 4, 64]); k.dma("sp", lamv[:], lam_d, lamv)
    lt = k.sb("lt", [128, 8]); lp = k.sb("lp", [128, 64])
    for i in range(2):
        k.tt("dve", lp[:], lamv[:, 2 * i, :], lamv[:, 2 * i + 1, :], ALU.mult, [lamv], [lp])
        k.op("dve", lambda e: e.reduce_sum(out=lt[:, i:i + 1], in_=lp[:], axis=AX.X), [lp], [lt])
    k.act(lt[:, 2:4], lt[:, 0:2], AF.Exp, [lt], [lt])
    k.tt("dve", lt[:, 4:5], lt[:, 3:4], lt[:, 2:3], ALU.subtract, [lt], [lt])
    k.ts("dve", lt[:, 5:6], lt[:, 4:5], -LAMBDA_INIT, None, ALU.add, None, [lt], [lt])
    Kt = [k.sb(f"Kt{i}", [128, 2 * NT], BF16) for i in range(2)]
    Vt = [k.sb(f"Vt{i}", [128, 32, 128], BF16) for i in range(2)]
    Qz = [[k.sb(f"Qz{i}_{c}", [128, NT], BF16) for c in range(2)] for i in range(2)]
    for i in range(2):
        for c in range(2):
            k.memset("dve", Qz[i][c][:], 0.0, [Qz[i][c]])
    Et = [k.sb(f"Et{i}", [128, 512], BF16) for i in range(6)]
    rs = [k.sb(f"rs{c}", [128, 512]) for c in range(2)]; Oc = [k.sb(f"Oc{c}", [128, 512]) for c in range(2)]
    t1 = k.sb("t1", [128, 512]); od = k.sb("od", [128, 512])
    osq = k.sb("a_osq", [128, 512], BF16); rstd = k.sb("a_rstd", [128, 512]); onb = k.sb("onb", [128, 512], BF16)
    gc0 = COLS["gsubc"][0]
    k.ts("dve", lt[:, 6:7], cols[:, gc0:gc0 + 1], 1.0 - LAMBDA_INIT, None, ALU.mult, None, [cols], [lt])
    Oa = [B[2], B[3]]; Sm = [B[4], B[5]]
    Bs = B[7]; Sbanks = [B[0], B[1], B[6]]
    for h in range(8):
        Kh, Vh, Qh = Kt[h % 2], Vt[h % 2], Qz[h % 2]
        k.dma("sp", Kh[:], s_kT[2 * h:2 * h + 2].rearrange("c d t -> (c d) t"), Kh, [s_kT])
        k.dma("sp", Vh[:], s_v[:, h * 128:(h + 1) * 128].rearrange("(tt p) f -> p tt f", p=128), Vh, [s_v])
        k.dma("sp", Qh[0][0:64, :], s_q[2 * h], Qh[0], [s_q])
        k.dma("sp", Qh[1][64:128, :], s_q[2 * h + 1], Qh[1], [s_q])
        blocks = [(g, kb, c) for g in range(4) for kb in range(16 + 4 * (g + 1)) for c in range(2)]

        def emit_S(i):
            g, kb, c = blocks[i]
            m = kb - (16 + 4 * g)
            n0 = 128 * m if m > 0 else 0
            Sb = Sbanks[i % 3]
            k.mm(Sb[:, n0:512], Kh[:, kb * 128:(kb + 1) * 128], Qh[c][:, g * 512 + n0:(g + 1) * 512], True, True,
                 [Kh, Qh[c]], [Sb])

        for j in range(2):
            emit_S(j)
        for i, (g, kb, c) in enumerate(blocks):
            if i + 2 < len(blocks):
                emit_S(i + 2)
            m = kb - (16 + 4 * g)
            n0 = 128 * m if m > 0 else 0
            last = 16 + 4 * g + 3
            Sb = Sbanks[i % 3]; E = Et[i % 6]
            if kb < 16:
                k.act(E[:, n0:512], Sb[:, n0:512], AF.Exp, [Sb, cols], [E], bias=cols[:, nb0:nb0 + 1], scale=0.125)
            else:
                k.act(E[:, n0:512], Sb[:, n0:512], AF.Exp, [Sb], [E], scale=0.125)
            if m >= 0:
                k.tt("dve", E[:, n0:n0 + 128], E[:, n0:n0 + 128], cst["trib"][:], ALU.mult, [E, cst["trib"]], [E])
            k.mm(Oa[c][:, n0:512], Vh[:, kb, :], E[:, n0:512], kb == 0, kb == last, [Vh, E], [Oa[c]])
            k.mm(Sm[c][:, n0:512], cst["ones"][:], E[:, n0:512], kb == 0, kb == last, [cst["ones"], E], [Sm[c]])
            if kb == last and c == 1:
                for cc in range(2):
                    k.op("dve", lambda e: e.reciprocal(out=rs[cc][:], in_=Sm[cc][:]), [Sm[cc]], [rs[cc]])
                    k.copy("act", Oc[cc][:], Oa[cc][:], [Oa[cc]], [Oc[cc]])
                k.tt("pool", t1[:], Oc[0][:], rs[0][:], ALU.mult, [Oc[0], rs[0]], [t1])
                k.tt("dve", od[:], Oc[1][:], rs[1][:], ALU.mult, [Oc[1], rs[1]], [od])
                k.stt("dve", od[:], od[:], lt[:, 5:6], t1[:], ALU.mult, ALU.add, [od, lt, t1], [od])
                k.tt("pool", osq[:], od[:], od[:], ALU.mult, [od], [osq])
                k.mm(Bs[:], cst["ones"][:], osq[:], True, True, [cst["ones"], osq], [Bs])
                k.rsqrt(rstd[:], Bs[:], 1.0 / 128, EPS, [Bs], [rstd])
                k.stt("dve", onb[:], od[:], lt[:, 6:7], rstd[:], ALU.mult, ALU.mult, [od, lt, rstd], [onb])
                k.dma("sp", s_oT[h * 128:(h + 1) * 128, g * 512:(g + 1) * 512], onb[:], s_oT, [onb])
    k.end_phase()

    k.phase()
    wo = k.sb("wo", [128, 8, 1024], BF16); load_w(k, "pool", wo, wo_d, 0, 1024, 0, 1024, 8)
    wr = k.sb("wr", [128, 8, 8]); k.dma("sp", wr[:], wr_d.rearrange("(kc p) e -> p kc e", p=128), wr)
    oTs = k.sb("oTs", [128, 8, 512], BF16); x32 = k.sb("r_x32", [128, 8, 512]); xbf = k.sb("r_xbf", [128, 8, 512], BF16)
    tmp = dict(ybf=k.sb("ybf", [128, 8, 512], BF16), ysq=k.sb("ysq", [128, 8, 512], BF16), mean=k.sb("mean", [128, 512]),
               rstd=k.sb("rstd", [128, 512]), msq=k.sb("msq", [128, 512]))
    xtok = k.sb("xtok", [128, 4, 1024], BF16)
    lg = k.sb("lg", [128, 4, 8]); m8 = k.sb("m8", [128, 4, 8]); sel = k.sb("sel", [128, 4, 8]); is1 = k.sb("is1", [128, 4, 8])
    selb = k.sb("selb", [128, 32], BF16); gw = k.sb("gw", [128, 4, 8]); gq = k.sb("gq", [128, 8])
    off = k.sb("off", [128, 4, 8]); d1 = k.sb("d1", [128, 4, 8]); ghl = k.sb("ghl", [128, 4, 8, 2], BF16)
    ghi32 = k.sb("ghi32", [128, 4, 8]); g2t = k.sb("g2t", [128, 2])
    io_i = k.sb("io_i", [128, CAP], I32); io1 = k.sb("io1", [128, CAP])
    k.op("pool", lambda e: e.iota(io_i[:], pattern=[[1, CAP]], base=1, channel_multiplier=0), (), [io_i])
    k.copy("dve", io1[:], io_i[:], [io_i], [io1])
    tris_bf = k.sb("tris_bf", [128, 128], BF16)
    k.tt("dve", tris_bf[:], cst["tri"][:], cst["idf"][:], ALU.subtract, [cst["tri"], cst["idf"]], [tris_bf])
    P = k.sb("P", [128, 4, 8, CAP], BF16)
    xg = k.sb("xg", [128, 8, CAP], BF16); PTs = k.sb("PTs", [128, 8, 2, 512], BF16)
    B7bf = T(banks[7][:, :].bitcast(BF16), B[7].b, excl=True)
    B6bf = T(banks[6][:, :].bitcast(BF16), B[6].b, excl=True)
    for g in range(4):
        gs = slice(g * 512, (g + 1) * 512)
        k.dma("sp", oTs[:], s_oT[:, gs].rearrange("(kc p) t -> p kc t", p=128), oTs, [s_oT])
        k.dma("sp", x32[:], s_x1[:, gs].rearrange("(kc p) t -> p kc t", p=128), x32, [s_x1])
        for nb in range(8):
            pb = B[nb % 2]
            for ch in range(8):
                k.mm(pb[:], wo[:, ch, nb * 128:(nb + 1) * 128], oTs[:, ch, :], ch == 0, ch == 7, [wo, oTs], [pb])
            k.stt("dve", x32[:, nb, :], x32[:, nb, :], ALPHA, pb[:], ALU.mult, ALU.add, [x32, pb], [x32])
        ln_fm(k, cst, x32, B[0], B[1], "o_ln1_g", "o_ln1_b", cols, x32, xbf, 512, tmp)
        k.dma("sp", s_xl[:, gs].rearrange("(kc p) t -> p kc t", p=128), x32[:], s_xl, [x32])
        for tt in range(4):
            cs = slice(tt * 128, (tt + 1) * 128)
            for half in range(2):
                pbf = B6bf if half == 0 else B7bf
                for i in range(4):
                    ch = half * 4 + i
                    k.op("pe", lambda e: e.transpose(pbf[:, i * 128:(i + 1) * 128], xbf[:, ch, cs], cst["ident"][:]),
                         [xbf, cst["ident"]], [pbf])
                k.copy("act" if half else "dve", xtok[:, tt, half * 512:(half + 1) * 512], pbf[:, 0:512], [pbf], [xtok])
            for ch in range(8):
                k.mm(B[2][:, tt * 8:(tt + 1) * 8], x32[:, ch, cs], wr[:, ch, :], ch == 0, ch == 7, [x32, wr], [B[2]])
        k.copy("dve", lg[:].rearrange("p a b -> p (a b)"), B[2][:, 0:32], [B[2]], [lg])
        for tt in range(4):
            k.op("dve", lambda e: e.max(out=m8[:, tt, :], in_=lg[:, tt, :]), [lg], [m8])
            k.ts("dve", sel[:, tt, :], lg[:, tt, :], m8[:, tt, 1:2], None, ALU.is_ge, None, [lg, m8], [sel])
            k.ts("dve", is1[:, tt, :], lg[:, tt, :], m8[:, tt, 0:1], None, ALU.is_ge, None, [lg, m8], [is1])
        for tt in range(4):
            k.tt("dve", gq[:, tt:tt + 1], m8[:, tt, 1:2], m8[:, tt, 0:1], ALU.subtract, [m8], [gq])
        k.act(gq[:, 0:4], gq[:, 0:4], AF.Exp, [gq], [gq])
        k.ts("dve", gq[:, 4:8], gq[:, 0:4], 1.0, None, ALU.add, None, [gq], [gq])
        k.op("dve", lambda e: e.reciprocal(out=gq[:, 4:8], in_=gq[:, 4:8]), [gq], [gq])
        k.tt("dve", gq[:, 0:4], gq[:, 0:4], gq[:, 4:8], ALU.mult, [gq], [gq])
        for tt in range(4):
            k.ts("dve", gw[:, tt, :], sel[:, tt, :], gq[:, tt:tt + 1], None, ALU.mult, None, [sel, gq], [gw])
            k.tt("dve", off[:, 0, 0:1], gq[:, 4 + tt:5 + tt], gq[:, tt:tt + 1], ALU.subtract, [gq], [off])
            k.stt("dve", gw[:, tt, :], is1[:, tt, :], off[:, 0, 0:1], gw[:, tt, :], ALU.mult, ALU.add, [is1, off, gw], [gw])
        k.copy("dve", ghl[:, :, :, 0], gw[:], [gw], [ghl])
        k.copy("dve", ghi32[:], ghl[:, :, :, 0], [ghl], [ghi32])
        k.tt("dve", ghi32[:], gw[:], ghi32[:], ALU.subtract, [gw, ghi32], [ghi32])
        k.copy("dve", ghl[:, :, :, 1], ghi32[:], [ghi32], [ghl])
        k.copy("dve", selb[:], sel[:].rearrange("p a b -> p (a b)"), [sel], [selb])
        k.mm(B[2][:, 0:32], tris_bf[:], selb[:], True, True, [tris_bf, selb], [B[2]])
        k.mm(B[3][:, 0:32], cst["ones"][:], selb[:], True, True, [cst["ones"], selb], [B[3]])
        k.memset("dve", off[:, 0, :], 0.0, [off])
        for tt in range(1, 4):
            k.tt("dve", off[:, tt, :], off[:, tt - 1, :], B[3][:, (tt - 1) * 8:tt * 8], ALU.add, [off, B[3]], [off])
        k.tt("dve", d1[:].rearrange("p a b -> p (a b)"), off[:].rearrange("p a b -> p (a b)"), B[2][:, 0:32], ALU.add,
             [off, B[2]], [d1])
        k.stt("dve", d1[:], d1[:], 1.0, sel[:], ALU.add, ALU.mult, [d1, sel], [d1])
        for tt in range(4):
            for e in range(8):
                k.ts("dve", P[:, tt, e, :], io1[:], d1[:, tt, e:e + 1], None, ALU.is_equal, None, [io1, d1], [P])
        for e in range(8):
            for ch in range(8):
                pb = B[ch % 2]
                for tt in range(4):
                    k.mm(pb[:, 0:CAP], xtok[:, tt, ch * 128:(ch + 1) * 128], P[:, tt, e, :], tt == 0, tt == 3, [xtok, P], [pb])
                k.copy("act" if ch % 2 else "dve", xg[:, ch, :], pb[:, 0:CAP], [pb], [xg])
            k.dma("sp", s_xg[e, :, g * CAP:(g + 1) * CAP].rearrange("(kc p) s -> p kc s", p=128), xg[:], s_xg, [xg])
            for st in range(2):
                pbf = B6bf if st == 0 else B7bf
                for tt in range(4):
                    k.op("pe", lambda e_: e_.transpose(pbf[:, tt * 128:(tt + 1) * 128], P[:, tt, e, st * 128:(st + 1) * 128],
                                                       cst["ident"][:]), [P, cst["ident"]], [pbf])
                k.copy("act" if st else "dve", PTs[:, e, st, :], pbf[:, 0:512], [pbf], [PTs])
                for tt in range(4):
                    k.mm(B[4][:, 0:2], P[:, tt, e, st * 128:(st + 1) * 128], ghl[:, tt, e, :], tt == 0, tt == 3, [P, ghl], [B[4]])
                k.copy("dve", g2t[:], B[4][:, 0:2], [B[4]], [g2t])
                k.tt("dve", gsl[:, e, g * 2 + st:g * 2 + st + 1], g2t[:, 0:1], g2t[:, 1:2], ALU.add, [g2t], [gsl])
        k.dma("sp", s_PT[g].rearrange("p (a b c) -> p a b c", a=8, b=2), PTs[:], s_PT, [PTs])
    k.end_phase()
    expert_phase(k, B, s_xg, s_yg, ewg, ewu, ewd, gsl)
    combine_phase(k, cst, cols, B, s_yg, s_PT, s_xl, outT, wp1, wg1, p1T)
    k.em.wait_all("sp", _bufs([outT]))


def build_fused(k):
    names = [("xT", [1024, NT]), ("xpT", [1024, NT]), ("p0T", [256, NT]), ("p0pT", [256, NT]), ("p1T", [256, NT]),
             ("cols", [128, NCOLS]), ("w_in", [1024, 2576]), ("wsT", [8, 128, 128]), ("alng", [128, 512]),
             ("alnb", [128, 512]), ("wup", [32, 256]), ("w_out0", [1024, 1024]), ("ffn_wg", [1024, 2816]),
             ("ffn_wu", [1024, 2816]), ("ffn_wd", [2816, 1024]), ("wp0", [256, 1024]), ("wg0", [1024, 1024]),
             ("w_qkv", [1024, 3072]), ("lamv", [128, 4, 64]), ("gsub", [128, 128]), ("w_out1", [1024, 1024]),
             ("w_router", [1024, 8]), ("exp_wg", [8, 1024, 3584]), ("exp_wu", [8, 1024, 3584]),
             ("exp_wd", [8, 3584, 1024]), ("wp1", [256, 1024]), ("wg1", [1024, 1024])]
    I = {n: k.din(n, shp) for n, shp in names}
    outT = k.dout("outT", [1024, NT])
    s_xln1 = k.dscr("s_xln1", [1024, NT]); s_x1p = k.dscr("s_x1p", [1024, NT]); s_x1 = k.dscr("s_x1", [1024, NT])
    s_q = k.dscr("s_q", [16, 64, NT], BF16); s_kT = k.dscr("s_kT", [16, 64, 2 * NT], BF16)
    s_v = k.dscr("s_v", [2 * NT, 1024], BF16)
    cst = make_consts(k)
    banks, reg = psum_banks(k)
    cols = k.sb("cols", [128, NCOLS]); k.dma("sp", cols[:], I["cols"], cols)
    B = [reg(i, 0, 512) for i in range(8)]
    for (src, psrc, pref, x1dst, qdst, koff) in ((I["xpT"], I["p0pT"], None, s_x1p, None, 0),
                                                 (I["xT"], I["p0T"], I["xpT"], s_x1, s_q, NT)):
        mixer_phase(k, I, cst, cols, banks, reg, src, pref, s_xln1)
        ffn_phase(k, cst, cols, B[0], B[1], B[2], B[3], s_xln1, x1dst, I["ffn_wg"], I["ffn_wu"], I["ffn_wd"], 22,
                  "e_ln2_g", "e_ln2_b", I["wp0"], I["wg0"], psrc, "ple_b0")
        qkv_phase(k, B, x1dst, I["w_qkv"], qdst, s_kT, s_v, koff)
    l2_phases(k, I, cst, cols, banks, reg, s_x1, s_q, s_kT, s_v, outT)


def expert_phase(k, B, s_xg, s_yg, ewg, ewu, ewd, gsl):
    k.phase()
    NS = 4 * CAP
    NJ = 28
    xg = [k.sb(f"e_xg{i}", [128, 8, NS], BF16) for i in range(2)]
    hT = k.sb("e_hT", [128, NJ, NS], BF16)
    wgt = [k.sb(f"e_wg{i}", [128, 8, 256], BF16) for i in range(4)]
    wut = [k.sb(f"e_wu{i}", [128, 8, 256], BF16) for i in range(4)]
    wdt = [k.sb(f"e_wd{i}", [128, NJ, 256], BF16) for i in range(2)]
    sg = [k.sb(f"e_sg{i}", [128, 512], BF16) for i in range(2)]
    yg = k.sb("e_yg", [128, NS // 128, 1024], BF16)
    n = 0
    for e in range(8):
        xe = xg[e % 2]
        k.dma("sp", xe[:], s_xg[e].rearrange("(kc p) s -> p kc s", p=128), xe, [s_xg])
        for jb in range(NJ // 2):
            a, b = wgt[jb % 4], wut[jb % 4]
            load_w(k, "pool", a, ewg[e], 0, 1024, jb * 256, jb * 256 + 256, 8)
            load_w(k, "pool", b, ewu[e], 0, 1024, jb * 256, jb * 256 + 256, 8)
            for jj in range(2):
                j = jb * 2 + jj
                for tg in range(NS // 512):
                    ts_ = slice(tg * 512, (tg + 1) * 512)
                    pg, pu = (B[0], B[1]) if n % 2 == 0 else (B[2], B[3])
                    s_ = sg[n % 2]; n += 1
                    for kc in range(8):
                        k.mm(pg[:], a[:, kc, jj * 128:(jj + 1) * 128], xe[:, kc, ts_], kc == 0, kc == 7, [a, xe], [pg])
                    for kc in range(8):
                        k.mm(pu[:], b[:, kc, jj * 128:(jj + 1) * 128], xe[:, kc, ts_], kc == 0, kc == 7, [b, xe], [pu])
                    k.act(s_[:], pg[:], AF.Silu, [pg], [s_])
                    k.tt("dve", hT[:, j, ts_], pu[:], s_[:], ALU.mult, [pu, s_], [hT])
        for nbb in range(4):
            wd_ = wdt[nbb % 2]
            k.dma("pool", wd_[:], ewd[e][:, nbb * 256:(nbb + 1) * 256].rearrange("(j p) n -> p j n", p=128), wd_)
            for st in range(NS // 128):
                pb = B[4 + (st % 4)]
                for j in range(NJ):
                    k.mm(pb[:, 0:256], hT[:, j, st * 128:(st + 1) * 128], wd_[:, j, :], j == 0, j == NJ - 1, [hT, wd_], [pb])
                k.ts("dve", yg[:, st, nbb * 256:(nbb + 1) * 256], pb[:, 0:256], gsl[:, e, st:st + 1], None, ALU.mult, None,
                     [pb, gsl], [yg])
        k.dma("sp", s_yg[e].rearrange("(st p) f -> p st f", p=128), yg[:], s_yg, [yg])
    k.end_phase()


def combine_phase(k, cst, cols, B, s_yg, s_PT, s_xl, outT, wp_d, wgate_d, pT_d):
    k.phase()
    ygl = k.sb("c_ygl", [128, 8, 2, 1024], BF16); PT = k.sb("c_PT", [128, 8, 2, 512], BF16)
    x32 = k.sb("c_x32", [128, 8, 512]); x2bf = k.sb("c_x2bf", [128, 8, 512], BF16)
    wgate = k.sb("c_wgate", [128, 8, 1024], BF16); wproj = k.sb("c_wproj", [128, 2, 1024], BF16)
    load_w(k, "pool", wgate, wgate_d, 0, 1024, 0, 1024, 8)
    load_w(k, "pool", wproj, wp_d, 0, 256, 0, 1024, 2)
    pTb = k.sb("c_pTb", [128, 2, 512], BF16); gt = k.sb("c_gt", [128, 512])
    tmp = dict(ybf=k.sb("ybf", [128, 8, 512], BF16), ysq=k.sb("ysq", [128, 8, 512], BF16), mean=k.sb("mean", [128, 512]),
               rstd=k.sb("rstd", [128, 512]), msq=k.sb("msq", [128, 512]))
    pb0 = COLS["ple_b1"][0]
    for g in range(4):
        gs = slice(g * 512, (g + 1) * 512)
        for e in range(8):
            k.dma("sp", ygl[:, e, :, :], s_yg[e, g * CAP:(g + 1) * CAP, :].rearrange("(st p) f -> p st f", p=128), ygl, [s_yg])
        k.dma("sp", PT[:], s_PT[g].rearrange("p (a b c) -> p a b c", a=8, b=2), PT, [s_PT])
        k.dma("sp", x32[:], s_xl[:, gs].rearrange("(kc p) t -> p kc t", p=128), x32, [s_xl])
        for nb in range(8):
            pb = B[nb % 2]
            i = 0
            for e in range(8):
                for st in range(2):
                    k.mm(pb[:], ygl[:, e, st, nb * 128:(nb + 1) * 128], PT[:, e, st, :], i == 0, i == 15, [ygl, PT], [pb])
                    i += 1
            k.stt("dve", x32[:, nb, :], x32[:, nb, :], ALPHA, pb[:], ALU.mult, ALU.add, [x32, pb], [x32])
        ln_fm(k, cst, x32, B[0], B[1], "o_ln2_g", "o_ln2_b", cols, x32, x2bf, 512, tmp)
        k.dma("pool", pTb[:], pT_d[:, gs].rearrange("(kc p) t -> p kc t", p=128), pTb)
        for nb in range(8):
            ns = slice(nb * 128, (nb + 1) * 128)
            pg, pp = (B[0], B[1]) if nb % 2 == 0 else (B[2], B[3])
            for kc in range(8):
                k.mm(pg[:], wgate[:, kc, ns], x2bf[:, kc, :], kc == 0, kc == 7, [wgate, x2bf], [pg])
            for kc in range(2):
                k.mm(pp[:], wproj[:, kc, ns], pTb[:, kc, :], kc == 0, kc == 1, [wproj, pTb], [pp])
            k.act(gt[:], pg[:], AF.Sigmoid, [pg, cols], [gt], bias=cols[:, pb0 + nb:pb0 + nb + 1], scale=1.0)
            k.tt("dve", gt[:], gt[:], pp[:], ALU.mult, [gt, pp], [gt])
            k.tt("dve", x32[:, nb, :], x32[:, nb, :], gt[:], ALU.add, [x32, gt], [x32])
        k.dma("sp", outT[:, gs].rearrange("(kc p) t -> p kc t", p=128), x32[:], outT, [x32])
    k.end_phase()


def fused_inputs(inp, c):
    b, h = c // 2, c % 2
    d = l1_inputs(inp, c)
    p = np.asarray(inp["p"], np.float32)
    sl = slice(h * NT, (h + 1) * NT)
    lamv = np.stack([inp["o_lam_q1"][0], inp["o_lam_k1"][0], inp["o_lam_q2"][0], inp["o_lam_k2"][0]]).astype(np.float32)
    d.update({
        "p0pT": np.ascontiguousarray(p[0, b, 0:NT].T),
        "p1T": np.ascontiguousarray(p[1, b, sl].T),
        "lamv": np.ascontiguousarray(np.broadcast_to(lamv, (128, 4, 64))),
        "gsub": np.ascontiguousarray(np.broadcast_to(np.asarray(inp["o_subln_g"][0], np.float32), (128, 128))),
        "w_out1": np.asarray(inp["o_w_out"][0], np.float32), "w_router": np.asarray(inp["o_router"][0], np.float32),
        "exp_wg": np.asarray(inp["o_exp_wg"][0], np.float32), "exp_wu": np.asarray(inp["o_exp_wu"][0], np.float32),
        "exp_wd": np.asarray(inp["o_exp_wd"][0], np.float32),
        "wp1": np.asarray(inp["ple_w_proj"][1], np.float32), "wg1": np.asarray(inp["ple_w_gate"][1], np.float32),
    })
    return d


_CACHE = {}


def kernel(**inputs):
    inp = {k_: np.asarray(v) for k_, v in inputs.items()}
    if "k" not in _CACHE:
        kb = KB(); build_fused(kb); _CACHE["k"] = kb
    kb = _CACHE["k"]
    res = run_bass_kernel_spmd(kb.nc, [fused_inputs(inp, c) for c in range(8)], core_ids=list(range(8))).results
    out = np.empty((4, 2 * NT, 1024), np.float32)
    for c in range(8):
        b, h = c // 2, c % 2
        out[b, h * NT:(h + 1) * NT] = res[c]["outT"].T
    return out
```
